# Optimizing a Trainium2 kernel written in Bass

```python
import math
import jax, jax.numpy as jnp
from jax import lax
import numpy as np

D_MODEL = 1024
BATCH = 4
SEQ = 8192
DEPTH = 1

D_MIX = D_MODEL
D_ATTN = D_MIX // 2
D_SSM = D_MIX - D_ATTN
ATTN_HEADS = 4
ATTN_VDIM = D_ATTN // ATTN_HEADS
ATTN_QKDIM = ATTN_VDIM // 2
ROPE_DIM = ATTN_QKDIM // 4
ROPE_THETA = 500000.0
Q_BLOCK = 128
SSM_GROUP = 16
SSM_GROUPS = D_SSM // SSM_GROUP
SSM_STATE = 64
DT_MIN = 1e-3
DT_MAX = 1e-1
N_EXPERTS = 32
TOP_K = 4
D_FF = D_MODEL
SWIGLU_LIMIT = 7.0
SWIGLU_ALPHA = 1.702
MOE_BLOCK = 128
RMS_EPS = 1e-6

kernel_name = "hybrid_diffattn_s5_moe_layer"


def rmsnorm(x, g):
    xf = x.astype(jnp.float32)
    y = xf * lax.rsqrt(jnp.mean(xf * xf, axis=-1, keepdims=True) + RMS_EPS)
    return (y * g.astype(jnp.float32)).astype(x.dtype)


def partial_rope(t, positions):
    half = ROPE_DIM // 2
    inv_freq = ROPE_THETA ** (-jnp.arange(0, ROPE_DIM, 2, dtype=jnp.float32) / ROPE_DIM)
    ang = positions.astype(jnp.float32)[..., None] * inv_freq
    cos = jnp.cos(ang)[:, :, None, None, :]
    sin = jnp.sin(ang)[:, :, None, None, :]
    tf = t.astype(jnp.float32)
    t1 = tf[..., :half]
    t2 = tf[..., half:ROPE_DIM]
    rot = jnp.concatenate([t1 * cos - t2 * sin, t2 * cos + t1 * sin], axis=-1)
    return jnp.concatenate([rot, tf[..., ROPE_DIM:]], axis=-1)


def diff_attention(q, k, v, lam_q1, lam_k1, lam_q2, lam_k2, norm_g, lambda_init):
    bsz, s_len = q.shape[0], q.shape[1]
    scale = ATTN_QKDIM ** -0.5
    qf = jnp.transpose(q, (0, 2, 3, 1, 4)) * scale
    kf = jnp.transpose(k, (0, 2, 3, 1, 4))
    vf = jnp.transpose(v.astype(jnp.float32), (0, 2, 1, 3))
    lam = (jnp.exp(jnp.sum(lam_q1.astype(jnp.float32) * lam_k1.astype(jnp.float32)))
           - jnp.exp(jnp.sum(lam_q2.astype(jnp.float32) * lam_k2.astype(jnp.float32)))
           + lambda_init)
    outs = []
    for blk in range(s_len // Q_BLOCK):
        q0 = blk * Q_BLOCK
        kv_end = q0 + Q_BLOCK
        qb = qf[:, :, :, q0:kv_end]
        kb = kf[:, :, :, :kv_end]
        vb = vf[:, :, :kv_end]
        sc = jnp.einsum('bhmqd,bhmkd->bhmqk', qb, kb)
        mask = jnp.arange(kv_end)[None, :] <= (q0 + jnp.arange(Q_BLOCK))[:, None]
        sc = jnp.where(mask, sc, -jnp.inf)
        p = jax.nn.softmax(sc, axis=-1)
        w = p[:, :, 0] - lam * p[:, :, 1]
        outs.append(jnp.einsum('bhqk,bhkd->bhqd', w, vb))
    o = jnp.concatenate(outs, axis=2)
    o = rmsnorm(o, norm_g) * (1.0 - lambda_init)
    return jnp.transpose(o, (0, 2, 1, 3)).reshape(bsz, s_len, D_ATTN)


def s5_branch(u, lam_re, lam_im, b_re, b_im, c_re, c_im, d_skip, log_dt, w_glu, b_glu, norm_g):
    bsz, s_len, _ = u.shape
    uf = u.astype(jnp.float32)
    ug = uf.reshape(bsz, s_len, SSM_GROUPS, SSM_GROUP)
    dt = jnp.exp(log_dt.astype(jnp.float32))[:, None]
    lam = lax.complex(lam_re.astype(jnp.float32), lam_im.astype(jnp.float32))
    a_bar = jnp.exp(lam * dt)
    b_cplx = lax.complex(b_re.astype(jnp.float32), b_im.astype(jnp.float32))
    b_bar = ((a_bar - 1.0) / lam)[..., None] * b_cplx
    bu = jnp.einsum('bsgc,gnc->bsgn', ug.astype(jnp.complex64), b_bar)
    a_seq = jnp.broadcast_to(a_bar, (1, s_len) + a_bar.shape)

    def combine(left, right):
        a_l, b_l = left
        a_r, b_r = right
        return a_r * a_l, a_r * b_l + b_r

    _, states = lax.associative_scan(combine, (a_seq, bu), axis=1)
    y = (jnp.einsum('bsgn,gcn->bsgc', jnp.real(states), c_re.astype(jnp.float32))
         - jnp.einsum('bsgn,gcn->bsgc', jnp.imag(states), c_im.astype(jnp.float32)))
    y = y.reshape(bsz, s_len, D_SSM) + d_skip.astype(jnp.float32) * uf
    y = jax.nn.gelu(y, approximate=False)
    y = y * jax.nn.sigmoid(y @ w_glu.astype(jnp.float32) + b_glu.astype(jnp.float32))
    return rmsnorm(y, norm_g).astype(u.dtype)


def moe_ffn(h, w_router, b_router, w_gate_up, b_gate_up, w_down, b_down):
    bsz, s_len, d = h.shape
    n_tok = bsz * s_len
    xf = h.reshape(n_tok, d)
    logits = xf.astype(jnp.float32) @ w_router.astype(jnp.float32) + b_router.astype(jnp.float32)
    top_val, top_idx = lax.top_k(logits, TOP_K)
    gates = jax.nn.softmax(top_val, axis=-1)
    n_assign = n_tok * TOP_K
    e_flat = top_idx.reshape(-1)
    tok_flat = jnp.arange(n_assign, dtype=jnp.int32) // TOP_K
    order = jnp.argsort(e_flat)
    e_sorted = e_flat[order]
    tok_sorted = tok_flat[order]
    gate_sorted = gates.reshape(-1)[order]
    counts = jnp.bincount(e_flat, length=N_EXPERTS)
    starts = jnp.cumsum(counts) - counts
    padded = (counts + MOE_BLOCK - 1) // MOE_BLOCK * MOE_BLOCK
    pad_ends = jnp.cumsum(padded)
    pad_starts = pad_ends - padded
    dest = pad_starts[e_sorted] + (jnp.arange(n_assign, dtype=jnp.int32) - starts[e_sorted])
    n_rows = n_assign + N_EXPERTS * MOE_BLOCK
    n_blocks = n_rows // MOE_BLOCK
    x_pad = jnp.zeros((n_rows, d), h.dtype).at[dest].set(xf[tok_sorted])
    blk_expert = jnp.minimum(
        jnp.searchsorted(pad_ends, jnp.arange(n_blocks, dtype=jnp.int32) * MOE_BLOCK, side='right'),
        N_EXPERTS - 1)

    def expert_block(args):
        xb, e = args
        gu = xb @ w_gate_up[e] + b_gate_up[e]
        gate = jnp.minimum(gu[:, 0::2], SWIGLU_LIMIT)
        up = jnp.clip(gu[:, 1::2], -SWIGLU_LIMIT, SWIGLU_LIMIT)
        act = gate * jax.nn.sigmoid(SWIGLU_ALPHA * gate) * (up + 1.0)
        return act @ w_down[e] + b_down[e]

    y_pad = lax.map(expert_block, (x_pad.reshape(n_blocks, MOE_BLOCK, d), blk_expert))
    y_rows = y_pad.reshape(n_rows, d)[dest]
    out = jnp.zeros((n_tok, d), jnp.float32).at[tok_sorted].add(
        y_rows.astype(jnp.float32) * gate_sorted[:, None])
    return out.reshape(bsz, s_len, d).astype(h.dtype)


def setup_inputs(seed: int = 0) -> dict:
    key = jax.random.key(seed)
    ks = jax.random.split(key, 32)
    f32 = jnp.float32
    L, D, G, N, C = DEPTH, D_MODEL, SSM_GROUPS, SSM_STATE, SSM_GROUP
    nrm = lambda k, shp, s: jax.random.normal(k, shp, f32) * s
    x = jax.random.normal(ks[0], (BATCH, SEQ, D), f32)
    offsets = jax.random.randint(ks[1], (BATCH, 1), 0, 1024, dtype=jnp.int32)
    positions = jnp.arange(SEQ, dtype=jnp.int32)[None, :] + offsets
    n_in = 3 * D_ATTN + D_SSM
    lam_im = jnp.broadcast_to(jnp.pi * jnp.arange(N, dtype=f32), (L, G, N))
    return {
        "x": x,
        "positions": positions,
        "ln_mix_g": 1.0 + nrm(ks[2], (L, D), 0.02),
        "w_in": nrm(ks[3], (L, D, n_in), D ** -0.5),
        "lam_q1": nrm(ks[4], (L, ATTN_QKDIM), 0.1),
        "lam_k1": nrm(ks[5], (L, ATTN_QKDIM), 0.1),
        "lam_q2": nrm(ks[6], (L, ATTN_QKDIM), 0.1),
        "lam_k2": nrm(ks[7], (L, ATTN_QKDIM), 0.1),
        "diff_norm_g": 1.0 + nrm(ks[8], (L, ATTN_VDIM), 0.02),
        "ssm_lam_re": -0.5 + nrm(ks[9], (L, G, N), 0.01),
        "ssm_lam_im": lam_im + nrm(ks[10], (L, G, N), 0.01),
        "ssm_b_re": nrm(ks[11], (L, G, N, C), (0.5 / C) ** 0.5),
        "ssm_b_im": nrm(ks[12], (L, G, N, C), (0.5 / C) ** 0.5),
        "ssm_c_re": nrm(ks[13], (L, G, C, N), (0.5 / N) ** 0.5),
        "ssm_c_im": nrm(ks[14], (L, G, C, N), (0.5 / N) ** 0.5),
        "ssm_d": nrm(ks[15], (L, D_SSM), 1.0),
        "ssm_log_dt": jax.random.uniform(ks[16], (L, G), f32, math.log(DT_MIN), math.log(DT_MAX)),
        "ssm_w_glu": nrm(ks[17], (L, D_SSM, D_SSM), D_SSM ** -0.5),
        "ssm_b_glu": nrm(ks[18], (L, D_SSM), 0.01),
        "ssm_norm_g": 1.0 + nrm(ks[19], (L, D_SSM), 0.02),
        "w_out": nrm(ks[20], (L, D_MIX, D), D_MIX ** -0.5),
        "ln_ffn_g": 1.0 + nrm(ks[21], (L, D), 0.02),
        "w_router": nrm(ks[22], (L, D, N_EXPERTS), D ** -0.5),
        "b_router": nrm(ks[23], (L, N_EXPERTS), 0.01),
        "w_gate_up": nrm(ks[24], (L, N_EXPERTS, D, 2 * D_FF), D ** -0.5),
        "b_gate_up": nrm(ks[25], (L, N_EXPERTS, 2 * D_FF), 0.01),
        "w_down": nrm(ks[26], (L, N_EXPERTS, D_FF, D), D_FF ** -0.5),
        "b_down": nrm(ks[27], (L, N_EXPERTS, D), 0.01),
        "final_norm_g": 1.0 + nrm(ks[28], (D,), 0.02),
    }


def reference(x, positions, ln_mix_g, w_in, lam_q1, lam_k1, lam_q2, lam_k2, diff_norm_g,
              ssm_lam_re, ssm_lam_im, ssm_b_re, ssm_b_im, ssm_c_re, ssm_c_im, ssm_d,
              ssm_log_dt, ssm_w_glu, ssm_b_glu, ssm_norm_g, w_out, ln_ffn_g,
              w_router, b_router, w_gate_up, b_gate_up, w_down, b_down, final_norm_g):
    bsz, s_len, _ = x.shape
    h = x
    for li in range(DEPTH):
        lambda_init = 0.8 - 0.6 * math.exp(-0.3 * li)
        n = rmsnorm(h, ln_mix_g[li])
        proj = n @ w_in[li]
        q, k, v, u = jnp.split(proj, [D_ATTN, 2 * D_ATTN, 3 * D_ATTN], axis=-1)
        q = partial_rope(q.reshape(bsz, s_len, ATTN_HEADS, 2, ATTN_QKDIM), positions)
        k = partial_rope(k.reshape(bsz, s_len, ATTN_HEADS, 2, ATTN_QKDIM), positions)
        v = v.reshape(bsz, s_len, ATTN_HEADS, ATTN_VDIM)
        a_out = diff_attention(q, k, v, lam_q1[li], lam_k1[li], lam_q2[li], lam_k2[li],
                               diff_norm_g[li], lambda_init).astype(x.dtype)
        s_out = s5_branch(u, ssm_lam_re[li], ssm_lam_im[li], ssm_b_re[li], ssm_b_im[li],
                          ssm_c_re[li], ssm_c_im[li], ssm_d[li], ssm_log_dt[li],
                          ssm_w_glu[li], ssm_b_glu[li], ssm_norm_g[li])
        h = h + jnp.concatenate([a_out, s_out], axis=-1) @ w_out[li]
        h = h + moe_ffn(rmsnorm(h, ln_ffn_g[li]), w_router[li], b_router[li],
                        w_gate_up[li], b_gate_up[li], w_down[li], b_down[li])
    return rmsnorm(h, final_norm_g)
```

```python
import contextlib
import math
import numpy as np
import concourse.bass as bass
import concourse.mybir as mybir
from concourse.bass_utils import run_bass_kernel_spmd

F32 = mybir.dt.float32
BF16 = mybir.dt.bfloat16
I32 = mybir.dt.int32
AF = mybir.ActivationFunctionType
ALU = mybir.AluOpType
AX = mybir.AxisListType

D = 1024
NE = 32
TOPK = 4
EPS = 1e-6
NEG = -30000.0
TWO_PI = 2.0 * math.pi
PI_LO = 3.1415925
LAMBDA_INIT = 0.8 - 0.6 * math.exp(0.0)


class Ctx:
    def __init__(self, nc, es):
        self.nc = nc
        self.es = es
        self.eng = {"pe": nc.tensor, "act": nc.scalar, "dve": nc.vector,
                    "pool": nc.gpsimd, "sp": nc.sync}
        self.sems = {}
        self.cnt = {}
        for k in ("pe", "act", "dve", "pool"):
            self.sems["s_" + k] = es.enter_context(nc.semaphore("s_" + k))
            self.cnt["s_" + k] = 0
        self.seen = {k: {} for k in self.eng}
        self.w = {}
        self.r = {}
        self.group = None

    def _wait(self, e, tk, kind):
        if tk is None:
            return
        name, val = tk
        own = (name == "s_" + e)
        if own and e == "pe":
            return
        if self.seen[e].get(name, 0) >= val:
            return
        self.eng[e].wait_ge(self.sems[name], val)
        self.seen[e][name] = val

    def _deps(self, e, reads, writes):
        for b in reads:
            self._wait(e, self.w.get(b), "raw")
        for b in writes:
            self._wait(e, self.w.get(b), "waw")
            for name, val in self.r.get(b, {}).items():
                self._wait(e, (name, val), "war")

    def _commit(self, tk, reads, writes):
        for b in writes:
            self.w[b] = tk
            self.r[b] = {}
        for b in reads:
            d = self.r.setdefault(b, {})
            d[tk[0]] = max(d.get(tk[0], 0), tk[1])

    def op(self, e, fn, reads=(), writes=()):
        self._deps(e, reads, writes)
        inst = fn(self.eng[e])
        name = "s_" + e
        self.cnt[name] += 1
        inst.then_inc(self.sems[name], 1)
        self._commit((name, self.cnt[name]), reads, writes)

    def dma(self, e, key, out, in_, reads=(), writes=(), indirect=None):
        self._deps(e, reads, writes)
        if key not in self.sems:
            self.sems[key] = self.es.enter_context(self.nc.semaphore("d_" + key))
            self.cnt[key] = 0
        if self.cnt[key] > 0 and (self.group is None or not self.group[1]):
            self._wait(e, (key, self.cnt[key]), "raw")
        if indirect is None:
            inst = self.eng[e].dma_start(out=out, in_=in_)
        else:
            inst = self.eng[e].indirect_dma_start(out=out, in_=in_, **indirect)
        self.cnt[key] += 16
        inst.then_inc(self.sems[key], 16)
        if self.group is not None:
            assert self.group[0] == key
            self.group[1].append((reads, writes))
        else:
            self._commit((key, self.cnt[key]), reads, writes)

    @contextlib.contextmanager
    def dma_group(self, key):
        self.group = (key, [])
        yield
        pend = self.group[1]
        self.group = None
        for reads, writes in pend:
            self._commit((key, self.cnt[key]), reads, writes)

    def barrier(self):
        for e in self.eng:
            for name, val in self.cnt.items():
                if val > 0 and self.seen[e].get(name, 0) < val:
                    self.eng[e].wait_ge(self.sems[name], val)
                    self.seen[e][name] = val
        self.w = {}
        self.r = {}


def build(S, C, dbg=False, phases="ABCDEF"):
    NB = S // 128
    NOWN = NB // 2
    SO = S // 2
    GT = min(512, SO)
    NBG = GT // 128
    NG = S // GT
    NGO = SO // GT
    NBLK = (SO * TOPK + NE * 511) // 512
    NSLOT = NBLK * 512

    nc = bass.Bass("TRN2", target_bir_lowering=False)
    es = contextlib.ExitStack()

    def din(name, shape, dt=F32):
        return nc.dram_tensor(name, list(shape), dt, kind="ExternalInput").ap()

    def dscr(name, shape, dt):
        kind = "ExternalOutput" if dbg else "Internal"
        return nc.dram_tensor(name, list(shape), dt, kind=kind).ap()

    x_perm = din("x_perm", [S, D])
    pos_rep = din("pos_rep", [128, S], I32)
    par = din("par", [128, 4])
    triB_in = din("triB", [128, 128])
    maskoth_in = din("maskoth", [128, 128])
    w_in_ext = din("w_in_ext", [D, 2304])
    ropef = din("ropef", [128, 2])
    ident_in = din("ident", [128, 128])
    triA_in = din("triA", [128, 128])
    maskown_in = din("maskown", [128, 128])
    lnmix_col = din("lnmix_col", [128, 8])
    lamvec = din("lamvec", [128, 4, 64])
    diffg_rep = din("diffg_rep", [128, 128])
    lamre_row = din("lamre_row", [128, 2048])
    lamim_row = din("lamim_row", [128, 2048])
    logdt_row = din("logdt_row", [128, 2048])
    lamre_col = din("lamre_col", [128, 16])
    lamim_col = din("lamim_col", [128, 16])
    logdt_col = din("logdt_col", [128, 16])
    bre_blk = din("bre_blk", [128, 4, 512])
    bim_blk = din("bim_blk", [128, 4, 512])
    cre_blk = din("cre_blk", [128, 16, 128])
    cim_blk = din("cim_blk", [128, 16, 128])
    ssmcols = din("ssmcols", [128, 3, 4])
    wglu_in = din("w_glu", [512, 512])
    iota_in = din("iotas", [128, 129])
    wout_in = din("w_out", [D, D])
    lnffn_rep = din("lnffn_rep", [128, D])
    wrt_in = din("w_router", [D, NE])
    brt_rep = din("brt_rep", [128, NE])
    bstart_rep = din("bstart_rep", [128, NBLK])
    bgu_col = din("bgu_col", [128, NE, 16])
    if "E" in phases:
        wgu_in = din("w_gu", [NE, D, 2 * D])
        wdn_in = din("w_dn", [NE, D, D])
        bdn_rep = din("bdn_rep", [NE, 128, D])
    fin_rep = din("fin_rep", [128, D])

    out = nc.dram_tensor("out", [SO, D], F32, kind="ExternalOutput").ap()
    cnt_out = nc.dram_tensor("cnt", [128, NE], F32, kind="ExternalOutput").ap()

    kT_d = dscr("kT_d", [4, 128, S], BF16)
    qT_d = dscr("qT_d", [4, 128, SO], BF16)
    uT_d = dscr("uT_d", [4, 128, S], BF16)
    v_d = dscr("v_d", [S, 512], BF16)
    mixT_d = dscr("mixT_d", [8, 128, SO], BF16)
    a_d = dscr("a_d", [SO, 512], BF16)
    h_d = dscr("h_d", [SO, D], F32)
    xpad_d = dscr("xpad_d", [NSLOT, D], BF16)
    y_d = dscr("y_d", [NSLOT, D], F32)

    with es:
        cx = Ctx(nc, es)

        def sb(name, shape, dt=F32):
            return es.enter_context(nc.sbuf_tensor("sb_" + name, list(shape), dt))

        ident = sb("ident", [128, 128], BF16)
        cx.dma("pool", "c0", ident[:], ident_in, writes=["ident"])
        onesb = sb("onesb", [128, 128], BF16)
        cx.op("dve", lambda e: e.memset(onesb[:], 1.0), writes=["onesb"])
        parsb = sb("parsb", [128, 4])
        cx.dma("sp", "c1", parsb[:], par, writes=["par"])
        iotas = sb("iotas", [128, 129])
        cx.dma("sp", "c2", iotas[:], iota_in, writes=["iotas"])
        slots_all = sb("slots_all", [128, NOWN, 4], I32)
        gates_all = sb("gates_all", [128, NOWN, 4])
        widx = sb("widx", [128, NBLK, 8], I32)
        bidx = sb("bidx", [128, NBLK], I32)
        gidx = sb("gidx", [128, NBLK], I32)
        bcw_reg = nc.gpsimd.to_reg(NE * D - 1)
        bcb_reg = nc.gpsimd.to_reg(NE * 128 - 1)
        bc_reg = nc.gpsimd.to_reg(NSLOT - 1)

        if "A" in phases:
            pa = contextlib.ExitStack()
            with pa:
                def sa(name, shape, dt=F32):
                    return pa.enter_context(nc.sbuf_tensor("sb_" + name, list(shape), dt))

                def psa(name, shape, dt=F32):
                    return pa.enter_context(nc.psum_tensor("ps_" + name, list(shape), dt))

                win = sa("win", [128, 8, 2304], BF16)
                with cx.dma_group("win"):
                    for kt in range(8):
                        cx.dma("pool", "win", win[:, kt, :],
                               w_in_ext[kt * 128:(kt + 1) * 128, :], writes=["win%d" % kt])
                WIN = ["win%d" % kt for kt in range(8)]
                gcol = sa("gcol", [128, 8])
                cx.dma("sp", "c3", gcol[:], lnmix_col, writes=["gcol"])
                rf = sa("rf", [128, 2])
                cx.dma("sp", "c4", rf[:], ropef, writes=["rf"])

                xt = [sa("xt%d" % i, [128, NBG, D]) for i in range(2)]
                xn = [sa("xn%d" % i, [128, NBG, D], BF16) for i in range(2)]
                xnT = [sa("xnT%d" % i, [128, 8, GT], BF16) for i in range(2)]
                junk = sa("junkA", [128, D], BF16)
                ss = sa("ssA", [128, 2, NBG])
                rstd = sa("rstdA", [128, 2, NBG])
                posi = [sa("posi%d" % i, [128, GT], I32) for i in range(2)]
                ang = sa("ang", [128, GT])
                angk = sa("angk", [128, GT], I32)
                angf = sa("angf", [128, GT])
                angc = sa("angc", [128, GT])
                ctab = [sa("ctab%d" % i, [128, GT]) for i in range(2)]
                stab = [sa("stab%d" % i, [128, GT]) for i in range(2)]
                t1 = [sa("t1_%d" % i, [128, GT]) for i in range(2)]
                t2 = [sa("t2_%d" % i, [128, GT]) for i in range(2)]
                kst = [sa("kst%d" % i, [128, 4, GT], BF16) for i in range(2)]
                qst = [sa("qst%d" % i, [128, 4, GT], BF16) for i in range(2)]
                ust = [sa("ust%d" % i, [128, 4, GT], BF16) for i in range(2)]
                vst = [sa("vst%d" % i, [128, NBG, 512], BF16) for i in range(2)]
                tp = [psa("tpA%d" % i, [128, 1024], BF16) for i in range(2)]
                pm = [psa("pmA%d" % i, [128, 512]) for i in range(2)]
                pp = [psa("ppA%d" % i, [128, 512]) for i in range(2)]
                pv = [psa("pvA%d" % i, [128, 512]) for i in range(2)]
                ctr = {"tp": 0, "pm": 0, "pv": 0, "t": 0}

                def load_group(gi):
                    s = gi % 2
                    cx.dma("sp", "xt%d" % s, xt[s][:],
                           x_perm[gi * GT:(gi + 1) * GT, :].rearrange("(n p) d -> p n d", p=128),
                           writes=["xt%d" % s])
                    cx.dma("sp", "posi%d" % s, posi[s][:], pos_rep[:, gi * GT:(gi + 1) * GT],
                           writes=["posi%d" % s])

                def stage1(gi):
                    s = gi % 2
                    cx.op("dve", lambda e: e.tensor_copy(out=ang[:], in_=posi[s][:]),
                          reads=["posi%d" % s], writes=["ang"])
                    cx.op("dve", lambda e: e.tensor_scalar(out=ang[:], in0=ang[:], scalar1=rf[:, 0:1],
                                                            scalar2=None, op0=ALU.mult),
                          reads=["ang", "rf"], writes=["ang"])

                    def sin_of(dst, key, shift):
                        cx.op("dve", lambda e: e.tensor_scalar(out=angc[:], in0=ang[:], scalar1=float(shift),
                                                                scalar2=None, op0=ALU.add),
                              reads=["ang"], writes=["angc"])
                        cx.op("dve", lambda e: e.tensor_scalar(out=angk[:], in0=angc[:],
                                                                scalar1=float(1.0 / TWO_PI), scalar2=None,
                                                                op0=ALU.mult),
                              reads=["angc"], writes=["angk"])
                        cx.op("dve", lambda e: e.tensor_copy(out=angf[:], in_=angk[:]),
                              reads=["angk"], writes=["angf"])
                        cx.op("dve", lambda e: e.tensor_scalar(out=angf[:], in0=angf[:], scalar1=float(-TWO_PI),
                                                                scalar2=None, op0=ALU.mult),
                              reads=["angf"], writes=["angf"])
                        cx.op("dve", lambda e: e.tensor_tensor(out=angc[:], in0=angc[:], in1=angf[:], op=ALU.add),
                              reads=["angf", "angc"], writes=["angc"])
                        cx.op("dve", lambda e: e.tensor_scalar(out=angc[:], in0=angc[:], scalar1=PI_LO, scalar2=-PI_LO,
                                                                op0=ALU.min, op1=ALU.max),
                              reads=["angc"], writes=["angc"])
                        cx.op("act", lambda e: e.activation(out=dst[:], in_=angc[:], func=AF.Sin),
                              reads=["angc"], writes=[key])

                    sin_of(stab[s], "stab%d" % s, 0.0)
                    cx.op("dve", lambda e: e.tensor_scalar(out=stab[s][:], in0=stab[s][:], scalar1=rf[:, 1:2],
                                                            scalar2=None, op0=ALU.mult),
                          reads=["stab%d" % s, "rf"], writes=["stab%d" % s])
                    sin_of(ctab[s], "ctab%d" % s, math.pi / 2)

                    for n in range(NBG):
                        cx.op("act", lambda e: e.activation(out=junk[:], in_=xt[s][:, n, :], func=AF.Square,
                                                            accum_out=ss[:, s, n:n + 1]),
                              reads=["xt%d" % s], writes=["junkA", "ss%d_%d" % (s, n)])
                    SSK = ["ss%d_%d" % (s, n) for n in range(NBG)]
                    cx.op("dve", lambda e: e.tensor_scalar(out=rstd[:, s, :], in0=ss[:, s, :], scalar1=1.0 / D,
                                                           scalar2=EPS, op0=ALU.mult, op1=ALU.add),
                          reads=SSK, writes=["rstd%d" % s])
                    cx.op("act", lambda e: e.activation(out=rstd[:, s, :], in_=rstd[:, s, :], func=AF.Sqrt),
                          reads=["rstd%d" % s], writes=["rstd%d" % s])
                    cx.op("dve", lambda e: e.reciprocal(out=rstd[:, s, :], in_=rstd[:, s, :]),
                          reads=["rstd%d" % s], writes=["rstd%d" % s])
                    for n in range(NBG):
                        cx.op("dve", lambda e: e.tensor_scalar(out=xn[s][:, n, :], in0=xt[s][:, n, :],
                                                               scalar1=rstd[:, s, n:n + 1], scalar2=None,
                                                               op0=ALU.mult),
                              reads=["xt%d" % s, "rstd%d" % s], writes=["xn%d_%d" % (s, n)])

                def stage1c(gi):
                    s = gi % 2
                    XNK = ["xn%d_%d" % (s, n) for n in range(NBG)]
                    for kt in range(8):
                        p = ctr["tp"] % 2
                        ctr["tp"] += 1
                        for n in range(NBG):
                            cx.op("pe", lambda e: e.transpose(out=tp[p][:, n * 128:(n + 1) * 128],
                                                              in_=xn[s][:, n, kt * 128:(kt + 1) * 128],
                                                              identity=ident[:]),
                                  reads=XNK + ["ident"], writes=["tp%d" % p])
                        if kt % 2 == 0:
                            cx.op("act", lambda e: e.activation(out=xnT[s][:, kt, :], in_=tp[p][:, 0:GT],
                                                                func=AF.Copy, scale=gcol[:, kt:kt + 1]),
                                  reads=["tp%d" % p, "gcol"], writes=["xnT%d_%d" % (s, kt)])
                        else:
                            cx.op("dve", lambda e: e.tensor_scalar(out=xnT[s][:, kt, :], in0=tp[p][:, 0:GT],
                                                                   scalar1=gcol[:, kt:kt + 1], scalar2=None,
                                                                   op0=ALU.mult),
                                  reads=["tp%d" % p, "gcol"], writes=["xnT%d_%d" % (s, kt)])
                    XTK = ["xnT%d_%d" % (s, kt) for kt in range(8)]


                def stage2(gi, part):
                    s = gi % 2
                    own = gi < NGO
                    XTK = ["xnT%d_%d" % (s, kt) for kt in range(8)]
                    def proj(ps, pskey, col0):
                        for kt in range(8):
                            cx.op("pe", lambda e: e.matmul(ps[:, 0:GT], win[:, kt, col0:col0 + 128],
                                                           xnT[s][:, kt, :], start=(kt == 0), stop=(kt == 7)),
                                  reads=XTK + WIN, writes=[pskey])

                    def rope_tile(col_main, col_perm, dst, dkey):
                        i = ctr["pm"] % 2
                        ctr["pm"] += 1
                        proj(pm[i], "pm%d" % i, col_main)
                        proj(pp[i], "pp%d" % i, col_perm)
                        j = ctr["t"] % 2
                        ctr["t"] += 1
                        cx.op("dve", lambda e: e.tensor_tensor(out=t1[j][:], in0=pm[i][:, 0:GT], in1=ctab[s][:],
                                                               op=ALU.mult),
                              reads=["pm%d" % i, "ctab%d" % s], writes=["t1_%d" % j])
                        cx.op("dve", lambda e: e.tensor_tensor(out=t2[j][:], in0=pp[i][:, 0:GT], in1=stab[s][:],
                                                               op=ALU.mult),
                              reads=["pp%d" % i, "stab%d" % s], writes=["t2_%d" % j])
                        cx.op("dve", lambda e: e.tensor_tensor(out=dst, in0=t1[j][:], in1=t2[j][:], op=ALU.add),
                              reads=["t1_%d" % j, "t2_%d" % j], writes=[dkey])

                    def plain_tile(col0, dst, dkey):
                        i = ctr["pm"] % 2
                        ctr["pm"] += 1
                        proj(pm[i], "pm%d" % i, col0)
                        cx.op("act", lambda e: e.activation(out=dst, in_=pm[i][:, 0:GT], func=AF.Copy),
                              reads=["pm%d" % i], writes=[dkey])

                    def qk_tiles(col0, colperm, stage, skey, dst_d):
                        rope_tile(col0, colperm, stage[:, 0, :], skey)
                        for b_ in range(1, 4):
                            plain_tile(col0 + b_ * 128, stage[:, b_, :], skey)
                        with cx.dma_group(skey):
                            for hh in range(4):
                                for m in range(2):
                                    p0 = hh * 32 + m * 16
                                    cx.dma("sp", skey, dst_d[hh, m * 64:(m + 1) * 64, gi * GT:(gi + 1) * GT].rearrange(
                                        "(b r) t -> r b t", r=16), stage[p0:p0 + 16, :, :], reads=[skey])

                    if part == 0:
                        qk_tiles(512, 2176, kst[s], "kst%d" % s, kT_d)
                        if own:
                            qk_tiles(0, 2048, qst[s], "qst%d" % s, qT_d)
                        return
                    for ct in range(4):
                        i = ctr["pm"] % 2
                        ctr["pm"] += 1
                        proj(pm[i], "pm%d" % i, 1536 + ct * 128)
                        cx.op("act", lambda e: e.activation(out=ust[s][:, ct, :], in_=pm[i][:, 0:GT], func=AF.Copy),
                              reads=["pm%d" % i], writes=["ust%d" % s])
                    cx.dma("sp", "ust%d" % s, uT_d[:, :, gi * GT:(gi + 1) * GT].rearrange("c p t -> p c t"),
                           ust[s][:], reads=["ust%d" % s])
                    for n in range(NBG):
                        i = ctr["pv"] % 2
                        ctr["pv"] += 1
                        for kt in range(8):
                            cx.op("pe", lambda e: e.matmul(pv[i][:], xnT[s][:, kt, n * 128:(n + 1) * 128],
                                                           win[:, kt, 1024:1536], start=(kt == 0), stop=(kt == 7)),
                                  reads=XTK + WIN, writes=["pv%d" % i])
                        cx.op("act", lambda e: e.activation(out=vst[s][:, n, :], in_=pv[i][:], func=AF.Copy),
                              reads=["pv%d" % i], writes=["vst%d" % s])
                    cx.dma("sp", "vst%d" % s, v_d[gi * GT:(gi + 1) * GT, :].rearrange("(n p) c -> p n c", p=128),
                           vst[s][:], reads=["vst%d" % s])
                load_group(0)
                if NG > 1:
                    load_group(1)
                stage1(0)
                stage1c(0)
                if NG > 1:
                    stage1(1)
                if NG > 2:
                    load_group(2)
                for gi in range(NG):
                    stage2(gi, 0)
                    if gi + 1 < NG:
                        stage1c(gi + 1)
                    if gi + 2 < NG:
                        stage1(gi + 2)
                    if gi + 3 < NG:
                        load_group(gi + 3)
                    stage2(gi, 1)
                cx.barrier()

        if "B" in phases:
            pb = contextlib.ExitStack()
            with pb:
                def sbb(name, shape, dt=F32):
                    return pb.enter_context(nc.sbuf_tensor("sb_" + name, list(shape), dt))

                def psb(name, shape, dt=F32):
                    return pb.enter_context(nc.psum_tensor("ps_" + name, list(shape), dt))

                mown = sbb("mown", [128, 128], BF16)
                moth = sbb("moth", [128, 128], BF16)
                cx.dma("pool", "c5", mown[:], maskown_in, writes=["mown"])
                cx.dma("pool", "c6", moth[:], maskoth_in, writes=["moth"])
                lv = sbb("lv", [128, 4, 64])
                cx.dma("sp", "c1", lv[:], lamvec, writes=["lv"])
                dg = sbb("dg", [128, 128])
                cx.dma("sp", "c2", dg[:], diffg_rep, writes=["dg"])
                cx.op("dve", lambda e: e.tensor_scalar(out=dg[:], in0=dg[:], scalar1=float(1.0 - LAMBDA_INIT),
                                                       scalar2=None, op0=ALU.mult), reads=["dg"], writes=["dg"])
                lprod = sbb("lprod", [128, 2, 64])
                lsum = sbb("lsum", [128, 2])
                neglam = sbb("neglam", [128, 1])
                cx.op("dve", lambda e: e.tensor_tensor(out=lprod[:, 0, :], in0=lv[:, 0, :], in1=lv[:, 1, :], op=ALU.mult),
                      reads=["lv"], writes=["lprod"])
                cx.op("dve", lambda e: e.tensor_tensor(out=lprod[:, 1, :], in0=lv[:, 2, :], in1=lv[:, 3, :], op=ALU.mult),
                      reads=["lv", "lprod"], writes=["lprod"])
                cx.op("dve", lambda e: e.tensor_reduce(out=lsum[:], in_=lprod[:], axis=AX.X, op=ALU.add),
                      reads=["lprod"], writes=["lsum"])
                cx.op("act", lambda e: e.activation(out=lsum[:], in_=lsum[:], func=AF.Exp),
                      reads=["lsum"], writes=["lsum"])
                cx.op("dve", lambda e: e.scalar_tensor_tensor(out=neglam[:], in0=lsum[:, 1:2], scalar=float(-LAMBDA_INIT),
                                                              in1=lsum[:, 0:1], op0=ALU.add, op1=ALU.subtract),
                      reads=["lsum"], writes=["neglam"])

                kTs = [sbb("kTs%d" % i, [128, S], BF16) for i in range(2)]
                qq = [sbb("qq%d" % i, [128, NOWN, 2, 128], BF16) for i in range(2)]
                vs = [sbb("vs%d" % i, [128, NB, 129], BF16) for i in range(2)]
                ast = [sbb("ast%d" % i, [128, NOWN, 128], BF16) for i in range(2)]
                for i in range(2):
                    cx.op("dve", lambda e: e.memset(vs[i][:, :, 128:129], 1.0), writes=["vs%d" % i])
                    cx.op("dve", lambda e: e.memset(qq[i][:], 0.0), writes=["qq%d" % i])
                pt = [sbb("pt%d" % i, [128, 4, 2, 128], BF16) for i in range(2)]
                mk2 = {}
                for nm, src in (("mown", mown), ("moth", moth)):
                    t_ = sbb(nm + "2", [128, 2, 128], BF16)
                    for m in range(2):
                        cx.op("dve", lambda e: e.tensor_copy(out=t_[:, m, :], in_=src[:]), reads=[nm], writes=[nm + "2"])
                    mk2[nm] = t_
                rr = [sbb("rrB%d" % i, [128, 4]) for i in range(2)]
                a1 = sbb("a1B", [128, 128])
                a2 = sbb("a2B", [128, 128])
                asq = sbb("asqB", [128, 128])
                ssb = [sbb("ssB%d" % i, [128, 2]) for i in range(2)]
                scq = [psb("scB%d" % i, [128, 4, 2, 128]) for i in range(2)]
                o1 = [psb("o1B%d" % i, [128, 512]) for i in range(2)]
                o2 = [psb("o2B%d" % i, [128, 512]) for i in range(2)]

                def load_head(hh):
                    s = hh % 2
                    cx.dma("sp", "kTs%d" % s, kTs[s][:], kT_d[hh], writes=["kTs%d" % s])
                    with cx.dma_group("qq%d" % s):
                        for m in range(2):
                            cx.dma("sp", "qq%d" % s, qq[s][m * 64:(m + 1) * 64, :, m, :],
                                   qT_d[hh, m * 64:(m + 1) * 64, :].rearrange("p (n t) -> p n t", t=128),
                                   reads=["qq%d" % s], writes=["qq%d" % s])
                    with cx.dma_group("vs%d" % s):
                        for n0 in range(0, NB, 16):
                            n1 = min(NB, n0 + 16)
                            cx.dma("sp", "vs%d" % s, vs[s][:, n0:n1, 0:128],
                                   v_d[n0 * 128:n1 * 128, hh * 128:(hh + 1) * 128].rearrange("(n p) c -> p n c", p=128),
                                   reads=["vs%d" % s], writes=["vs%d" % s])

                groups = []
                nqb = 0
                for hh in range(4):
                    for j in range(NOWN):
                        tl = []
                        for i in range(j + 1):
                            tl.append((i, "mown" if i == j else None))
                            tl.append((NOWN + i, "moth" if i == j else None))
                        ngrp = (len(tl) + 3) // 4
                        for gi_ in range(ngrp):
                            groups.append(dict(hh=hh, j=j, tiles=tl[gi_ * 4:(gi_ + 1) * 4], first=(gi_ == 0),
                                               last=(gi_ == ngrp - 1), ob=nqb % 2, b=len(groups) % 2))
                        nqb += 1

                def emit_scores(G):
                    s = G["hh"] % 2
                    b = G["b"]
                    j = G["j"]
                    for t, (tile, mk) in enumerate(G["tiles"]):
                        ks = slice(tile * 128, (tile + 1) * 128)
                        cx.op("pe", lambda e: e.matmul(scq[b][:, t, :, :], kTs[s][:, ks], qq[s][:, j, :, :],
                                                       start=True, stop=(mk is None)),
                              reads=["kTs%d" % s, "qq%d" % s], writes=["sc%d" % b])
                        if mk is not None:
                            cx.op("pe", lambda e: e.matmul(scq[b][:, t, :, :], ident[:], mk2[mk][:], start=False, stop=True),
                                  reads=["ident", "mown2", "moth2"], writes=["sc%d" % b])
                    n = len(G["tiles"])
                    cx.op("act", lambda e: e.activation(out=pt[b][:, 0:n, :, :], in_=scq[b][:, 0:n, :, :], func=AF.Exp, scale=0.125),
                          reads=["sc%d" % b], writes=["pt%d" % b])

                def emit_pv(G):
                    s = G["hh"] % 2
                    b = G["b"]
                    ob = G["ob"]
                    n = len(G["tiles"])
                    for t, (tile, mk) in enumerate(G["tiles"]):
                        first = (G["first"] and t == 0)
                        last = (G["last"] and t == n - 1)
                        cx.op("pe", lambda e: e.matmul(o1[ob][:, 0:129], pt[b][:, t, 0, :], vs[s][:, tile, :], start=first, stop=last),
                              reads=["pt%d" % b, "vs%d" % s], writes=["o1_%d" % ob])
                        cx.op("pe", lambda e: e.matmul(o2[ob][:, 0:129], pt[b][:, t, 1, :], vs[s][:, tile, :], start=first, stop=last),
                              reads=["pt%d" % b, "vs%d" % s], writes=["o2_%d" % ob])

                def emit_epilogue(G):
                    s = G["hh"] % 2
                    ob = G["ob"]
                    j = G["j"]
                    O1 = o1[ob]; O2 = o2[ob]; R = rr[ob]; SS = ssb[ob]
                    RK = "rr%d" % ob; SK = "ssb%d" % ob
                    cx.op("dve", lambda e: e.reciprocal(out=R[:, 0:1], in_=O1[:, 128:129]), reads=["o1_%d" % ob], writes=[RK])
                    cx.op("dve", lambda e: e.reciprocal(out=R[:, 1:2], in_=O2[:, 128:129]), reads=["o2_%d" % ob, RK], writes=[RK])
                    cx.op("dve", lambda e: e.tensor_tensor(out=R[:, 2:3], in0=R[:, 1:2], in1=neglam[:], op=ALU.mult),
                          reads=[RK, "neglam"], writes=[RK])
                    cx.op("dve", lambda e: e.tensor_scalar(out=a1[:], in0=O1[:, 0:128], scalar1=R[:, 0:1], scalar2=None, op0=ALU.mult),
                          reads=["o1_%d" % ob, RK], writes=["a1"])
                    cx.op("dve", lambda e: e.scalar_tensor_tensor(out=a2[:], in0=O2[:, 0:128], scalar=R[:, 2:3], in1=a1[:],
                                                                  op0=ALU.mult, op1=ALU.add),
                          reads=["o2_%d" % ob, RK, "a1"], writes=["a2"])
                    cx.op("dve", lambda e: e.tensor_tensor(out=asq[:], in0=a2[:], in1=a2[:], op=ALU.mult), reads=["a2"], writes=["asq"])
                    cx.op("dve", lambda e: e.tensor_reduce(out=SS[:, 0:1], in_=asq[:], axis=AX.X, op=ALU.add), reads=["asq"], writes=[SK])
                    cx.op("dve", lambda e: e.tensor_scalar(out=SS[:, 0:1], in0=SS[:, 0:1], scalar1=1.0 / 128, scalar2=EPS,
                                                           op0=ALU.mult, op1=ALU.add), reads=[SK], writes=[SK])
                    cx.op("act", lambda e: e.activation(out=SS[:, 1:2], in_=SS[:, 0:1], func=AF.Ln), reads=[SK], writes=[SK + "b"])
                    cx.op("act", lambda e: e.activation(out=SS[:, 1:2], in_=SS[:, 1:2], func=AF.Exp, scale=-0.5),
                          reads=[SK + "b"], writes=[SK + "b"])
                    cx.op("dve", lambda e: e.scalar_tensor_tensor(out=ast[s][:, j, :], in0=a2[:], scalar=SS[:, 1:2], in1=dg[:],
                                                                  op0=ALU.mult, op1=ALU.mult),
                          reads=["a2", SK + "b", "dg"], writes=["ast%d" % s])
                    if j == NOWN - 1:
                        hh = G["hh"]
                        cx.dma("sp", "ast%d" % s, a_d[:, hh * 128:(hh + 1) * 128].rearrange("(n p) c -> p n c", p=128),
                               ast[s][:], reads=["ast%d" % s])

                load_head(0)
                zt = sbb("zt", [128, 8, D], BF16)
                cx.op("dve", lambda g: g.memset(zt[:], 0.0), writes=["zt"])
                nblk = NSLOT // 128
                with cx.dma_group("zero"):
                    for b0 in range(0, nblk, 8):
                        nb8 = min(8, nblk - b0)
                        cx.dma("sp", "zero", xpad_d[b0 * 128:(b0 + nb8) * 128, :].rearrange("(n p) d -> p n d", p=128),
                               zt[:, 0:nb8, :], reads=["zt"], writes=["xpad"])
                emit_scores(groups[0])
                for gi_, G in enumerate(groups):
                    if G["first"] and G["j"] == 0 and G["hh"] + 1 < 4:
                        load_head(G["hh"] + 1)
                    if gi_ + 1 < len(groups):
                        emit_scores(groups[gi_ + 1])
                    emit_pv(G)
                    if G["last"]:
                        emit_epilogue(G)
                cx.barrier()

        if "C" in phases:
            pc = contextlib.ExitStack()
            with pc:
                def sc_(name, shape, dt=F32):
                    return pc.enter_context(nc.sbuf_tensor("sb_" + name, list(shape), dt))

                def psc(name, shape, dt=F32):
                    return pc.enter_context(nc.psum_tensor("ps_" + name, list(shape), dt))

                def TT(e, o, a, b, op, R, W):
                    cx.op(e, lambda g: g.tensor_tensor(out=o, in0=a, in1=b, op=op), reads=R, writes=W)

                def TS(e, o, a, s1, op0, R, W, s2=None, op1=None):
                    if op1 is None:
                        cx.op(e, lambda g: g.tensor_scalar(out=o, in0=a, scalar1=s1, scalar2=None, op0=op0),
                              reads=R, writes=W)
                    else:
                        cx.op(e, lambda g: g.tensor_scalar(out=o, in0=a, scalar1=s1, scalar2=s2, op0=op0, op1=op1),
                              reads=R, writes=W)

                def ACT(o, a, func, R, W, **kw):
                    cx.op("act", lambda g: g.activation(out=o, in_=a, func=func, **kw), reads=R, writes=W)

                PAre = sc_("PAre", [128, 2048]); PAim = sc_("PAim", [128, 2048])
                PBre = sc_("PBre", [128, 2048]); PBim = sc_("PBim", [128, 2048])
                Qre = sc_("Qre", [128, 16, 128]); Qim = sc_("Qim", [128, 16, 128])
                A256 = sc_("A256", [128, 2, 16])
                Bblk = sc_("Bblk", [128, 4, 1024], BF16)
                Cre = sc_("Cre", [128, 16, 128], BF16)
                Cimn = sc_("Cimn", [128, 16, 128], BF16)
                wglu = sc_("wglu", [128, 4, 512], BF16)
                scol = sc_("scol", [128, 3, 4])
                triA = sc_("triA", [128, 128], BF16)
                triB = sc_("triB", [128, 128], BF16)
                cx.dma("pool", "c5", triA[:], triA_in, writes=["triA"])
                cx.dma("pool", "c6", triB[:], triB_in, writes=["triB"])
                triAn = sc_("triAn", [128, 128], BF16); triBn = sc_("triBn", [128, 128], BF16)
                onesn = sc_("onesn", [128, 1], BF16); Cren = sc_("Cren", [128, 16, 128], BF16)
                cx.op("dve", lambda g: g.tensor_scalar(out=triAn[:], in0=triA[:], scalar1=-1.0, scalar2=None, op0=ALU.mult),
                      reads=["triA"], writes=["triAn"])
                cx.op("dve", lambda g: g.tensor_scalar(out=triBn[:], in0=triB[:], scalar1=-1.0, scalar2=None, op0=ALU.mult),
                      reads=["triB"], writes=["triBn"])
                cx.op("dve", lambda g: g.memset(onesn[:], -1.0), writes=["onesn"])
                cx.dma("pool", "c7", Cre[:], cre_blk, writes=["Cre"])
                cx.dma("pool", "c8", Cimn[:], cim_blk, writes=["Cimn"])
                cx.op("dve", lambda g: g.tensor_scalar(out=Cimn[:], in0=Cimn[:], scalar1=-1.0, scalar2=None, op0=ALU.mult),
                      reads=["Cimn"], writes=["Cimn"])
                cx.op("dve", lambda g: g.tensor_scalar(out=Cren[:], in0=Cre[:], scalar1=-1.0, scalar2=None, op0=ALU.mult),
                      reads=["Cre"], writes=["Cren"])
                cx.dma("pool", "c9", wglu[:], wglu_in.rearrange("(k p) n -> p k n", p=128), writes=["wglu"])
                cx.dma("sp", "c3", scol[:], ssmcols, writes=["scol"])

                pcs = contextlib.ExitStack()
                with pcs:
                    def st_(name, shape, dt=F32):
                        return pcs.enter_context(nc.sbuf_tensor("sb_" + name, list(shape), dt))
                    ar = st_("ar", [128, 2048]); ai = st_("ai", [128, 2048]); dtr = st_("dtr", [128, 2048])
                    lre = st_("lre", [128, 2048]); lim = st_("lim", [128, 2048])
                    mag = st_("mag", [128, 2048]); ph = st_("ph", [128, 2048]); ph2 = st_("ph2", [128, 2048])
                    phk = st_("phk", [128, 2048], I32); phf = st_("phf", [128, 2048])
                    sn = st_("sn", [128, 2048]); cs = st_("cs", [128, 2048])
                    cx.dma("sp", "c1", lre[:], lamre_row, writes=["lre"])
                    cx.dma("sp", "c2", lim[:], lamim_row, writes=["lim"])
                    cx.dma("sp", "c4", dtr[:], logdt_row, writes=["dtr"])
                    ACT(dtr[:], dtr[:], AF.Exp, ["dtr"], ["dtr"])
                    TT("dve", ar[:], lre[:], dtr[:], ALU.mult, ["lre", "dtr"], ["ar"])
                    TT("dve", ai[:], lim[:], dtr[:], ALU.mult, ["lim", "dtr"], ["ai"])
                    posc = st_("posc", [128, 4])
                    TS("dve", posc[:, 0:1], iotas[:, 0:1], parsb[:, 0:1], ALU.add, ["iotas", "par"], ["posc"], -1.0, ALU.mult)
                    TS("dve", posc[:, 1:2], iotas[:, 0:1], parsb[:, 1:2], ALU.add, ["iotas", "par", "posc"], ["posc"], -1.0, ALU.mult)
                    cx.op("dve", lambda g: g.memset(posc[:, 2:3], 1.0), reads=["posc"], writes=["posc"])

                    def sincos(n):
                        for shift, dst, key in ((0.0, sn, "sn"), (math.pi / 2, cs, "cs")):
                            TS("dve", ph2[:, :n], ph[:, :n], float(shift), ALU.add, ["ph"], ["ph2"])
                            TS("dve", phk[:, :n], ph2[:, :n], float(1.0 / TWO_PI), ALU.mult, ["ph2"], ["phk"])
                            cx.op("dve", lambda g: g.tensor_copy(out=phf[:, :n], in_=phk[:, :n]), reads=["phk"], writes=["phf"])
                            cx.op("dve", lambda g: g.scalar_tensor_tensor(out=ph2[:, :n], in0=phf[:, :n], scalar=float(-TWO_PI),
                                                                          in1=ph2[:, :n], op0=ALU.mult, op1=ALU.add),
                                  reads=["phf", "ph2"], writes=["ph2"])
                            TS("dve", ph2[:, :n], ph2[:, :n], PI_LO, ALU.min, ["ph2"], ["ph2"], -PI_LO, ALU.max)
                            ACT(dst[:, :n], ph2[:, :n], AF.Sin, ["ph2"], [key])

                    def row_table(dre, dim, kre, kim, pcol):
                        ACT(mag[:], ar[:], AF.Exp, ["ar", "posc"], ["mag"], scale=posc[:, pcol:pcol + 1])
                        TS("dve", ph[:], ai[:], posc[:, pcol:pcol + 1], ALU.mult, ["ai", "posc"], ["ph"])
                        sincos(2048)
                        TT("dve", dre, mag[:], cs[:], ALU.mult, ["mag", "cs"], [kre])
                        TT("dve", dim, mag[:], sn[:], ALU.mult, ["mag", "sn"], [kim])

                    row_table(PAre[:], PAim[:], "PAre", "PAim", 0)
                    row_table(PBre[:], PBim[:], "PBre", "PBim", 1)
                    are = st_("are", [128, 2048]); aim = st_("aim", [128, 2048])
                    row_table(are[:], aim[:], "are", "aim", 2)
                    TS("dve", are[:], are[:], -1.0, ALU.add, ["are"], ["are"])
                    den = mag
                    TT("dve", den[:], lre[:], lre[:], ALU.mult, ["lre", "mag"], ["mag"])
                    TT("dve", ph[:], lim[:], lim[:], ALU.mult, ["lim", "ph"], ["ph"])
                    TT("dve", den[:], den[:], ph[:], ALU.add, ["mag", "ph"], ["mag"])
                    cx.op("dve", lambda g: g.reciprocal(out=den[:], in_=den[:]), reads=["mag"], writes=["mag"])
                    fre = sn; fim = cs
                    TT("dve", ph[:], are[:], lre[:], ALU.mult, ["are", "lre"], ["ph"])
                    TT("dve", ph2[:], aim[:], lim[:], ALU.mult, ["aim", "lim"], ["ph2"])
                    TT("dve", ph[:], ph[:], ph2[:], ALU.add, ["ph", "ph2"], ["ph"])
                    TT("dve", fre[:], ph[:], den[:], ALU.mult, ["ph", "mag", "sn"], ["sn"])
                    TT("dve", ph[:], aim[:], lre[:], ALU.mult, ["aim", "lre"], ["ph"])
                    TT("dve", ph2[:], are[:], lim[:], ALU.mult, ["are", "lim"], ["ph2"])
                    TT("dve", ph[:], ph[:], ph2[:], ALU.subtract, ["ph", "ph2"], ["ph"])
                    TT("dve", fim[:], ph[:], den[:], ALU.mult, ["ph", "mag", "cs"], ["cs"])
                    braw = st_("braw", [128, 2, 4, 512])
                    cx.dma("sp", "c1", braw[:, 0], bre_blk, writes=["braw0"])
                    cx.dma("sp", "c2", braw[:, 1], bim_blk, writes=["braw1"])
                    for ct in range(4):
                        fs = slice(ct * 512, (ct + 1) * 512)
                        TT("dve", ph[:, 0:512], braw[:, 0, ct, :], fre[:, fs], ALU.mult, ["braw0", "sn"], ["ph"])
                        TT("dve", ph2[:, 0:512], braw[:, 1, ct, :], fim[:, fs], ALU.mult, ["braw1", "cs"], ["ph2"])
                        TT("dve", Bblk[:, ct, 0:512], ph[:, 0:512], ph2[:, 0:512], ALU.subtract, ["ph", "ph2"], ["Bblk"])
                        TT("dve", ph[:, 0:512], braw[:, 0, ct, :], fim[:, fs], ALU.mult, ["braw0", "cs"], ["ph"])
                        TT("dve", ph2[:, 0:512], braw[:, 1, ct, :], fre[:, fs], ALU.mult, ["braw1", "sn"], ["ph2"])
                        TT("dve", Bblk[:, ct, 512:1024], ph[:, 0:512], ph2[:, 0:512], ALU.add, ["ph", "ph2"], ["Bblk"])
                    cc = st_("cc", [128, 3, 16])
                    cx.dma("sp", "c1", cc[:, 0, :], lamre_col, writes=["cc0"])
                    cx.dma("sp", "c2", cc[:, 1, :], lamim_col, writes=["cc1"])
                    cx.dma("sp", "c4", cc[:, 2, :], logdt_col, writes=["cc2"])
                    ACT(cc[:, 2, :], cc[:, 2, :], AF.Exp, ["cc2"], ["cc2"])
                    TT("dve", cc[:, 0, :], cc[:, 0, :], cc[:, 2, :], ALU.mult, ["cc0", "cc2"], ["cc0"])
                    TT("dve", cc[:, 1, :], cc[:, 1, :], cc[:, 2, :], ALU.mult, ["cc1", "cc2"], ["cc1"])
                    tpos = st_("tpos", [128, 128])
                    TS("dve", tpos[:], iotas[:, 1:129], parsb[:, 0:1], ALU.add, ["iotas", "par"], ["tpos"])
                    for p in range(16):
                        ACT(mag[:, p * 128:(p + 1) * 128], tpos[:], AF.Exp, ["tpos", "cc0"], ["mag"], scale=cc[:, 0, p:p + 1])
                        TS("dve", ph[:, p * 128:(p + 1) * 128], tpos[:], cc[:, 1, p:p + 1], ALU.mult, ["tpos", "cc1"], ["ph"])
                    sincos(2048)
                    TT("dve", Qre[:].rearrange("p a t -> p (a t)"), mag[:], cs[:], ALU.mult, ["mag", "cs"], ["Qre"])
                    TT("dve", Qim[:].rearrange("p a t -> p (a t)"), mag[:], sn[:], ALU.mult, ["mag", "sn"], ["Qim"])
                    ACT(mag[:, 0:16], cc[:, 0, :], AF.Exp, ["cc0", "mag"], ["mag"], scale=256.0)
                    TS("dve", ph[:, 0:16], cc[:, 1, :], 256.0, ALU.mult, ["cc1", "ph"], ["ph"])
                    sincos(16)
                    TT("dve", A256[:, 0, :], mag[:, 0:16], cs[:, 0:16], ALU.mult, ["mag", "cs"], ["A256"])
                    TT("dve", A256[:, 1, :], mag[:, 0:16], sn[:, 0:16], ALU.mult, ["mag", "sn", "A256"], ["A256"])
                    cx.barrier()

                u2 = [sc_("u2_%d" % i, [128, 4, 2, 128], BF16) for i in range(2)]
                sbq = [[sc_("sbq%d_%d" % (x, q), [128, 2048], BF16) for q in range(4)] for x in range(2)]
                bus = [[sc_("bus%d_%d" % (i, r), [128, 512]) for r in range(2)] for i in range(2)]
                carry = [sc_("carry%d" % i, [128, 2, 16]) for i in range(2)]
                cs_ = sc_("csum", [128, 2, 16]); cm = sc_("cmul", [128, 4, 16])
                spre = [sc_("spre%d" % i, [128, 4, 128]) for i in range(2)]
                spim = [sc_("spim%d" % i, [128, 4, 128]) for i in range(2)]
                mq = [[sc_("mq%d_%d" % (i, k), [128, 4, 128], BF16) for i in range(4)] for k in range(2)]
                ypre = sc_("ypre", [128, 4, 128]); yg = sc_("yg", [128, 4, 128]); ygb = sc_("ygb", [128, 4, 128], BF16)
                sg = sc_("sg", [128, 4, 128]); y2 = sc_("y2", [128, 4, 128]); sq = sc_("sq", [128, 4, 128], BF16)
                rs = sc_("rsC", [128, 128]); rs2 = sc_("rsC2", [128, 128])
                sst = [sc_("sst%d" % i, [128, 4, 128], BF16) for i in range(2)]
                bu1 = [psc("bu0_%d" % r, [128, 512]) for r in range(2)]
                bu = [bu1, bu1]
                ypsC = [psc("ypsC%d" % i, [128, 512]) for i in range(2)]
                Sps = [psc("Sps%d" % r, [128, 4, 128]) for r in range(2)]
                totp = psc("totp", [128, 16, 2])
                misc = psc("miscC", [128, 4, 128])
                cx.op("dve", lambda g: g.memset(carry[0][:], 0.0), writes=["carry0"])
                st = {"nbu": 0, "nrd": 0}
                SBKC = [["sbq%d_%d_%d" % (x, q, c_) for x in range(2) for q in range(4)] for c_ in range(4)]
                SBK = [k_ for c_ in range(4) for k_ in SBKC[c_]]

                def load_u(j):
                    sl = j % 2
                    cx.dma("sp", "u2a%d" % sl, u2[sl][:, :, 0, :],
                           uT_d[:, :, j * 128:(j + 1) * 128].rearrange("c p t -> p c t"), writes=["u2a%d" % sl])
                    cx.dma("sp", "u2b%d" % sl, u2[sl][:, :, 1, :],
                           uT_d[:, :, (NOWN + j) * 128:(NOWN + j + 1) * 128].rearrange("c p t -> p c t"),
                           writes=["u2b%d" % sl])

                def prescale_iter(j, X, ct):
                    sl = j % 2
                    UK = ["u2a%d" % sl, "u2b%d" % sl]
                    Pre = PAre if X == 0 else PBre
                    Pim = PAim if X == 0 else PBim
                    b = st["nbu"] % 2
                    st["nbu"] += 1
                    fs = slice(ct * 512, (ct + 1) * 512)
                    for r in range(2):
                        cx.op("pe", lambda g: g.matmul(bu[b][r][:], u2[sl][:, ct, X, :],
                                                       Bblk[:, ct, r * 512:(r + 1) * 512], start=True, stop=True),
                              reads=UK + ["Bblk"], writes=["bu0_%d" % r])
                    Q = sbq[X]
                    TT("dve", Q[0][:, fs], bu[b][0][:], Pre[:, fs], ALU.mult, ["bu0_0"], ["sbq%d_0_%d" % (X, ct)])
                    TT("dve", Q[1][:, fs], bu[b][1][:], Pim[:, fs], ALU.mult, ["bu0_1"], ["sbq%d_1_%d" % (X, ct)])
                    TT("dve", Q[2][:, fs], bu[b][0][:], Pim[:, fs], ALU.mult, ["bu0_0"], ["sbq%d_2_%d" % (X, ct)])
                    TT("dve", Q[3][:, fs], bu[b][1][:], Pre[:, fs], ALU.mult, ["bu0_1"], ["sbq%d_3_%d" % (X, ct)])

                def tail_steps(j):
                    sl = j % 2
                    so = sst[sl]
                    steps = []

                    def s0():
                        ACT(yg[:], ypre[:], AF.Gelu, ["ypre"], ["yg"])
                        cx.op("pool", lambda g: g.tensor_copy(out=ygb[:], in_=yg[:]), reads=["yg"], writes=["ygb"])

                    def s1():
                        for co in range(4):
                            for kt in range(4):
                                cx.op("pe", lambda g: g.matmul(misc[:, co, :], wglu[:, kt, co * 128:(co + 1) * 128],
                                                               ygb[:, kt, :], start=(kt == 0), stop=(kt == 3)),
                                      reads=["wglu", "ygb"], writes=["misc"])
                        for co in range(4):
                            ACT(sg[:, co, :], misc[:, co, :], AF.Sigmoid, ["misc", "scol"], ["sg"], bias=scol[:, 1, co:co + 1])

                    def s2():
                        TT("dve", y2[:], yg[:], sg[:], ALU.mult, ["yg", "sg"], ["y2"])
                        TT("pool", sq[:], y2[:], y2[:], ALU.mult, ["y2"], ["sq"])

                    def s3():
                        for kt in range(4):
                            cx.op("pe", lambda g: g.matmul(misc[:, 0, :], onesb[:], sq[:, kt, :], start=(kt == 0), stop=(kt == 3)),
                                  reads=["onesb", "sq"], writes=["misc"])

                    def s4():
                        TS("dve", rs[:], misc[:, 0, :], 1.0 / 512, ALU.mult, ["misc"], ["rs"], EPS, ALU.add)
                        ACT(rs2[:], rs[:], AF.Ln, ["rs"], ["rs2"])
                        ACT(rs2[:], rs2[:], AF.Exp, ["rs2"], ["rs2"], scale=-0.5)

                    def s5():
                        for ct in range(4):
                            cx.op("dve", lambda g: g.scalar_tensor_tensor(out=so[:, ct, :], in0=y2[:, ct, :],
                                                                          scalar=scol[:, 2, ct:ct + 1], in1=rs2[:],
                                                                          op0=ALU.mult, op1=ALU.mult),
                                  reads=["y2", "scol", "rs2"], writes=["sst%d" % sl])
                        cx.dma("sp", "sst%d" % sl, mixT_d[4:8, :, j * 128:(j + 1) * 128].rearrange("c p t -> p c t"),
                               so[:], reads=["sst%d" % sl])
                    return [s0, s1, s2, s3, s4, s5]

                def totals(j):
                    for p in range(16):
                        cols = slice(p * 128, (p + 1) * 128)
                        for r in range(2):
                            terms = [(x, 2 * r + q, (onesn if (r == 0 and q == 1) else onesb)) for x in range(2) for q in range(2)]
                            for ti, (x, q, ov) in enumerate(terms):
                                cx.op("pe", lambda g: g.matmul(totp[:, p, r:r + 1], sbq[x][q][:, cols], ov[:, 0:1],
                                                               start=(ti == 0), stop=(ti == 3)),
                                      reads=SBK + ["onesb", "onesn"], writes=["totp"])

                def round_S(j, rd):
                    cin = carry[j % 2]; ckin = "carry%d" % (j % 2)
                    k = rd % 2
                    for r in range(2):
                        for pp in range(4):
                            cols = slice((rd * 4 + pp) * 128, (rd * 4 + pp + 1) * 128)
                            terms = []
                            for x in range(2):
                                tp_, tn_ = (triA, triAn) if x == 0 else (triB, triBn)
                                terms.append((x, 2 * r, tp_))
                                terms.append((x, 2 * r + 1, tn_ if r == 0 else tp_))
                            for ti, (x, q, tm) in enumerate(terms):
                                cx.op("pe", lambda g: g.matmul(Sps[r][:, pp, :], sbq[x][q][:, cols], tm[:],
                                                               start=(ti == 0), stop=(ti == 3)),
                                      reads=SBKC[rd] + ["triA", "triB", "triAn", "triBn"], writes=["Sps%d" % r])
                    for pp in range(4):
                        p = rd * 4 + pp
                        ACT(spre[k][:, pp, :], Sps[0][:, pp, :], AF.Identity, ["Sps0", ckin], ["spre%d" % k], bias=cin[:, 0, p:p + 1])
                        ACT(spim[k][:, pp, :], Sps[1][:, pp, :], AF.Identity, ["Sps1", ckin], ["spim%d" % k], bias=cin[:, 1, p:p + 1])

                def round_X(j, rd):
                    sl = j % 2
                    k = rd % 2
                    M = mq[k]
                    MK = ["mq%d_%d" % (i, k) for i in range(4)]
                    qs_ = slice(rd * 4, rd * 4 + 4)
                    TT("dve", M[0][:], spre[k][:], Qre[:, qs_, :], ALU.mult, ["spre%d" % k], [MK[0]])
                    TT("dve", M[1][:], spim[k][:], Qim[:, qs_, :], ALU.mult, ["spim%d" % k], [MK[1]])
                    TT("dve", M[2][:], spre[k][:], Qim[:, qs_, :], ALU.mult, ["spre%d" % k], [MK[2]])
                    TT("dve", M[3][:], spim[k][:], Qre[:, qs_, :], ALU.mult, ["spim%d" % k], [MK[3]])
                    cw = [Cre, Cren, Cimn, Cimn]
                    for pp in range(4):
                        p = rd * 4 + pp
                        for q in range(4):
                            cx.op("pe", lambda g: g.matmul(ypsC[rd % 2][:, 0:128], cw[q][:, p, :], M[q][:, pp, :],
                                                           start=(pp == 0 and q == 0), stop=(pp == 3 and q == 3)),
                                  reads=["Cre", "Cren", "Cimn", MK[q]], writes=["ypsC%d" % (rd % 2)])

                def round_Y(j, rd):
                    sl = j % 2
                    cx.op("dve", lambda g: g.scalar_tensor_tensor(out=ypre[:, rd, :], in0=u2[sl][:, rd, 0, :],
                                                                  scalar=scol[:, 0, rd:rd + 1], in1=ypsC[rd % 2][:, 0:128],
                                                                  op0=ALU.mult, op1=ALU.add),
                          reads=["ypsC%d" % (rd % 2), "scol", "u2a%d" % sl], writes=["ypre"])

                def carry_update(j):
                    cin = carry[j % 2]; cout = carry[(j + 1) % 2]
                    ckin = "carry%d" % (j % 2); ckout = "carry%d" % ((j + 1) % 2)
                    TT("dve", cs_[:, 0, :], cin[:, 0, :], totp[:, :, 0], ALU.add, [ckin, "totp"], ["csum"])
                    TT("dve", cs_[:, 1, :], cin[:, 1, :], totp[:, :, 1], ALU.add, [ckin, "totp", "csum"], ["csum"])
                    TT("dve", cm[:, 0, :], A256[:, 0, :], cs_[:, 0, :], ALU.mult, ["csum"], ["cm"])
                    TT("dve", cm[:, 1, :], A256[:, 1, :], cs_[:, 1, :], ALU.mult, ["csum", "cm"], ["cm"])
                    TT("dve", cm[:, 2, :], A256[:, 0, :], cs_[:, 1, :], ALU.mult, ["csum", "cm"], ["cm"])
                    TT("dve", cm[:, 3, :], A256[:, 1, :], cs_[:, 0, :], ALU.mult, ["csum", "cm"], ["cm"])
                    TT("dve", cout[:, 0, :], cm[:, 0, :], cm[:, 1, :], ALU.subtract, ["cm"], [ckout])
                    TT("dve", cout[:, 1, :], cm[:, 2, :], cm[:, 3, :], ALU.add, ["cm", ckout], [ckout])

                load_u(0)
                pending = []
                for j in range(NOWN):
                    if j + 1 < NOWN:
                        load_u(j + 1)
                    it = 0
                    for ct in range(4):
                        for X in range(2):
                            prescale_iter(j, X, ct)
                            if pending and it < len(pending):
                                pending[it]()
                            it += 1
                        round_S(j, ct)
                        if ct >= 1:
                            round_X(j, ct - 1)
                        if ct >= 2:
                            round_Y(j, ct - 2)
                    pending = []
                    round_X(j, 3)
                    round_Y(j, 2)
                    totals(j)
                    round_Y(j, 3)
                    carry_update(j)
                    pending = tail_steps(j)
                for stp in pending:
                    stp()
                cx.barrier()

        if "D" in phases:
            pd = contextlib.ExitStack()
            with pd:
                def sd(name, shape, dt=F32):
                    return pd.enter_context(nc.sbuf_tensor("sb_" + name, list(shape), dt))

                def psd(name, shape, dt=F32):
                    return pd.enter_context(nc.psum_tensor("ps_" + name, list(shape), dt))

                wout = sd("wout", [128, 8, D], BF16)
                cx.dma("pool", "c5", wout[:], wout_in.rearrange("(k p) n -> p k n", p=128), writes=["wout"])
                wrt = sd("wrt", [128, 8, NE], BF16)
                cx.dma("pool", "c6", wrt[:], wrt_in.rearrange("(k p) n -> p k n", p=128), writes=["wrt"])
                lnf = sd("lnf", [128, D])
                cx.dma("sp", "c1", lnf[:], lnffn_rep, writes=["lnf"])
                brt = sd("brt", [128, NE])
                cx.dma("sp", "c2", brt[:], brt_rep, writes=["brt"])
                stri = sd("stri", [128, 128], BF16)
                triAd = sd("triAd", [128, 128], BF16)
                cx.dma("pool", "c7", triAd[:], triA_in, writes=["triAd"])
                cx.op("dve", lambda g: g.tensor_tensor(out=stri[:], in0=triAd[:], in1=ident[:], op=ALU.subtract),
                      reads=["triAd", "ident"], writes=["stri"])
                offs = sd("offs", [128, NE])
                cx.op("dve", lambda g: g.memset(offs[:], 0.0), writes=["offs"])
                bst = sd("bst", [128, NBLK])
                cx.dma("sp", "c3", bst[:], bstart_rep, writes=["bst"])

                mixb = [sd("mixb%d" % i, [128, 8, 128], BF16) for i in range(2)]
                xb = [sd("xb%d" % i, [128, D]) for i in range(2)]
                hb = [sd("hb%d" % i, [128, D]) for i in range(2)]
                hn_all = sd("hn_all", [128, NOWN, D], BF16)
                lg_all = sd("lg_all", [128, NOWN, NE])
                pos_all = sd("pos_all", [128, NOWN, NE])
                mx_all = sd("mx_all", [128, NOWN, 8])
                hnT = [sd("hnT%d" % i, [128, 8, 128], BF16) for i in range(2)]
                junkd = sd("junkD", [128, D], BF16)
                sd1 = [sd("sd1_%d" % i, [128, 4]) for i in range(2)]
                negm = [sd("negm%d" % i, [128, 1]) for i in range(2)]
                ex = [sd("ex%d" % i, [128, 4]) for i in range(2)]
                esum = [sd("esum%d" % i, [128, 2]) for i in range(2)]
                Mf = [sd("Mf%d" % i, [128, NE]) for i in range(2)]
                Mb = [sd("Mb%d" % i, [128, NE], BF16) for i in range(2)]
                sbase = sd("sbase", [128, NE])
                oh = sd("oh", [128, NE]); slotf = sd("slotf", [128, 4])
                hps = [psd("hps%d" % i, [128, 512]) for i in range(2)]
                tpd = [psd("tpD%d" % i, [128, 1024], BF16) for i in range(2)]
                lps = [psd("lpsD%d" % i, [128, 512]) for i in range(2)]
                cps = [psd("cpsD%d" % i, [128, 512]) for i in range(2)]
                ablk = [sd("ablk%d" % i, [128, 512], BF16) for i in range(2)]

                def load_d(j):
                    sl = j % 2
                    cx.dma("sp", "mixb%d" % sl, mixb[sl][:, 4:8, :], mixT_d[4:8, :, j * 128:(j + 1) * 128].rearrange("c p t -> p c t"),
                           writes=["mixs%d" % sl])
                    cx.dma("sp", "ablk%d" % sl, ablk[sl][:], a_d[j * 128:(j + 1) * 128, :], writes=["ablk%d" % sl])
                    cx.dma("sp", "xb%d" % sl, xb[sl][:], x_perm[j * 128:(j + 1) * 128, :], writes=["xb%d" % sl])

                def stage1(j):
                    sl = j % 2
                    T = tpd[sl]; TK = "tpd%d" % sl
                    for c4 in range(4):
                        cx.op("pe", lambda g: g.transpose(out=T[:, c4 * 128:(c4 + 1) * 128], in_=ablk[sl][:, c4 * 128:(c4 + 1) * 128],
                                                          identity=ident[:]), reads=["ablk%d" % sl, "ident"], writes=[TK])
                    cx.op("act", lambda g: g.activation(out=mixb[sl][:, 0:4, :].rearrange("p k t -> p (k t)"), in_=T[:, 0:512],
                                                        func=AF.Copy), reads=[TK], writes=["mixa%d" % sl])
                    for half in range(2):
                        for kt in range(8):
                            cx.op("pe", lambda g: g.matmul(hps[half][:], mixb[sl][:, kt, :],
                                                           wout[:, kt, half * 512:(half + 1) * 512],
                                                           start=(kt == 0), stop=(kt == 7)),
                                  reads=["mixa%d" % sl, "mixs%d" % sl, "wout"], writes=["hps%d" % half])
                        cx.op("dve", lambda g: g.tensor_tensor(out=hb[sl][:, half * 512:(half + 1) * 512], in0=hps[half][:],
                                                               in1=xb[sl][:, half * 512:(half + 1) * 512], op=ALU.add),
                              reads=["hps%d" % half, "xb%d" % sl], writes=["hb%d_%d" % (sl, half)])
                    HB = ["hb%d_0" % sl, "hb%d_1" % sl]
                    cx.dma("sp", "hst%d" % sl, h_d[j * 128:(j + 1) * 128, :], hb[sl][:], reads=HB)
                    S1 = sd1[sl]; SK = "sd1_%d" % sl
                    cx.op("act", lambda g: g.activation(out=junkd[:], in_=hb[sl][:], func=AF.Square, accum_out=S1[:, 0:1]),
                          reads=HB, writes=["junkD", SK])
                    cx.op("dve", lambda g: g.tensor_scalar(out=S1[:, 1:2], in0=S1[:, 0:1], scalar1=1.0 / D, scalar2=EPS,
                                                           op0=ALU.mult, op1=ALU.add), reads=[SK], writes=[SK + "b"])
                    cx.op("act", lambda g: g.activation(out=S1[:, 2:3], in_=S1[:, 1:2], func=AF.Ln), reads=[SK + "b"], writes=[SK + "c"])
                    cx.op("act", lambda g: g.activation(out=S1[:, 2:3], in_=S1[:, 2:3], func=AF.Exp, scale=-0.5),
                          reads=[SK + "c"], writes=[SK + "c"])
                    cx.op("dve", lambda g: g.scalar_tensor_tensor(out=hn_all[:, j, :], in0=hb[sl][:], scalar=S1[:, 2:3], in1=lnf[:],
                                                                  op0=ALU.mult, op1=ALU.mult),
                          reads=HB + [SK + "c", "lnf"], writes=["hn_%d" % j])

                def stage2(j):
                    sl = j % 2
                    HN = "hn_%d" % j
                    T = tpd[sl]; TK = "tpd%d" % sl
                    for kt in range(8):
                        cx.op("pe", lambda g: g.transpose(out=T[:, kt * 128:(kt + 1) * 128], in_=hn_all[:, j, kt * 128:(kt + 1) * 128],
                                                          identity=ident[:]), reads=[HN, "ident"], writes=[TK])
                    cx.op("act", lambda g: g.activation(out=hnT[sl][:].rearrange("p k t -> p (k t)"), in_=T[:], func=AF.Copy),
                          reads=[TK], writes=["hnT%d" % sl])
                    L = lps[sl]; LK = "lps%d" % sl
                    for kt in range(8):
                        cx.op("pe", lambda g: g.matmul(L[:, 0:NE], hnT[sl][:, kt, :], wrt[:, kt, :], start=(kt == 0), stop=(kt == 7)),
                              reads=["hnT%d" % sl, "wrt"], writes=[LK])
                    LG = "lg_%d" % j
                    cx.op("dve", lambda g: g.tensor_tensor(out=lg_all[:, j, :], in0=L[:, 0:NE], in1=brt[:], op=ALU.add),
                          reads=[LK, "brt"], writes=[LG])
                    MX = "mx_%d" % j
                    cx.op("dve", lambda g: g.max(out=mx_all[:, j, :], in_=lg_all[:, j, :]), reads=[LG], writes=[MX])
                    NK = "negm%d" % sl
                    cx.op("dve", lambda g: g.tensor_scalar(out=negm[sl][:], in0=mx_all[:, j, 0:1], scalar1=-1.0, scalar2=None, op0=ALU.mult),
                          reads=[MX], writes=[NK])
                    cx.op("act", lambda g: g.activation(out=ex[sl][:], in_=mx_all[:, j, 0:4], func=AF.Exp, bias=negm[sl][:],
                                                        accum_out=esum[sl][:, 0:1]),
                          reads=[MX, NK], writes=["ex%d" % sl, "esum%d" % sl])
                    cx.op("dve", lambda g: g.reciprocal(out=esum[sl][:, 1:2], in_=esum[sl][:, 0:1]), reads=["esum%d" % sl],
                          writes=["esumb%d" % sl])
                    cx.op("dve", lambda g: g.tensor_scalar(out=gates_all[:, j, :], in0=ex[sl][:], scalar1=esum[sl][:, 1:2], scalar2=None,
                                                           op0=ALU.mult), reads=["ex%d" % sl, "esumb%d" % sl], writes=["gates%d" % j])
                    cx.op("dve", lambda g: g.tensor_scalar(out=Mf[sl][:], in0=lg_all[:, j, :], scalar1=mx_all[:, j, 3:4], scalar2=None,
                                                           op0=ALU.is_ge), reads=[LG, MX], writes=["Mf%d" % sl])
                    cx.op("dve", lambda g: g.tensor_copy(out=Mb[sl][:], in_=Mf[sl][:]), reads=["Mf%d" % sl], writes=["Mb%d" % sl])
                    Cp = cps[sl]; CK = "cps%d" % sl
                    cx.op("pe", lambda g: g.matmul(Cp[:, 0:NE], stri[:], Mb[sl][:], start=True, stop=True),
                          reads=["stri", "Mb%d" % sl], writes=[CK])
                    cx.op("pe", lambda g: g.matmul(Cp[:, 64:64 + NE], onesb[:], Mb[sl][:], start=True, stop=True),
                          reads=["onesb", "Mb%d" % sl], writes=[CK])
                    cx.op("dve", lambda g: g.tensor_tensor(out=pos_all[:, j, :], in0=Cp[:, 0:NE], in1=offs[:], op=ALU.add),
                          reads=[CK, "offs"], writes=["pos_%d" % j])
                    cx.op("dve", lambda g: g.tensor_tensor(out=offs[:], in0=Cp[:, 64:64 + NE], in1=offs[:], op=ALU.add),
                          reads=[CK, "offs"], writes=["offs"])

                load_d(0)
                if NOWN > 1:
                    load_d(1)
                stage1(0)
                for j in range(NOWN):
                    if j + 1 < NOWN:
                        stage1(j + 1)
                    if j + 2 < NOWN:
                        load_d(j + 2)
                    stage2(j)
                cx.dma("sp", "c1", cnt_out, offs[:], reads=["offs"])
                padded = sd("padded", [128, NE]); pend = sd("pend", [128, NE]); pstart = sd("pstart", [128, NE])
                nbi = sd("nbi", [128, NE], I32)
                cx.op("dve", lambda g: g.tensor_scalar(out=padded[:], in0=offs[:], scalar1=511.0, scalar2=1.0 / 512,
                                                       op0=ALU.add, op1=ALU.mult), reads=["offs"], writes=["padded"])
                cx.op("dve", lambda g: g.tensor_scalar(out=nbi[:], in0=padded[:], scalar1=-511.0 / 1024, scalar2=None, op0=ALU.add),
                      reads=["padded"], writes=["nbi"])
                cx.op("dve", lambda g: g.tensor_copy(out=padded[:], in_=nbi[:]), reads=["nbi"], writes=["padded"])
                cx.op("dve", lambda g: g.tensor_scalar(out=padded[:], in0=padded[:], scalar1=512.0, scalar2=None, op0=ALU.mult),
                      reads=["padded"], writes=["padded"])
                cx.op("dve", lambda g: g.tensor_copy(out=pend[:, 0:1], in_=padded[:, 0:1]), reads=["padded"], writes=["pend"])
                for e_ in range(1, NE):
                    cx.op("dve", lambda g: g.tensor_tensor(out=pend[:, e_:e_ + 1], in0=pend[:, e_ - 1:e_], in1=padded[:, e_:e_ + 1],
                                                           op=ALU.add), reads=["padded", "pend"], writes=["pend"])
                cx.op("dve", lambda g: g.tensor_tensor(out=pstart[:], in0=pend[:], in1=padded[:], op=ALU.subtract),
                      reads=["pend", "padded"], writes=["pstart"])
                bacc = sd("bacc", [128, NBLK])
                cx.op("dve", lambda g: g.memset(bacc[:], 0.0), writes=["bacc"])
                for e_ in range(NE):
                    cx.op("dve", lambda g: g.scalar_tensor_tensor(out=bacc[:], in0=bst[:], scalar=pend[:, e_:e_ + 1], in1=bacc[:],
                                                                  op0=ALU.is_ge, op1=ALU.add),
                          reads=["bst", "pend", "bacc"], writes=["bacc"])
                cx.op("dve", lambda g: g.tensor_scalar(out=bacc[:], in0=bacc[:], scalar1=float(NE - 1), scalar2=None, op0=ALU.min),
                      reads=["bacc"], writes=["bacc"])
                widf = sd("widf", [128, NBLK, 8]); bidf = sd("bidf", [128, 2, NBLK]); kp = sd("kp", [128, 8])
                for kt in range(8):
                    cx.op("dve", lambda g: g.tensor_scalar(out=kp[:, kt:kt + 1], in0=iotas[:, 0:1], scalar1=float(kt * 128), scalar2=None,
                                                           op0=ALU.add), reads=["iotas", "kp"], writes=["kp"])
                for kt in range(8):
                    cx.op("dve", lambda g: g.tensor_scalar(out=widf[:, :, kt], in0=bacc[:], scalar1=float(D), scalar2=kp[:, kt:kt + 1],
                                                           op0=ALU.mult, op1=ALU.add), reads=["bacc", "kp", "widf"], writes=["widf"])
                cx.op("dve", lambda g: g.tensor_copy(out=widx[:], in_=widf[:]), reads=["widf"], writes=["widx"])
                cx.op("dve", lambda g: g.tensor_scalar(out=bidf[:, 0, :], in0=bacc[:], scalar1=128.0, scalar2=iotas[:, 0:1],
                                                       op0=ALU.mult, op1=ALU.add), reads=["bacc", "iotas"], writes=["bidf0"])
                cx.op("dve", lambda g: g.tensor_copy(out=bidx[:], in_=bidf[:, 0, :]), reads=["bidf0"], writes=["bidx"])
                cx.op("dve", lambda g: g.tensor_scalar(out=kp[:, 0:1], in0=iotas[:, 0:1], scalar1=float(NE), scalar2=None, op0=ALU.mult),
                      reads=["iotas", "kp", "widf"], writes=["kp"])
                cx.op("dve", lambda g: g.tensor_scalar(out=bidf[:, 1, :], in0=bacc[:], scalar1=kp[:, 0:1], scalar2=None, op0=ALU.add),
                      reads=["bacc", "kp"], writes=["bidf1"])
                cx.op("dve", lambda g: g.tensor_copy(out=gidx[:], in_=bidf[:, 1, :]), reads=["bidf1"], writes=["gidx"])
                for j in range(NOWN):
                    sl = j % 2
                    cx.op("dve", lambda g: g.tensor_tensor(out=sbase[:], in0=pos_all[:, j, :], in1=pstart[:], op=ALU.add),
                          reads=["pos_%d" % j, "pstart"], writes=["sbase"])
                    for k in range(4):
                        cx.op("dve", lambda g: g.tensor_scalar(out=oh[:], in0=lg_all[:, j, :], scalar1=mx_all[:, j, k:k + 1], scalar2=None,
                                                               op0=ALU.is_equal), reads=["lg_%d" % j, "mx_%d" % j], writes=["oh"])
                        cx.op("dve", lambda g: g.tensor_tensor(out=oh[:], in0=oh[:], in1=sbase[:], op=ALU.mult),
                              reads=["oh", "sbase"], writes=["oh"])
                        cx.op("dve", lambda g: g.tensor_reduce(out=slotf[:, k:k + 1], in_=oh[:], axis=AX.X, op=ALU.add),
                              reads=["oh"], writes=["slotf"])
                    cx.op("dve", lambda g: g.tensor_copy(out=slots_all[:, j, :], in_=slotf[:]), reads=["slotf"], writes=["slots%d" % j])
                    with cx.dma_group("scat%d" % sl):
                        for k in range(4):
                            cx.dma("pool", "scat%d" % sl, xpad_d, hn_all[:, j, :], reads=["hn_%d" % j, "slots%d" % j, "xpad"],
                                   indirect=dict(out_offset=bass.IndirectOffsetOnAxis(ap=slots_all[:, j, k:k + 1], axis=0),
                                                 in_offset=None, bounds_check=bc_reg, oob_is_err=False))
                cx.barrier()

        if "E" in phases:
            pe_ = contextlib.ExitStack()
            with pe_:
                def se(name, shape, dt=F32):
                    return pe_.enter_context(nc.sbuf_tensor("sb_" + name, list(shape), dt))

                def pse(name, shape, dt=F32):
                    return pe_.enter_context(nc.psum_tensor("ps_" + name, list(shape), dt))

                NSL = 512
                wgu = [se("wgu%d" % i, [128, 8, 2 * D], BF16) for i in range(2)]
                wdn = [se("wdn%d" % i, [128, 8, D], BF16) for i in range(2)]
                bdn = [se("bdn%d" % i, [128, D]) for i in range(2)]
                bgu = [se("bgu%d" % i, [128, 16]) for i in range(2)]
                xs = [se("xs%d" % i, [128, 4, D], BF16) for i in range(2)]
                xT = se("xT", [128, 8, NSL], BF16)
                gt = [se("gt%d" % i, [128, NSL]) for i in range(2)]
                sgm = [se("sgm%d" % i, [128, NSL]) for i in range(2)]
                ut = [se("ut%d" % i, [128, NSL]) for i in range(2)]
                p1 = [se("p1_%d" % i, [128, NSL]) for i in range(2)]
                actT = se("actT", [128, 8, NSL], BF16)
                ysb = [se("ysb%d" % i, [128, D]) for i in range(2)]
                tpe = [pse("tpE%d" % i, [128, 1024], BF16) for i in range(2)]
                gps = [pse("gps%d" % i, [128, 512]) for i in range(2)]
                ups = [pse("ups%d" % i, [128, 512]) for i in range(2)]
                yps = [pse("yps%d" % i, [128, 512]) for i in range(2)]

                wgu_rows = wgu_in.rearrange("e d n -> (e d) n")
                wdn_rows = wdn_in.rearrange("e d n -> (e d) n")
                bdn_rows = bdn_rep.rearrange("e p n -> (e p) n")
                bgu_rows = bgu_col.rearrange("p e n -> (p e) n")

                def load_w(bi):
                    sl = bi % 2
                    with cx.dma_group("wgu%d" % sl):
                        for kt in range(8):
                            cx.dma("pool", "wgu%d" % sl, wgu[sl][:, kt, :], wgu_rows, reads=["widx"], writes=["wgu%d" % sl],
                                   indirect=dict(out_offset=None, in_offset=bass.IndirectOffsetOnAxis(ap=widx[:, bi, kt:kt + 1], axis=0),
                                                 bounds_check=bcw_reg, oob_is_err=False))
                    with cx.dma_group("wdn%d" % sl):
                        for kt in range(8):
                            cx.dma("pool", "wdn%d" % sl, wdn[sl][:, kt, :], wdn_rows, reads=["widx"], writes=["wdn%d" % sl],
                                   indirect=dict(out_offset=None, in_offset=bass.IndirectOffsetOnAxis(ap=widx[:, bi, kt:kt + 1], axis=0),
                                                 bounds_check=bcw_reg, oob_is_err=False))
                    cx.dma("pool", "bdn%d" % sl, bdn[sl][:, :], bdn_rows, reads=["bidx"], writes=["bdn%d" % sl],
                           indirect=dict(out_offset=None, in_offset=bass.IndirectOffsetOnAxis(ap=bidx[:, bi:bi + 1], axis=0),
                                         bounds_check=bcb_reg, oob_is_err=False))
                    cx.dma("pool", "bgu%d" % sl, bgu[sl][:, :], bgu_rows, reads=["gidx"], writes=["bgu%d" % sl],
                           indirect=dict(out_offset=None, in_offset=bass.IndirectOffsetOnAxis(ap=gidx[:, bi:bi + 1], axis=0),
                                         bounds_check=bcb_reg, oob_is_err=False))

                ny = 0
                ntp = 0
                nft = 0

                def load_x(bi):
                    sl = bi % 2
                    r0 = bi * NSL
                    cx.dma("sp", "xs%d" % sl, xs[sl][:], xpad_d[r0:r0 + NSL, :].rearrange("(n p) d -> p n d", p=128),
                           writes=["xs%d" % sl])

                def emit_trans(bi):
                    nonlocal ntp
                    xsl = bi % 2
                    for kt in range(8):
                        tb = ntp % 2
                        ntp += 1
                        for n in range(4):
                            cx.op("pe", lambda g: g.transpose(out=tpe[tb][:, n * 128:(n + 1) * 128],
                                                              in_=xs[xsl][:, n, kt * 128:(kt + 1) * 128], identity=ident[:]),
                                  reads=["xs%d" % xsl, "ident"], writes=["tpe%d" % tb])
                        if kt % 2 == 0:
                            cx.op("act", lambda g: g.activation(out=xT[:, kt, :], in_=tpe[tb][:, 0:NSL], func=AF.Copy),
                                  reads=["tpe%d" % tb], writes=["xT%d" % kt])
                        else:
                            cx.op("dve", lambda g: g.tensor_copy(out=xT[:, kt, :], in_=tpe[tb][:, 0:NSL]),
                                  reads=["tpe%d" % tb], writes=["xT%d" % kt])

                load_w(0)
                load_x(0)
                for bi in range(NBLK):
                    wsl = bi % 2
                    xsl = bi % 2
                    if bi + 1 < NBLK:
                        load_w(bi + 1)
                        load_x(bi + 1)
                    r0 = bi * NSL
                    nsl = NSL
                    if bi == 0:
                        emit_trans(0)
                    XT = ["xT%d" % kt for kt in range(8)]
                    for ft in range(8):
                        b = nft % 2
                        nft += 1
                        for kt in range(8):
                            cx.op("pe", lambda g: g.matmul(gps[b][:], wgu[wsl][:, kt, ft * 128:(ft + 1) * 128],
                                                           xT[:, kt, :], start=(kt == 0), stop=(kt == 7)),
                                  reads=XT + ["wgu%d" % wsl], writes=["gps%d" % b])
                        for kt in range(8):
                            cx.op("pe", lambda g: g.matmul(ups[b][:], wgu[wsl][:, kt, D + ft * 128:D + (ft + 1) * 128],
                                                           xT[:, kt, :], start=(kt == 0), stop=(kt == 7)),
                                  reads=XT + ["wgu%d" % wsl], writes=["ups%d" % b])
                        cx.op("dve", lambda g: g.tensor_scalar(out=gt[b][:], in0=gps[b][:], scalar1=bgu[wsl][:, ft:ft + 1],
                                                               scalar2=7.0, op0=ALU.add, op1=ALU.min),
                              reads=["gps%d" % b, "bgu%d" % wsl], writes=["gt%d" % b])
                        cx.op("act", lambda g: g.activation(out=sgm[b][:], in_=gt[b][:], func=AF.Sigmoid, scale=1.702),
                              reads=["gt%d" % b], writes=["sgm%d" % b])
                        cx.op("dve", lambda g: g.tensor_scalar(out=ut[b][:], in0=ups[b][:], scalar1=bgu[wsl][:, 8 + ft:9 + ft],
                                                               scalar2=7.0, op0=ALU.add, op1=ALU.min),
                              reads=["ups%d" % b, "bgu%d" % wsl], writes=["ut%d" % b])
                        cx.op("dve", lambda g: g.tensor_scalar(out=ut[b][:], in0=ut[b][:], scalar1=-7.0, scalar2=1.0,
                                                               op0=ALU.max, op1=ALU.add),
                              reads=["ut%d" % b], writes=["ut%d" % b])
                        cx.op("dve", lambda g: g.tensor_tensor(out=p1[b][:], in0=gt[b][:], in1=sgm[b][:], op=ALU.mult),
                              reads=["gt%d" % b, "sgm%d" % b], writes=["p1_%d" % b])
                        cx.op("dve", lambda g: g.tensor_tensor(out=actT[:, ft, :], in0=p1[b][:], in1=ut[b][:], op=ALU.mult),
                              reads=["p1_%d" % b, "ut%d" % b], writes=["actT%d" % ft])
                    AT = ["actT%d" % ft for ft in range(8)]
                    if bi + 1 < NBLK:
                        emit_trans(bi + 1)
                    for n in range(4):
                        yb = ny % 2
                        ny += 1
                        for half in range(2):
                            for ft in range(8):
                                cx.op("pe", lambda g: g.matmul(yps[half][:], actT[:, ft, n * 128:(n + 1) * 128],
                                                               wdn[wsl][:, ft, half * 512:(half + 1) * 512],
                                                               start=(ft == 0), stop=(ft == 7)),
                                      reads=AT + ["wdn%d" % wsl], writes=["yps%d" % half])
                            cx.op("dve", lambda g: g.tensor_tensor(out=ysb[yb][:, half * 512:(half + 1) * 512], in0=yps[half][:],
                                                                   in1=bdn[wsl][:, half * 512:(half + 1) * 512], op=ALU.add),
                                  reads=["yps%d" % half, "bdn%d" % wsl], writes=["ysb%d_%d" % (yb, half)])
                        cx.dma("sp", "yst%d" % yb, y_d[r0 + n * 128:r0 + (n + 1) * 128, :], ysb[yb][:],
                               reads=["ysb%d_0" % yb, "ysb%d_1" % yb])
                cx.barrier()

        if "F" in phases:
            pf = contextlib.ExitStack()
            with pf:
                def sf(name, shape, dt=F32):
                    return pf.enter_context(nc.sbuf_tensor("sb_" + name, list(shape), dt))

                fin = sf("fin", [128, D])
                cx.dma("sp", "c1", fin[:], fin_rep, writes=["fin"])
                hf = [sf("hf%d" % i, [128, D]) for i in range(2)]
                gk = [[sf("gk%d_%d" % (i, k), [128, D]) for k in range(4)] for i in range(2)]
                acc = sf("accF", [128, D])
                junkf = sf("junkF", [128, D], BF16)
                sf1 = sf("sf1", [128, 4])
                ot = [sf("ot%d" % i, [128, D]) for i in range(2)]

                def load_f(j):
                    sl = j % 2
                    cx.dma("sp", "hf%d" % sl, hf[sl][:], h_d[j * 128:(j + 1) * 128, :], writes=["hf%d" % sl])
                    for k in range(4):
                        cx.op("pool", lambda g: g.memset(gk[sl][k][:], 0.0), writes=["gk%d_%d" % (sl, k)])
                    with cx.dma_group("gath%d" % sl):
                        for k in range(4):
                            cx.dma("pool", "gath%d" % sl, gk[sl][k][:, :], y_d, reads=["slots%d" % j, "gk%d_%d" % (sl, k)],
                                   writes=["gk%d_%d" % (sl, k)],
                                   indirect=dict(out_offset=None,
                                                 in_offset=bass.IndirectOffsetOnAxis(ap=slots_all[:, j, k:k + 1], axis=0),
                                                 bounds_check=bc_reg, oob_is_err=False))

                load_f(0)
                for j in range(NOWN):
                    sl = j % 2
                    if j + 1 < NOWN:
                        load_f(j + 1)
                    prev = hf[sl]
                    pk = "hf%d" % sl
                    for k in range(4):
                        cx.op("dve", lambda g: g.scalar_tensor_tensor(out=acc[:], in0=gk[sl][k][:], scalar=gates_all[:, j, k:k + 1],
                                                                      in1=prev[:], op0=ALU.mult, op1=ALU.add),
                              reads=["gk%d_%d" % (sl, k), "gates%d" % j, pk], writes=["acc"])
                        prev = acc
                        pk = "acc"
                    cx.op("act", lambda g: g.activation(out=junkf[:], in_=acc[:], func=AF.Square, accum_out=sf1[:, 0:1]),
                          reads=["acc"], writes=["junkF", "sf1"])
                    cx.op("dve", lambda g: g.tensor_scalar(out=sf1[:, 1:2], in0=sf1[:, 0:1], scalar1=1.0 / D, scalar2=EPS,
                                                           op0=ALU.mult, op1=ALU.add), reads=["sf1"], writes=["sf1b"])
                    cx.op("act", lambda g: g.activation(out=sf1[:, 2:3], in_=sf1[:, 1:2], func=AF.Ln), reads=["sf1b"], writes=["sf1c"])
                    cx.op("act", lambda g: g.activation(out=sf1[:, 2:3], in_=sf1[:, 2:3], func=AF.Exp, scale=-0.5),
                          reads=["sf1c"], writes=["sf1c"])
                    cx.op("dve", lambda g: g.scalar_tensor_tensor(out=ot[sl][:], in0=acc[:], scalar=sf1[:, 2:3], in1=fin[:],
                                                                  op0=ALU.mult, op1=ALU.mult),
                          reads=["acc", "sf1c", "fin"], writes=["ot%d" % sl])
                    cx.dma("sp", "ost%d" % sl, out[j * 128:(j + 1) * 128, :], ot[sl][:], reads=["ot%d" % sl])
                cx.barrier()

        cx.barrier()
    return nc


def _rep(v, n=128):
    v = np.asarray(v, np.float32).reshape(1, -1)
    return np.ascontiguousarray(np.broadcast_to(v, (n, v.shape[1])))


def prep(inputs, S, C):
    f32 = np.float32
    NB = S // 128
    x = np.asarray(inputs["x"], f32)
    positions = np.asarray(inputs["positions"], np.int32)
    w_in = np.asarray(inputs["w_in"], f32)[0]
    pidx = np.arange(128)
    hh_, mm_, rr_ = pidx // 32, (pidx // 16) % 2, pidx % 16
    def qk_cols(base):
        cols = [base + hh_ * 128 + mm_ * 64 + 16 * b_ + rr_ for b_ in range(4)]
        return np.concatenate(cols)
    partner = np.where(rr_ < 8, rr_ + 8, rr_ - 8)
    def perm_cols(base):
        return base + hh_ * 128 + mm_ * 64 + partner
    w_in_ext = np.ascontiguousarray(np.concatenate(
        [w_in[:, qk_cols(0)], w_in[:, qk_cols(512)], w_in[:, 1024:2048], w_in[:, perm_cols(0)], w_in[:, perm_cols(512)]], axis=1))
    inv_freq = (500000.0 ** (-np.arange(0, 16, 2, dtype=np.float32) / 16)).astype(f32)
    ropef = np.zeros((128, 2), f32)
    ropef[:, 0] = inv_freq[rr_ % 8]
    ropef[:, 1] = np.where(rr_ < 8, -1.0, 1.0)
    ident = np.eye(128, dtype=f32)
    kk = np.arange(128)
    triA = (kk[:, None] <= kk[None, :]).astype(f32)
    maskown = np.where(kk[:, None] <= kk[None, :], 0.0, NEG).astype(f32)
    iotas = np.zeros((128, 129), f32)
    iotas[:, 0] = kk
    iotas[:, 1:] = kk[None, :]
    shared = {
        "w_in_ext": w_in_ext, "ropef": ropef, "ident": ident, "triA": triA, "maskown": maskown,
        "lnmix_col": np.ascontiguousarray(np.asarray(inputs["ln_mix_g"], f32)[0].reshape(8, 128).T),
        "lamvec": np.ascontiguousarray(np.broadcast_to(
            np.stack([np.asarray(inputs[k], f32)[0] for k in ("lam_q1", "lam_k1", "lam_q2", "lam_k2")])[None],
            (128, 4, 64))),
        "diffg_rep": _rep(inputs["diff_norm_g"][0]),
        "iotas": iotas,
    }
    lre = np.asarray(inputs["ssm_lam_re"], f32)[0]
    lim = np.asarray(inputs["ssm_lam_im"], f32)[0]
    ldt = np.asarray(inputs["ssm_log_dt"], f32)[0]
    shared["lamre_row"] = _rep(lre.reshape(-1))
    shared["lamim_row"] = _rep(lim.reshape(-1))
    shared["logdt_row"] = _rep(np.repeat(ldt, 64))

    def col(a):
        return np.ascontiguousarray(a.reshape(16, 2, 64).transpose(1, 2, 0).reshape(128, 16))
    shared["lamre_col"] = col(lre)
    shared["lamim_col"] = col(lim)
    shared["logdt_col"] = col(np.repeat(ldt[:, None], 64, axis=1))
    bre = np.asarray(inputs["ssm_b_re"], f32)[0]
    bim = np.asarray(inputs["ssm_b_im"], f32)[0]
    cre = np.asarray(inputs["ssm_c_re"], f32)[0]
    cim = np.asarray(inputs["ssm_c_im"], f32)[0]

    def bblk(b):
        o = np.zeros((128, 4, 512), f32)
        for g in range(32):
            ct, gl = divmod(g, 8)
            o[gl * 16:(gl + 1) * 16, ct, gl * 64:(gl + 1) * 64] = b[g].T
        return o

    def cblk(c):
        o = np.zeros((128, 16, 128), f32)
        for g in range(32):
            pair, g2 = divmod(g, 2)
            gl = g % 8
            o[g2 * 64:(g2 + 1) * 64, pair, gl * 16:(gl + 1) * 16] = c[g].T
        return o
    shared["bre_blk"] = bblk(bre)
    shared["bim_blk"] = bblk(bim)
    shared["cre_blk"] = cblk(cre)
    shared["cim_blk"] = cblk(cim)
    ssmcols = np.zeros((128, 3, 4), f32)
    for i, k in enumerate(("ssm_d", "ssm_b_glu", "ssm_norm_g")):
        ssmcols[:, i, :] = np.asarray(inputs[k], f32)[0].reshape(4, 128).T
    shared["ssmcols"] = ssmcols
    shared["w_glu"] = np.ascontiguousarray(np.asarray(inputs["ssm_w_glu"], f32)[0])
    shared["w_out"] = np.ascontiguousarray(np.asarray(inputs["w_out"], f32)[0])
    shared["lnffn_rep"] = _rep(inputs["ln_ffn_g"][0])
    shared["w_router"] = np.ascontiguousarray(np.asarray(inputs["w_router"], f32)[0])
    shared["brt_rep"] = _rep(inputs["b_router"][0])
    nblk = (S // 2 * TOPK + NE * 511) // 512
    shared["bstart_rep"] = _rep(np.arange(nblk, dtype=f32) * 512.0)
    wgu = np.asarray(inputs["w_gate_up"], f32)[0]
    shared["w_gu"] = np.ascontiguousarray(np.concatenate([wgu[:, :, 0::2], wgu[:, :, 1::2]], axis=2))
    bgu = np.asarray(inputs["b_gate_up"], f32)[0]
    bgu2 = np.concatenate([bgu[:, 0::2], bgu[:, 1::2]], axis=1)
    shared["bgu_col"] = np.ascontiguousarray(bgu2.reshape(NE, 16, 128).transpose(2, 0, 1))
    shared["w_dn"] = np.ascontiguousarray(np.asarray(inputs["w_down"], f32)[0])
    bdn = np.asarray(inputs["b_down"], f32)[0]
    shared["bdn_rep"] = np.ascontiguousarray(np.broadcast_to(bdn[:, None, :], (NE, 128, D)))
    shared["fin_rep"] = _rep(inputs["final_norm_g"])

    in_maps = []
    for c in range(8):
        b, h = divmod(c, 2)
        own = np.arange(h, NB, 2)
        oth = np.arange(1 - h, NB, 2)
        order = np.concatenate([own, oth])
        tok = (order[:, None] * 128 + np.arange(128)[None, :]).reshape(-1)
        m = dict(shared)
        m["x_perm"] = np.ascontiguousarray(x[b][tok])
        m["pos_rep"] = np.ascontiguousarray(np.broadcast_to(positions[b][tok][None, :], (128, S)))
        parc = np.zeros((128, 4), f32)
        parc[:, 0] = 128.0 * h
        parc[:, 1] = 128.0 * (1 - h)
        m["par"] = parc
        m["triB"] = np.full((128, 128), float(h), f32)
        m["maskoth"] = np.full((128, 128), 0.0 if h == 1 else NEG, f32)
        in_maps.append(m)
    return in_maps


_CACHE = {}


def kernel(**inputs):
    S = int(np.asarray(inputs["x"]).shape[1])
    C = 0
    key = (S, C)
    if key not in _CACHE:
        _CACHE[key] = build(S, C)
    nc = _CACHE[key]
    in_maps = prep(inputs, S, C)
    res = run_bass_kernel_spmd(nc, in_maps, core_ids=list(range(8)))
    _CACHE["last_cnt"] = [np.asarray(r["cnt"])[0] for r in res.results]
    B = int(np.asarray(inputs["x"]).shape[0])
    NB = S // 128
    outp = np.zeros((B, S, D), np.float32)
    for c in range(8):
        b, h = divmod(c, 2)
        own = np.arange(h, NB, 2)
        tok = (own[:, None] * 128 + np.arange(128)[None, :]).reshape(-1)
        outp[b][tok] = res.results[c]["out"]
    return outp
```

```python
import contextlib
import math
import numpy as np
import concourse.bass as bass
import concourse.mybir as mybir
from concourse.bass_utils import run_bass_kernel_spmd

F32 = mybir.dt.float32
BF16 = mybir.dt.bfloat16
I32 = mybir.dt.int32
AF = mybir.ActivationFunctionType
ALU = mybir.AluOpType
AX = mybir.AxisListType

D = 1024
NE = 32
TOPK = 4
EPS = 1e-6
NEG = -30000.0
TWO_PI = 2.0 * math.pi
PI_LO = 3.1415925
LAMBDA_INIT = 0.8 - 0.6 * math.exp(0.0)


class Ctx:
    def __init__(self, nc, es):
        self.nc = nc
        self.es = es
        self.eng = {"pe": nc.tensor, "act": nc.scalar, "dve": nc.vector,
                    "pool": nc.gpsimd, "sp": nc.sync}
        self.sems = {}
        self.cnt = {}
        for k in ("pe", "act", "dve", "pool"):
            self.sems["s_" + k] = es.enter_context(nc.semaphore("s_" + k))
            self.cnt["s_" + k] = 0
        self.seen = {k: {} for k in self.eng}
        self.w = {}
        self.r = {}
        self.group = None

    def _wait(self, e, tk, kind):
        if tk is None:
            return
        name, val = tk
        own = (name == "s_" + e)
        if own and e == "pe":
            return
        if self.seen[e].get(name, 0) >= val:
            return
        self.eng[e].wait_ge(self.sems[name], val)
        self.seen[e][name] = val

    def _deps(self, e, reads, writes):
        for b in reads:
            self._wait(e, self.w.get(b), "raw")
        for b in writes:
            self._wait(e, self.w.get(b), "waw")
            for name, val in self.r.get(b, {}).items():
                self._wait(e, (name, val), "war")

    def _commit(self, tk, reads, writes):
        for b in writes:
            self.w[b] = tk
            self.r[b] = {}
        for b in reads:
            d = self.r.setdefault(b, {})
            d[tk[0]] = max(d.get(tk[0], 0), tk[1])

    def op(self, e, fn, reads=(), writes=()):
        self._deps(e, reads, writes)
        inst = fn(self.eng[e])
        name = "s_" + e
        self.cnt[name] += 1
        inst.then_inc(self.sems[name], 1)
        self._commit((name, self.cnt[name]), reads, writes)

    def dma(self, e, key, out, in_, reads=(), writes=(), indirect=None):
        self._deps(e, reads, writes)
        if key not in self.sems:
            self.sems[key] = self.es.enter_context(self.nc.semaphore("d_" + key))
            self.cnt[key] = 0
        if self.cnt[key] > 0 and (self.group is None or not self.group[1]):
            self._wait(e, (key, self.cnt[key]), "raw")
        if indirect is None:
            inst = self.eng[e].dma_start(out=out, in_=in_)
        else:
            inst = self.eng[e].indirect_dma_start(out=out, in_=in_, **indirect)
        self.cnt[key] += 16
        inst.then_inc(self.sems[key], 16)
        if self.group is not None:
            assert self.group[0] == key
            self.group[1].append((reads, writes))
        else:
            self._commit((key, self.cnt[key]), reads, writes)

    @contextlib.contextmanager
    def dma_group(self, key):
        self.group = (key, [])
        yield
        pend = self.group[1]
        self.group = None
        for reads, writes in pend:
            self._commit((key, self.cnt[key]), reads, writes)

    def barrier(self):
        for e in self.eng:
            for name, val in self.cnt.items():
                if val > 0 and self.seen[e].get(name, 0) < val:
                    self.eng[e].wait_ge(self.sems[name], val)
                    self.seen[e][name] = val
        self.w = {}
        self.r = {}


def build(S, C, dbg=False, phases="ABCDEF"):
    NB = S // 128
    NOWN = NB // 2
    SO = S // 2
    GT = min(512, SO)
    NBG = GT // 128
    NG = S // GT
    NGO = SO // GT
    NBLK = (SO * TOPK + NE * 511) // 512
    NSLOT = NBLK * 512

    nc = bass.Bass("TRN2", target_bir_lowering=False)
    es = contextlib.ExitStack()

    def din(name, shape, dt=F32):
        return nc.dram_tensor(name, list(shape), dt, kind="ExternalInput").ap()

    def dscr(name, shape, dt):
        kind = "ExternalOutput" if dbg else "Internal"
        return nc.dram_tensor(name, list(shape), dt, kind=kind).ap()

    x_perm = din("x_perm", [S, D])
    pos_rep = din("pos_rep", [128, S], I32)
    par = din("par", [128, 4])
    triB_in = din("triB", [128, 128])
    maskoth_in = din("maskoth", [128, 128])
    w_in_ext = din("w_in_ext", [D, 2304])
    ropef = din("ropef", [128, 2])
    ident_in = din("ident", [128, 128])
    triA_in = din("triA", [128, 128])
    maskown_in = din("maskown", [128, 128])
    lnmix_col = din("lnmix_col", [128, 8])
    lamvec = din("lamvec", [128, 4, 64])
    diffg_rep = din("diffg_rep", [128, 128])
    lamre_row = din("lamre_row", [128, 2048])
    lamim_row = din("lamim_row", [128, 2048])
    logdt_row = din("logdt_row", [128, 2048])
    lamre_col = din("lamre_col", [128, 16])
    lamim_col = din("lamim_col", [128, 16])
    logdt_col = din("logdt_col", [128, 16])
    bre_blk = din("bre_blk", [128, 4, 512])
    bim_blk = din("bim_blk", [128, 4, 512])
    cre_blk = din("cre_blk", [128, 16, 128])
    cim_blk = din("cim_blk", [128, 16, 128])
    ssmcols = din("ssmcols", [128, 3, 4])
    wglu_in = din("w_glu", [512, 512])
    iota_in = din("iotas", [128, 129])
    wout_in = din("w_out", [D, D])
    lnffn_rep = din("lnffn_rep", [128, D])
    wrt_in = din("w_router", [D, NE])
    brt_rep = din("brt_rep", [128, NE])
    bstart_rep = din("bstart_rep", [128, NBLK])
    bgu_col = din("bgu_col", [128, NE, 16])
    if "E" in phases:
        wgu_in = din("w_gu", [NE, D, 2 * D])
        wdn_in = din("w_dn", [NE, D, D])
        bdn_rep = din("bdn_rep", [NE, 128, D])
    fin_rep = din("fin_rep", [128, D])

    out = nc.dram_tensor("out", [SO, D], F32, kind="ExternalOutput").ap()
    cnt_out = nc.dram_tensor("cnt", [128, NE], F32, kind="ExternalOutput").ap()

    kT_d = dscr("kT_d", [4, 128, S], BF16)
    qT_d = dscr("qT_d", [4, 128, SO], BF16)
    uT_d = dscr("uT_d", [4, 128, S], BF16)
    v_d = dscr("v_d", [S, 512], BF16)
    mixT_d = dscr("mixT_d", [8, 128, SO], BF16)
    a_d = dscr("a_d", [SO, 512], BF16)
    h_d = dscr("h_d", [SO, D], F32)
    xpad_d = dscr("xpad_d", [NSLOT, D], BF16)
    y_d = dscr("y_d", [NSLOT, D], F32)

    with es:
        cx = Ctx(nc, es)

        def sb(name, shape, dt=F32):
            return es.enter_context(nc.sbuf_tensor("sb_" + name, list(shape), dt))

        ident = sb("ident", [128, 128], BF16)
        cx.dma("pool", "c0", ident[:], ident_in, writes=["ident"])
        onesb = sb("onesb", [128, 128], BF16)
        cx.op("dve", lambda e: e.memset(onesb[:], 1.0), writes=["onesb"])
        parsb = sb("parsb", [128, 4])
        cx.dma("sp", "c1", parsb[:], par, writes=["par"])
        iotas = sb("iotas", [128, 129])
        cx.dma("sp", "c2", iotas[:], iota_in, writes=["iotas"])
        slots_all = sb("slots_all", [128, NOWN, 4], I32)
        gates_all = sb("gates_all", [128, NOWN, 4])
        widx = sb("widx", [128, NBLK, 8], I32)
        bidx = sb("bidx", [128, NBLK], I32)
        gidx = sb("gidx", [128, NBLK], I32)
        bcw_reg = nc.gpsimd.to_reg(NE * D - 1)
        bcb_reg = nc.gpsimd.to_reg(NE * 128 - 1)
        bc_reg = nc.gpsimd.to_reg(NSLOT - 1)

        if "A" in phases:
            pa = contextlib.ExitStack()
            with pa:
                def sa(name, shape, dt=F32):
                    return pa.enter_context(nc.sbuf_tensor("sb_" + name, list(shape), dt))

                def psa(name, shape, dt=F32):
                    return pa.enter_context(nc.psum_tensor("ps_" + name, list(shape), dt))

                win = sa("win", [128, 8, 2304], BF16)
                with cx.dma_group("win"):
                    for kt in range(8):
                        cx.dma("pool", "win", win[:, kt, :],
                               w_in_ext[kt * 128:(kt + 1) * 128, :], writes=["win%d" % kt])
                WIN = ["win%d" % kt for kt in range(8)]
                gcol = sa("gcol", [128, 8])
                cx.dma("sp", "c3", gcol[:], lnmix_col, writes=["gcol"])
                rf = sa("rf", [128, 2])
                cx.dma("sp", "c4", rf[:], ropef, writes=["rf"])

                xt = [sa("xt%d" % i, [128, NBG, D]) for i in range(2)]
                xn = [sa("xn%d" % i, [128, NBG, D], BF16) for i in range(2)]
                xnT = [sa("xnT%d" % i, [128, 8, GT], BF16) for i in range(2)]
                junk = sa("junkA", [128, D], BF16)
                ss = sa("ssA", [128, 2, NBG])
                rstd = sa("rstdA", [128, 2, NBG])
                posi = [sa("posi%d" % i, [128, GT], I32) for i in range(2)]
                ang = sa("ang", [128, GT])
                angk = sa("angk", [128, GT], I32)
                angf = sa("angf", [128, GT])
                angc = sa("angc", [128, GT])
                ctab = [sa("ctab%d" % i, [128, GT]) for i in range(2)]
                stab = [sa("stab%d" % i, [128, GT]) for i in range(2)]
                t1 = [sa("t1_%d" % i, [128, GT]) for i in range(2)]
                t2 = [sa("t2_%d" % i, [128, GT]) for i in range(2)]
                kst = [sa("kst%d" % i, [128, 4, GT], BF16) for i in range(2)]
                qst = [sa("qst%d" % i, [128, 4, GT], BF16) for i in range(2)]
                ust = [sa("ust%d" % i, [128, 4, GT], BF16) for i in range(2)]
                vst = [sa("vst%d" % i, [128, NBG, 512], BF16) for i in range(2)]
                tp = [psa("tpA%d" % i, [128, 1024], BF16) for i in range(2)]
                pm = [psa("pmA%d" % i, [128, 512]) for i in range(2)]
                pp = [psa("ppA%d" % i, [128, 512]) for i in range(2)]
                pv = [psa("pvA%d" % i, [128, 512]) for i in range(2)]
                ctr = {"tp": 0, "pm": 0, "pv": 0, "t": 0}

                def load_group(gi):
                    s = gi % 2
                    cx.dma("sp", "xt%d" % s, xt[s][:],
                           x_perm[gi * GT:(gi + 1) * GT, :].rearrange("(n p) d -> p n d", p=128),
                           writes=["xt%d" % s])
                    cx.dma("sp", "posi%d" % s, posi[s][:], pos_rep[:, gi * GT:(gi + 1) * GT],
                           writes=["posi%d" % s])

                def stage1(gi):
                    s = gi % 2
                    cx.op("dve", lambda e: e.tensor_copy(out=ang[:], in_=posi[s][:]),
                          reads=["posi%d" % s], writes=["ang"])
                    cx.op("dve", lambda e: e.tensor_scalar(out=ang[:], in0=ang[:], scalar1=rf[:, 0:1],
                                                            scalar2=None, op0=ALU.mult),
                          reads=["ang", "rf"], writes=["ang"])

                    def sin_of(dst, key, shift):
                        cx.op("dve", lambda e: e.tensor_scalar(out=angc[:], in0=ang[:], scalar1=float(shift),
                                                                scalar2=None, op0=ALU.add),
                              reads=["ang"], writes=["angc"])
                        cx.op("dve", lambda e: e.tensor_scalar(out=angk[:], in0=angc[:],
                                                                scalar1=float(1.0 / TWO_PI), scalar2=None,
                                                                op0=ALU.mult),
                              reads=["angc"], writes=["angk"])
                        cx.op("dve", lambda e: e.tensor_copy(out=angf[:], in_=angk[:]),
                              reads=["angk"], writes=["angf"])
                        cx.op("dve", lambda e: e.tensor_scalar(out=angf[:], in0=angf[:], scalar1=float(-TWO_PI),
                                                                scalar2=None, op0=ALU.mult),
                              reads=["angf"], writes=["angf"])
                        cx.op("dve", lambda e: e.tensor_tensor(out=angc[:], in0=angc[:], in1=angf[:], op=ALU.add),
                              reads=["angf", "angc"], writes=["angc"])
                        cx.op("dve", lambda e: e.tensor_scalar(out=angc[:], in0=angc[:], scalar1=PI_LO, scalar2=-PI_LO,
                                                                op0=ALU.min, op1=ALU.max),
                              reads=["angc"], writes=["angc"])
                        cx.op("act", lambda e: e.activation(out=dst[:], in_=angc[:], func=AF.Sin),
                              reads=["angc"], writes=[key])

                    sin_of(stab[s], "stab%d" % s, 0.0)
                    cx.op("dve", lambda e: e.tensor_scalar(out=stab[s][:], in0=stab[s][:], scalar1=rf[:, 1:2],
                                                            scalar2=None, op0=ALU.mult),
                          reads=["stab%d" % s, "rf"], writes=["stab%d" % s])
                    sin_of(ctab[s], "ctab%d" % s, math.pi / 2)

                    for n in range(NBG):
                        cx.op("act", lambda e: e.activation(out=junk[:], in_=xt[s][:, n, :], func=AF.Square,
                                                            accum_out=ss[:, s, n:n + 1]),
                              reads=["xt%d" % s], writes=["junkA", "ss%d_%d" % (s, n)])
                    SSK = ["ss%d_%d" % (s, n) for n in range(NBG)]
                    cx.op("dve", lambda e: e.tensor_scalar(out=rstd[:, s, :], in0=ss[:, s, :], scalar1=1.0 / D,
                                                           scalar2=EPS, op0=ALU.mult, op1=ALU.add),
                          reads=SSK, writes=["rstd%d" % s])
                    cx.op("act", lambda e: e.activation(out=rstd[:, s, :], in_=rstd[:, s, :], func=AF.Sqrt),
                          reads=["rstd%d" % s], writes=["rstd%d" % s])
                    cx.op("dve", lambda e: e.reciprocal(out=rstd[:, s, :], in_=rstd[:, s, :]),
                          reads=["rstd%d" % s], writes=["rstd%d" % s])
                    for n in range(NBG):
                        cx.op("dve", lambda e: e.tensor_scalar(out=xn[s][:, n, :], in0=xt[s][:, n, :],
                                                               scalar1=rstd[:, s, n:n + 1], scalar2=None,
                                                               op0=ALU.mult),
                              reads=["xt%d" % s, "rstd%d" % s], writes=["xn%d_%d" % (s, n)])

                def stage1c(gi):
                    s = gi % 2
                    XNK = ["xn%d_%d" % (s, n) for n in range(NBG)]
                    for kt in range(8):
                        p = ctr["tp"] % 2
                        ctr["tp"] += 1
                        for n in range(NBG):
                            cx.op("pe", lambda e: e.transpose(out=tp[p][:, n * 128:(n + 1) * 128],
                                                              in_=xn[s][:, n, kt * 128:(kt + 1) * 128],
                                                              identity=ident[:]),
                                  reads=XNK + ["ident"], writes=["tp%d" % p])
                        if kt % 2 == 0:
                            cx.op("act", lambda e: e.activation(out=xnT[s][:, kt, :], in_=tp[p][:, 0:GT],
                                                                func=AF.Copy, scale=gcol[:, kt:kt + 1]),
                                  reads=["tp%d" % p, "gcol"], writes=["xnT%d_%d" % (s, kt)])
                        else:
                            cx.op("dve", lambda e: e.tensor_scalar(out=xnT[s][:, kt, :], in0=tp[p][:, 0:GT],
                                                                   scalar1=gcol[:, kt:kt + 1], scalar2=None,
                                                                   op0=ALU.mult),
                                  reads=["tp%d" % p, "gcol"], writes=["xnT%d_%d" % (s, kt)])
                    XTK = ["xnT%d_%d" % (s, kt) for kt in range(8)]


                def stage2(gi, part):
                    s = gi % 2
                    own = gi < NGO
                    XTK = ["xnT%d_%d" % (s, kt) for kt in range(8)]
                    def proj(ps, pskey, col0):
                        for kt in range(8):
                            cx.op("pe", lambda e: e.matmul(ps[:, 0:GT], win[:, kt, col0:col0 + 128],
                                                           xnT[s][:, kt, :], start=(kt == 0), stop=(kt == 7)),
                                  reads=XTK + WIN, writes=[pskey])

                    def rope_tile(col_main, col_perm, dst, dkey):
                        i = ctr["pm"] % 2
                        ctr["pm"] += 1
                        proj(pm[i], "pm%d" % i, col_main)
                        proj(pp[i], "pp%d" % i, col_perm)
                        j = ctr["t"] % 2
                        ctr["t"] += 1
                        cx.op("dve", lambda e: e.tensor_tensor(out=t1[j][:], in0=pm[i][:, 0:GT], in1=ctab[s][:],
                                                               op=ALU.mult),
                              reads=["pm%d" % i, "ctab%d" % s], writes=["t1_%d" % j])
                        cx.op("dve", lambda e: e.tensor_tensor(out=t2[j][:], in0=pp[i][:, 0:GT], in1=stab[s][:],
                                                               op=ALU.mult),
                              reads=["pp%d" % i, "stab%d" % s], writes=["t2_%d" % j])
                        cx.op("dve", lambda e: e.tensor_tensor(out=dst, in0=t1[j][:], in1=t2[j][:], op=ALU.add),
                              reads=["t1_%d" % j, "t2_%d" % j], writes=[dkey])

                    def plain_tile(col0, dst, dkey):
                        i = ctr["pm"] % 2
                        ctr["pm"] += 1
                        proj(pm[i], "pm%d" % i, col0)
                        cx.op("act", lambda e: e.activation(out=dst, in_=pm[i][:, 0:GT], func=AF.Copy),
                              reads=["pm%d" % i], writes=[dkey])

                    def qk_tiles(col0, colperm, stage, skey, dst_d):
                        rope_tile(col0, colperm, stage[:, 0, :], skey)
                        for b_ in range(1, 4):
                            plain_tile(col0 + b_ * 128, stage[:, b_, :], skey)
                        with cx.dma_group(skey):
                            for hh in range(4):
                                for m in range(2):
                                    p0 = hh * 32 + m * 16
                                    cx.dma("sp", skey, dst_d[hh, m * 64:(m + 1) * 64, gi * GT:(gi + 1) * GT].rearrange(
                                        "(b r) t -> r b t", r=16), stage[p0:p0 + 16, :, :], reads=[skey])

                    if part == 0:
                        qk_tiles(512, 2176, kst[s], "kst%d" % s, kT_d)
                        if own:
                            qk_tiles(0, 2048, qst[s], "qst%d" % s, qT_d)
                        return
                    for ct in range(4):
                        i = ctr["pm"] % 2
                        ctr["pm"] += 1
                        proj(pm[i], "pm%d" % i, 1536 + ct * 128)
                        cx.op("act", lambda e: e.activation(out=ust[s][:, ct, :], in_=pm[i][:, 0:GT], func=AF.Copy),
                              reads=["pm%d" % i], writes=["ust%d" % s])
                    cx.dma("sp", "ust%d" % s, uT_d[:, :, gi * GT:(gi + 1) * GT].rearrange("c p t -> p c t"),
                           ust[s][:], reads=["ust%d" % s])
                    for n in range(NBG):
                        i = ctr["pv"] % 2
                        ctr["pv"] += 1
                        for kt in range(8):
                            cx.op("pe", lambda e: e.matmul(pv[i][:], xnT[s][:, kt, n * 128:(n + 1) * 128],
                                                           win[:, kt, 1024:1536], start=(kt == 0), stop=(kt == 7)),
                                  reads=XTK + WIN, writes=["pv%d" % i])
                        cx.op("act", lambda e: e.activation(out=vst[s][:, n, :], in_=pv[i][:], func=AF.Copy),
                              reads=["pv%d" % i], writes=["vst%d" % s])
                    cx.dma("sp", "vst%d" % s, v_d[gi * GT:(gi + 1) * GT, :].rearrange("(n p) c -> p n c", p=128),
                           vst[s][:], reads=["vst%d" % s])
                load_group(0)
                if NG > 1:
                    load_group(1)
                stage1(0)
                stage1c(0)
                if NG > 1:
                    stage1(1)
                if NG > 2:
                    load_group(2)
                for gi in range(NG):
                    stage2(gi, 0)
                    if gi + 1 < NG:
                        stage1c(gi + 1)
                    stage2(gi, 1)
                    if gi + 2 < NG:
                        stage1(gi + 2)
                    if gi + 3 < NG:
                        load_group(gi + 3)
                cx.barrier()

        if "B" in phases:
            pb = contextlib.ExitStack()
            with pb:
                def sbb(name, shape, dt=F32):
                    return pb.enter_context(nc.sbuf_tensor("sb_" + name, list(shape), dt))

                def psb(name, shape, dt=F32):
                    return pb.enter_context(nc.psum_tensor("ps_" + name, list(shape), dt))

                mown = sbb("mown", [128, 128], BF16)
                moth = sbb("moth", [128, 128], BF16)
                cx.dma("pool", "c5", mown[:], maskown_in, writes=["mown"])
                cx.dma("pool", "c6", moth[:], maskoth_in, writes=["moth"])
                lv = sbb("lv", [128, 4, 64])
                cx.dma("sp", "c1", lv[:], lamvec, writes=["lv"])
                dg = sbb("dg", [128, 128])
                cx.dma("sp", "c2", dg[:], diffg_rep, writes=["dg"])
                cx.op("dve", lambda e: e.tensor_scalar(out=dg[:], in0=dg[:], scalar1=float(1.0 - LAMBDA_INIT),
                                                       scalar2=None, op0=ALU.mult), reads=["dg"], writes=["dg"])
                lprod = sbb("lprod", [128, 2, 64])
                lsum = sbb("lsum", [128, 2])
                neglam = sbb("neglam", [128, 1])
                cx.op("dve", lambda e: e.tensor_tensor(out=lprod[:, 0, :], in0=lv[:, 0, :], in1=lv[:, 1, :], op=ALU.mult),
                      reads=["lv"], writes=["lprod"])
                cx.op("dve", lambda e: e.tensor_tensor(out=lprod[:, 1, :], in0=lv[:, 2, :], in1=lv[:, 3, :], op=ALU.mult),
                      reads=["lv", "lprod"], writes=["lprod"])
                cx.op("dve", lambda e: e.tensor_reduce(out=lsum[:], in_=lprod[:], axis=AX.X, op=ALU.add),
                      reads=["lprod"], writes=["lsum"])
                cx.op("act", lambda e: e.activation(out=lsum[:], in_=lsum[:], func=AF.Exp),
                      reads=["lsum"], writes=["lsum"])
                cx.op("dve", lambda e: e.scalar_tensor_tensor(out=neglam[:], in0=lsum[:, 1:2], scalar=float(-LAMBDA_INIT),
                                                              in1=lsum[:, 0:1], op0=ALU.add, op1=ALU.subtract),
                      reads=["lsum"], writes=["neglam"])

                kTs = [sbb("kTs%d" % i, [128, S], BF16) for i in range(2)]
                qq = [sbb("qq%d" % i, [128, NOWN, 2, 128], BF16) for i in range(2)]
                vs = [sbb("vs%d" % i, [128, NB, 129], BF16) for i in range(2)]
                ast = [sbb("ast%d" % i, [128, NOWN, 128], BF16) for i in range(2)]
                for i in range(2):
                    cx.op("dve", lambda e: e.memset(vs[i][:, :, 128:129], 1.0), writes=["vs%d" % i])
                    cx.op("dve", lambda e: e.memset(qq[i][:], 0.0), writes=["qq%d" % i])
                pt = [sbb("pt%d" % i, [128, 4, 2, 128], BF16) for i in range(2)]
                mk2 = {}
                for nm, src in (("mown", mown), ("moth", moth)):
                    t_ = sbb(nm + "2", [128, 2, 128], BF16)
                    for m in range(2):
                        cx.op("dve", lambda e: e.tensor_copy(out=t_[:, m, :], in_=src[:]), reads=[nm], writes=[nm + "2"])
                    mk2[nm] = t_
                rr = [sbb("rrB%d" % i, [128, 4]) for i in range(2)]
                a1 = sbb("a1B", [128, 128])
                a2 = sbb("a2B", [128, 128])
                asq = sbb("asqB", [128, 128])
                ssb = [sbb("ssB%d" % i, [128, 2]) for i in range(2)]
                scq = [psb("scB%d" % i, [128, 4, 2, 128]) for i in range(2)]
                o1 = [psb("o1B%d" % i, [128, 512]) for i in range(2)]
                o2 = [psb("o2B%d" % i, [128, 512]) for i in range(2)]

                def load_head(hh):
                    s = hh % 2
                    cx.dma("sp", "kTs%d" % s, kTs[s][:], kT_d[hh], writes=["kTs%d" % s])
                    with cx.dma_group("qq%d" % s):
                        for m in range(2):
                            cx.dma("sp", "qq%d" % s, qq[s][m * 64:(m + 1) * 64, :, m, :],
                                   qT_d[hh, m * 64:(m + 1) * 64, :].rearrange("p (n t) -> p n t", t=128),
                                   reads=["qq%d" % s], writes=["qq%d" % s])
                    with cx.dma_group("vs%d" % s):
                        for n0 in range(0, NB, 16):
                            n1 = min(NB, n0 + 16)
                            cx.dma("sp", "vs%d" % s, vs[s][:, n0:n1, 0:128],
                                   v_d[n0 * 128:n1 * 128, hh * 128:(hh + 1) * 128].rearrange("(n p) c -> p n c", p=128),
                                   reads=["vs%d" % s], writes=["vs%d" % s])

                groups = []
                nqb = 0
                for hh in range(4):
                    for j in range(NOWN):
                        tl = []
                        for i in range(j + 1):
                            tl.append((i, "mown" if i == j else None))
                            tl.append((NOWN + i, "moth" if i == j else None))
                        ngrp = (len(tl) + 3) // 4
                        for gi_ in range(ngrp):
                            groups.append(dict(hh=hh, j=j, tiles=tl[gi_ * 4:(gi_ + 1) * 4], first=(gi_ == 0),
                                               last=(gi_ == ngrp - 1), ob=nqb % 2, b=len(groups) % 2))
                        nqb += 1

                def emit_scores(G):
                    s = G["hh"] % 2
                    b = G["b"]
                    j = G["j"]
                    for t, (tile, mk) in enumerate(G["tiles"]):
                        ks = slice(tile * 128, (tile + 1) * 128)
                        cx.op("pe", lambda e: e.matmul(scq[b][:, t, :, :], kTs[s][:, ks], qq[s][:, j, :, :],
                                                       start=True, stop=(mk is None)),
                              reads=["kTs%d" % s, "qq%d" % s], writes=["sc%d" % b])
                        if mk is not None:
                            cx.op("pe", lambda e: e.matmul(scq[b][:, t, :, :], ident[:], mk2[mk][:], start=False, stop=True),
                                  reads=["ident", "mown2", "moth2"], writes=["sc%d" % b])
                    n = len(G["tiles"])
                    cx.op("act", lambda e: e.activation(out=pt[b][:, 0:n, :, :], in_=scq[b][:, 0:n, :, :], func=AF.Exp, scale=0.125),
                          reads=["sc%d" % b], writes=["pt%d" % b])

                def emit_pv(G):
                    s = G["hh"] % 2
                    b = G["b"]
                    ob = G["ob"]
                    n = len(G["tiles"])
                    for t, (tile, mk) in enumerate(G["tiles"]):
                        first = (G["first"] and t == 0)
                        last = (G["last"] and t == n - 1)
                        cx.op("pe", lambda e: e.matmul(o1[ob][:, 0:129], pt[b][:, t, 0, :], vs[s][:, tile, :], start=first, stop=last),
                              reads=["pt%d" % b, "vs%d" % s], writes=["o1_%d" % ob])
                        cx.op("pe", lambda e: e.matmul(o2[ob][:, 0:129], pt[b][:, t, 1, :], vs[s][:, tile, :], start=first, stop=last),
                              reads=["pt%d" % b, "vs%d" % s], writes=["o2_%d" % ob])

                def emit_epilogue(G):
                    s = G["hh"] % 2
                    ob = G["ob"]
                    j = G["j"]
                    O1 = o1[ob]; O2 = o2[ob]; R = rr[ob]; SS = ssb[ob]
                    RK = "rr%d" % ob; SK = "ssb%d" % ob
                    cx.op("dve", lambda e: e.reciprocal(out=R[:, 0:1], in_=O1[:, 128:129]), reads=["o1_%d" % ob], writes=[RK])
                    cx.op("dve", lambda e: e.reciprocal(out=R[:, 1:2], in_=O2[:, 128:129]), reads=["o2_%d" % ob, RK], writes=[RK])
                    cx.op("dve", lambda e: e.tensor_tensor(out=R[:, 2:3], in0=R[:, 1:2], in1=neglam[:], op=ALU.mult),
                          reads=[RK, "neglam"], writes=[RK])
                    cx.op("dve", lambda e: e.tensor_scalar(out=a1[:], in0=O1[:, 0:128], scalar1=R[:, 0:1], scalar2=None, op0=ALU.mult),
                          reads=["o1_%d" % ob, RK], writes=["a1"])
                    cx.op("dve", lambda e: e.scalar_tensor_tensor(out=a2[:], in0=O2[:, 0:128], scalar=R[:, 2:3], in1=a1[:],
                                                                  op0=ALU.mult, op1=ALU.add),
                          reads=["o2_%d" % ob, RK, "a1"], writes=["a2"])
                    cx.op("dve", lambda e: e.tensor_tensor(out=asq[:], in0=a2[:], in1=a2[:], op=ALU.mult), reads=["a2"], writes=["asq"])
                    cx.op("dve", lambda e: e.tensor_reduce(out=SS[:, 0:1], in_=asq[:], axis=AX.X, op=ALU.add), reads=["asq"], writes=[SK])
                    cx.op("dve", lambda e: e.tensor_scalar(out=SS[:, 0:1], in0=SS[:, 0:1], scalar1=1.0 / 128, scalar2=EPS,
                                                           op0=ALU.mult, op1=ALU.add), reads=[SK], writes=[SK])
                    cx.op("act", lambda e: e.activation(out=SS[:, 1:2], in_=SS[:, 0:1], func=AF.Ln), reads=[SK], writes=[SK + "b"])
                    cx.op("act", lambda e: e.activation(out=SS[:, 1:2], in_=SS[:, 1:2], func=AF.Exp, scale=-0.5),
                          reads=[SK + "b"], writes=[SK + "b"])
                    cx.op("dve", lambda e: e.scalar_tensor_tensor(out=ast[s][:, j, :], in0=a2[:], scalar=SS[:, 1:2], in1=dg[:],
                                                                  op0=ALU.mult, op1=ALU.mult),
                          reads=["a2", SK + "b", "dg"], writes=["ast%d" % s])
                    if j == NOWN - 1:
                        hh = G["hh"]
                        cx.dma("sp", "ast%d" % s, a_d[:, hh * 128:(hh + 1) * 128].rearrange("(n p) c -> p n c", p=128),
                               ast[s][:], reads=["ast%d" % s])

                load_head(0)
                zt = sbb("zt", [128, 8, D], BF16)
                cx.op("dve", lambda g: g.memset(zt[:], 0.0), writes=["zt"])
                nblk = NSLOT // 128
                with cx.dma_group("zero"):
                    for b0 in range(0, nblk, 8):
                        nb8 = min(8, nblk - b0)
                        cx.dma("sp", "zero", xpad_d[b0 * 128:(b0 + nb8) * 128, :].rearrange("(n p) d -> p n d", p=128),
                               zt[:, 0:nb8, :], reads=["zt"], writes=["xpad"])
                emit_scores(groups[0])
                for gi_, G in enumerate(groups):
                    if G["first"] and G["j"] == 0 and G["hh"] + 1 < 4:
                        load_head(G["hh"] + 1)
                    if gi_ + 1 < len(groups):
                        emit_scores(groups[gi_ + 1])
                    emit_pv(G)
                    if G["last"]:
                        emit_epilogue(G)
                cx.barrier()

        if "C" in phases:
            pc = contextlib.ExitStack()
            with pc:
                def sc_(name, shape, dt=F32):
                    return pc.enter_context(nc.sbuf_tensor("sb_" + name, list(shape), dt))

                def psc(name, shape, dt=F32):
                    return pc.enter_context(nc.psum_tensor("ps_" + name, list(shape), dt))

                def TT(e, o, a, b, op, R, W):
                    cx.op(e, lambda g: g.tensor_tensor(out=o, in0=a, in1=b, op=op), reads=R, writes=W)

                def TS(e, o, a, s1, op0, R, W, s2=None, op1=None):
                    if op1 is None:
                        cx.op(e, lambda g: g.tensor_scalar(out=o, in0=a, scalar1=s1, scalar2=None, op0=op0),
                              reads=R, writes=W)
                    else:
                        cx.op(e, lambda g: g.tensor_scalar(out=o, in0=a, scalar1=s1, scalar2=s2, op0=op0, op1=op1),
                              reads=R, writes=W)

                def ACT(o, a, func, R, W, **kw):
                    cx.op("act", lambda g: g.activation(out=o, in_=a, func=func, **kw), reads=R, writes=W)

                PAre = sc_("PAre", [128, 2048]); PAim = sc_("PAim", [128, 2048])
                PBre = sc_("PBre", [128, 2048]); PBim = sc_("PBim", [128, 2048])
                Qre = sc_("Qre", [128, 16, 128]); Qim = sc_("Qim", [128, 16, 128])
                A256 = sc_("A256", [128, 2, 16])
                Bblk = sc_("Bblk", [128, 4, 1024], BF16)
                Cre = sc_("Cre", [128, 16, 128], BF16)
                Cimn = sc_("Cimn", [128, 16, 128], BF16)
                wglu = sc_("wglu", [128, 4, 512], BF16)
                scol = sc_("scol", [128, 3, 4])
                triA = sc_("triA", [128, 128], BF16)
                triB = sc_("triB", [128, 128], BF16)
                cx.dma("pool", "c5", triA[:], triA_in, writes=["triA"])
                cx.dma("pool", "c6", triB[:], triB_in, writes=["triB"])
                triAn = sc_("triAn", [128, 128], BF16); triBn = sc_("triBn", [128, 128], BF16)
                onesn = sc_("onesn", [128, 1], BF16); Cren = sc_("Cren", [128, 16, 128], BF16)
                cx.op("dve", lambda g: g.tensor_scalar(out=triAn[:], in0=triA[:], scalar1=-1.0, scalar2=None, op0=ALU.mult),
                      reads=["triA"], writes=["triAn"])
                cx.op("dve", lambda g: g.tensor_scalar(out=triBn[:], in0=triB[:], scalar1=-1.0, scalar2=None, op0=ALU.mult),
                      reads=["triB"], writes=["triBn"])
                cx.op("dve", lambda g: g.memset(onesn[:], -1.0), writes=["onesn"])
                cx.dma("pool", "c7", Cre[:], cre_blk, writes=["Cre"])
                cx.dma("pool", "c8", Cimn[:], cim_blk, writes=["Cimn"])
                cx.op("dve", lambda g: g.tensor_scalar(out=Cimn[:], in0=Cimn[:], scalar1=-1.0, scalar2=None, op0=ALU.mult),
                      reads=["Cimn"], writes=["Cimn"])
                cx.op("dve", lambda g: g.tensor_scalar(out=Cren[:], in0=Cre[:], scalar1=-1.0, scalar2=None, op0=ALU.mult),
                      reads=["Cre"], writes=["Cren"])
                cx.dma("pool", "c9", wglu[:], wglu_in.rearrange("(k p) n -> p k n", p=128), writes=["wglu"])
                cx.dma("sp", "c3", scol[:], ssmcols, writes=["scol"])

                pcs = contextlib.ExitStack()
                with pcs:
                    def st_(name, shape, dt=F32):
                        return pcs.enter_context(nc.sbuf_tensor("sb_" + name, list(shape), dt))
                    ar = st_("ar", [128, 2048]); ai = st_("ai", [128, 2048]); dtr = st_("dtr", [128, 2048])
                    lre = st_("lre", [128, 2048]); lim = st_("lim", [128, 2048])
                    mag = st_("mag", [128, 2048]); ph = st_("ph", [128, 2048]); ph2 = st_("ph2", [128, 2048])
                    phk = st_("phk", [128, 2048], I32); phf = st_("phf", [128, 2048])
                    sn = st_("sn", [128, 2048]); cs = st_("cs", [128, 2048])
                    cx.dma("sp", "c1", lre[:], lamre_row, writes=["lre"])
                    cx.dma("sp", "c2", lim[:], lamim_row, writes=["lim"])
                    cx.dma("sp", "c4", dtr[:], logdt_row, writes=["dtr"])
                    ACT(dtr[:], dtr[:], AF.Exp, ["dtr"], ["dtr"])
                    TT("dve", ar[:], lre[:], dtr[:], ALU.mult, ["lre", "dtr"], ["ar"])
                    TT("dve", ai[:], lim[:], dtr[:], ALU.mult, ["lim", "dtr"], ["ai"])
                    posc = st_("posc", [128, 4])
                    TS("dve", posc[:, 0:1], iotas[:, 0:1], parsb[:, 0:1], ALU.add, ["iotas", "par"], ["posc"], -1.0, ALU.mult)
                    TS("dve", posc[:, 1:2], iotas[:, 0:1], parsb[:, 1:2], ALU.add, ["iotas", "par", "posc"], ["posc"], -1.0, ALU.mult)
                    cx.op("dve", lambda g: g.memset(posc[:, 2:3], 1.0), reads=["posc"], writes=["posc"])

                    def sincos(n):
                        for shift, dst, key in ((0.0, sn, "sn"), (math.pi / 2, cs, "cs")):
                            TS("dve", ph2[:, :n], ph[:, :n], float(shift), ALU.add, ["ph"], ["ph2"])
                            TS("dve", phk[:, :n], ph2[:, :n], float(1.0 / TWO_PI), ALU.mult, ["ph2"], ["phk"])
                            cx.op("dve", lambda g: g.tensor_copy(out=phf[:, :n], in_=phk[:, :n]), reads=["phk"], writes=["phf"])
                            cx.op("dve", lambda g: g.scalar_tensor_tensor(out=ph2[:, :n], in0=phf[:, :n], scalar=float(-TWO_PI),
                                                                          in1=ph2[:, :n], op0=ALU.mult, op1=ALU.add),
                                  reads=["phf", "ph2"], writes=["ph2"])
                            TS("dve", ph2[:, :n], ph2[:, :n], PI_LO, ALU.min, ["ph2"], ["ph2"], -PI_LO, ALU.max)
                            ACT(dst[:, :n], ph2[:, :n], AF.Sin, ["ph2"], [key])

                    def row_table(dre, dim, kre, kim, pcol):
                        ACT(mag[:], ar[:], AF.Exp, ["ar", "posc"], ["mag"], scale=posc[:, pcol:pcol + 1])
                        TS("dve", ph[:], ai[:], posc[:, pcol:pcol + 1], ALU.mult, ["ai", "posc"], ["ph"])
                        sincos(2048)
                        TT("dve", dre, mag[:], cs[:], ALU.mult, ["mag", "cs"], [kre])
                        TT("dve", dim, mag[:], sn[:], ALU.mult, ["mag", "sn"], [kim])

                    row_table(PAre[:], PAim[:], "PAre", "PAim", 0)
                    row_table(PBre[:], PBim[:], "PBre", "PBim", 1)
                    are = st_("are", [128, 2048]); aim = st_("aim", [128, 2048])
                    row_table(are[:], aim[:], "are", "aim", 2)
                    TS("dve", are[:], are[:], -1.0, ALU.add, ["are"], ["are"])
                    den = mag
                    TT("dve", den[:], lre[:], lre[:], ALU.mult, ["lre", "mag"], ["mag"])
                    TT("dve", ph[:], lim[:], lim[:], ALU.mult, ["lim", "ph"], ["ph"])
                    TT("dve", den[:], den[:], ph[:], ALU.add, ["mag", "ph"], ["mag"])
                    cx.op("dve", lambda g: g.reciprocal(out=den[:], in_=den[:]), reads=["mag"], writes=["mag"])
                    fre = sn; fim = cs
                    TT("dve", ph[:], are[:], lre[:], ALU.mult, ["are", "lre"], ["ph"])
                    TT("dve", ph2[:], aim[:], lim[:], ALU.mult, ["aim", "lim"], ["ph2"])
                    TT("dve", ph[:], ph[:], ph2[:], ALU.add, ["ph", "ph2"], ["ph"])
                    TT("dve", fre[:], ph[:], den[:], ALU.mult, ["ph", "mag", "sn"], ["sn"])
                    TT("dve", ph[:], aim[:], lre[:], ALU.mult, ["aim", "lre"], ["ph"])
                    TT("dve", ph2[:], are[:], lim[:], ALU.mult, ["are", "lim"], ["ph2"])
                    TT("dve", ph[:], ph[:], ph2[:], ALU.subtract, ["ph", "ph2"], ["ph"])
                    TT("dve", fim[:], ph[:], den[:], ALU.mult, ["ph", "mag", "cs"], ["cs"])
                    braw = st_("braw", [128, 2, 4, 512])
                    cx.dma("sp", "c1", braw[:, 0], bre_blk, writes=["braw0"])
                    cx.dma("sp", "c2", braw[:, 1], bim_blk, writes=["braw1"])
                    for ct in range(4):
                        fs = slice(ct * 512, (ct + 1) * 512)
                        TT("dve", ph[:, 0:512], braw[:, 0, ct, :], fre[:, fs], ALU.mult, ["braw0", "sn"], ["ph"])
                        TT("dve", ph2[:, 0:512], braw[:, 1, ct, :], fim[:, fs], ALU.mult, ["braw1", "cs"], ["ph2"])
                        TT("dve", Bblk[:, ct, 0:512], ph[:, 0:512], ph2[:, 0:512], ALU.subtract, ["ph", "ph2"], ["Bblk"])
                        TT("dve", ph[:, 0:512], braw[:, 0, ct, :], fim[:, fs], ALU.mult, ["braw0", "cs"], ["ph"])
                        TT("dve", ph2[:, 0:512], braw[:, 1, ct, :], fre[:, fs], ALU.mult, ["braw1", "sn"], ["ph2"])
                        TT("dve", Bblk[:, ct, 512:1024], ph[:, 0:512], ph2[:, 0:512], ALU.add, ["ph", "ph2"], ["Bblk"])
                    cc = st_("cc", [128, 3, 16])
                    cx.dma("sp", "c1", cc[:, 0, :], lamre_col, writes=["cc0"])
                    cx.dma("sp", "c2", cc[:, 1, :], lamim_col, writes=["cc1"])
                    cx.dma("sp", "c4", cc[:, 2, :], logdt_col, writes=["cc2"])
                    ACT(cc[:, 2, :], cc[:, 2, :], AF.Exp, ["cc2"], ["cc2"])
                    TT("dve", cc[:, 0, :], cc[:, 0, :], cc[:, 2, :], ALU.mult, ["cc0", "cc2"], ["cc0"])
                    TT("dve", cc[:, 1, :], cc[:, 1, :], cc[:, 2, :], ALU.mult, ["cc1", "cc2"], ["cc1"])
                    tpos = st_("tpos", [128, 128])
                    TS("dve", tpos[:], iotas[:, 1:129], parsb[:, 0:1], ALU.add, ["iotas", "par"], ["tpos"])
                    for p in range(16):
                        ACT(mag[:, p * 128:(p + 1) * 128], tpos[:], AF.Exp, ["tpos", "cc0"], ["mag"], scale=cc[:, 0, p:p + 1])
                        TS("dve", ph[:, p * 128:(p + 1) * 128], tpos[:], cc[:, 1, p:p + 1], ALU.mult, ["tpos", "cc1"], ["ph"])
                    sincos(2048)
                    TT("dve", Qre[:].rearrange("p a t -> p (a t)"), mag[:], cs[:], ALU.mult, ["mag", "cs"], ["Qre"])
                    TT("dve", Qim[:].rearrange("p a t -> p (a t)"), mag[:], sn[:], ALU.mult, ["mag", "sn"], ["Qim"])
                    ACT(mag[:, 0:16], cc[:, 0, :], AF.Exp, ["cc0", "mag"], ["mag"], scale=256.0)
                    TS("dve", ph[:, 0:16], cc[:, 1, :], 256.0, ALU.mult, ["cc1", "ph"], ["ph"])
                    sincos(16)
                    TT("dve", A256[:, 0, :], mag[:, 0:16], cs[:, 0:16], ALU.mult, ["mag", "cs"], ["A256"])
                    TT("dve", A256[:, 1, :], mag[:, 0:16], sn[:, 0:16], ALU.mult, ["mag", "sn", "A256"], ["A256"])
                    cx.barrier()

                u2 = [sc_("u2_%d" % i, [128, 4, 2, 128], BF16) for i in range(2)]
                sbq = [[sc_("sbq%d_%d" % (x, q), [128, 2048], BF16) for q in range(4)] for x in range(2)]
                bus = [[sc_("bus%d_%d" % (i, r), [128, 512]) for r in range(2)] for i in range(2)]
                carry = [sc_("carry%d" % i, [128, 2, 16]) for i in range(2)]
                cs_ = sc_("csum", [128, 2, 16]); cm = sc_("cmul", [128, 4, 16])
                spre = [sc_("spre%d" % i, [128, 4, 128]) for i in range(2)]
                spim = [sc_("spim%d" % i, [128, 4, 128]) for i in range(2)]
                mq = [[sc_("mq%d_%d" % (i, k), [128, 4, 128], BF16) for i in range(4)] for k in range(2)]
                ypre = sc_("ypre", [128, 4, 128]); yg = sc_("yg", [128, 4, 128]); ygb = sc_("ygb", [128, 4, 128], BF16)
                sg = sc_("sg", [128, 4, 128]); y2 = sc_("y2", [128, 4, 128]); sq = sc_("sq", [128, 4, 128], BF16)
                rs = sc_("rsC", [128, 128]); rs2 = sc_("rsC2", [128, 128])
                sst = [sc_("sst%d" % i, [128, 4, 128], BF16) for i in range(2)]
                bu1 = [psc("bu0_%d" % r, [128, 512]) for r in range(2)]
                bu = [bu1, bu1]
                ypsC = [psc("ypsC%d" % i, [128, 512]) for i in range(2)]
                Sps = [psc("Sps%d" % r, [128, 4, 128]) for r in range(2)]
                totp = psc("totp", [128, 16, 2])
                misc = psc("miscC", [128, 4, 128])
                cx.op("dve", lambda g: g.memset(carry[0][:], 0.0), writes=["carry0"])
                st = {"nbu": 0, "nrd": 0}
                SBKC = [["sbq%d_%d_%d" % (x, q, c_) for x in range(2) for q in range(4)] for c_ in range(4)]
                SBK = [k_ for c_ in range(4) for k_ in SBKC[c_]]

                def load_u(j):
                    sl = j % 2
                    cx.dma("sp", "u2a%d" % sl, u2[sl][:, :, 0, :],
                           uT_d[:, :, j * 128:(j + 1) * 128].rearrange("c p t -> p c t"), writes=["u2a%d" % sl])
                    cx.dma("sp", "u2b%d" % sl, u2[sl][:, :, 1, :],
                           uT_d[:, :, (NOWN + j) * 128:(NOWN + j + 1) * 128].rearrange("c p t -> p c t"),
                           writes=["u2b%d" % sl])

                def prescale_iter(j, X, ct):
                    sl = j % 2
                    UK = ["u2a%d" % sl, "u2b%d" % sl]
                    Pre = PAre if X == 0 else PBre
                    Pim = PAim if X == 0 else PBim
                    b = st["nbu"] % 2
                    st["nbu"] += 1
                    fs = slice(ct * 512, (ct + 1) * 512)
                    for r in range(2):
                        cx.op("pe", lambda g: g.matmul(bu[b][r][:], u2[sl][:, ct, X, :],
                                                       Bblk[:, ct, r * 512:(r + 1) * 512], start=True, stop=True),
                              reads=UK + ["Bblk"], writes=["bu0_%d" % r])
                    Q = sbq[X]
                    TT("dve", Q[0][:, fs], bu[b][0][:], Pre[:, fs], ALU.mult, ["bu0_0"], ["sbq%d_0_%d" % (X, ct)])
                    TT("dve", Q[1][:, fs], bu[b][1][:], Pim[:, fs], ALU.mult, ["bu0_1"], ["sbq%d_1_%d" % (X, ct)])
                    TT("dve", Q[2][:, fs], bu[b][0][:], Pim[:, fs], ALU.mult, ["bu0_0"], ["sbq%d_2_%d" % (X, ct)])
                    TT("dve", Q[3][:, fs], bu[b][1][:], Pre[:, fs], ALU.mult, ["bu0_1"], ["sbq%d_3_%d" % (X, ct)])

                def tail_steps(j):
                    sl = j % 2
                    so = sst[sl]
                    steps = []

                    def s0():
                        ACT(yg[:], ypre[:], AF.Gelu, ["ypre"], ["yg"])
                        cx.op("pool", lambda g: g.tensor_copy(out=ygb[:], in_=yg[:]), reads=["yg"], writes=["ygb"])

                    def s1():
                        for co in range(4):
                            for kt in range(4):
                                cx.op("pe", lambda g: g.matmul(misc[:, co, :], wglu[:, kt, co * 128:(co + 1) * 128],
                                                               ygb[:, kt, :], start=(kt == 0), stop=(kt == 3)),
                                      reads=["wglu", "ygb"], writes=["misc"])
                        for co in range(4):
                            ACT(sg[:, co, :], misc[:, co, :], AF.Sigmoid, ["misc", "scol"], ["sg"], bias=scol[:, 1, co:co + 1])

                    def s2():
                        TT("dve", y2[:], yg[:], sg[:], ALU.mult, ["yg", "sg"], ["y2"])
                        TT("pool", sq[:], y2[:], y2[:], ALU.mult, ["y2"], ["sq"])

                    def s3():
                        for kt in range(4):
                            cx.op("pe", lambda g: g.matmul(misc[:, 0, :], onesb[:], sq[:, kt, :], start=(kt == 0), stop=(kt == 3)),
                                  reads=["onesb", "sq"], writes=["misc"])

                    def s4():
                        TS("dve", rs[:], misc[:, 0, :], 1.0 / 512, ALU.mult, ["misc"], ["rs"], EPS, ALU.add)
                        ACT(rs2[:], rs[:], AF.Ln, ["rs"], ["rs2"])
                        ACT(rs2[:], rs2[:], AF.Exp, ["rs2"], ["rs2"], scale=-0.5)

                    def s5():
                        for ct in range(4):
                            cx.op("dve", lambda g: g.scalar_tensor_tensor(out=so[:, ct, :], in0=y2[:, ct, :],
                                                                          scalar=scol[:, 2, ct:ct + 1], in1=rs2[:],
                                                                          op0=ALU.mult, op1=ALU.mult),
                                  reads=["y2", "scol", "rs2"], writes=["sst%d" % sl])
                        cx.dma("sp", "sst%d" % sl, mixT_d[4:8, :, j * 128:(j + 1) * 128].rearrange("c p t -> p c t"),
                               so[:], reads=["sst%d" % sl])
                    return [s0, s1, s2, s3, s4, s5]

                def totals(j):
                    for p in range(16):
                        cols = slice(p * 128, (p + 1) * 128)
                        for r in range(2):
                            terms = [(x, 2 * r + q, (onesn if (r == 0 and q == 1) else onesb)) for x in range(2) for q in range(2)]
                            for ti, (x, q, ov) in enumerate(terms):
                                cx.op("pe", lambda g: g.matmul(totp[:, p, r:r + 1], sbq[x][q][:, cols], ov[:, 0:1],
                                                               start=(ti == 0), stop=(ti == 3)),
                                      reads=SBK + ["onesb", "onesn"], writes=["totp"])

                def round_S(j, rd):
                    cin = carry[j % 2]; ckin = "carry%d" % (j % 2)
                    k = rd % 2
                    for r in range(2):
                        for pp in range(4):
                            cols = slice((rd * 4 + pp) * 128, (rd * 4 + pp + 1) * 128)
                            terms = []
                            for x in range(2):
                                tp_, tn_ = (triA, triAn) if x == 0 else (triB, triBn)
                                terms.append((x, 2 * r, tp_))
                                terms.append((x, 2 * r + 1, tn_ if r == 0 else tp_))
                            for ti, (x, q, tm) in enumerate(terms):
                                cx.op("pe", lambda g: g.matmul(Sps[r][:, pp, :], sbq[x][q][:, cols], tm[:],
                                                               start=(ti == 0), stop=(ti == 3)),
                                      reads=SBKC[rd] + ["triA", "triB", "triAn", "triBn"], writes=["Sps%d" % r])
                    for pp in range(4):
                        p = rd * 4 + pp
                        ACT(spre[k][:, pp, :], Sps[0][:, pp, :], AF.Identity, ["Sps0", ckin], ["spre%d" % k], bias=cin[:, 0, p:p + 1])
                        ACT(spim[k][:, pp, :], Sps[1][:, pp, :], AF.Identity, ["Sps1", ckin], ["spim%d" % k], bias=cin[:, 1, p:p + 1])

                def round_X(j, rd):
                    sl = j % 2
                    k = rd % 2
                    M = mq[k]
                    MK = ["mq%d_%d" % (i, k) for i in range(4)]
                    qs_ = slice(rd * 4, rd * 4 + 4)
                    TT("dve", M[0][:], spre[k][:], Qre[:, qs_, :], ALU.mult, ["spre%d" % k], [MK[0]])
                    TT("dve", M[1][:], spim[k][:], Qim[:, qs_, :], ALU.mult, ["spim%d" % k], [MK[1]])
                    TT("dve", M[2][:], spre[k][:], Qim[:, qs_, :], ALU.mult, ["spre%d" % k], [MK[2]])
                    TT("dve", M[3][:], spim[k][:], Qre[:, qs_, :], ALU.mult, ["spim%d" % k], [MK[3]])
                    cw = [Cre, Cren, Cimn, Cimn]
                    for pp in range(4):
                        p = rd * 4 + pp
                        for q in range(4):
                            cx.op("pe", lambda g: g.matmul(ypsC[rd % 2][:, 0:128], cw[q][:, p, :], M[q][:, pp, :],
                                                           start=(pp == 0 and q == 0), stop=(pp == 3 and q == 3)),
                                  reads=["Cre", "Cren", "Cimn", MK[q]], writes=["ypsC%d" % (rd % 2)])

                def round_Y(j, rd):
                    sl = j % 2
                    cx.op("dve", lambda g: g.scalar_tensor_tensor(out=ypre[:, rd, :], in0=u2[sl][:, rd, 0, :],
                                                                  scalar=scol[:, 0, rd:rd + 1], in1=ypsC[rd % 2][:, 0:128],
                                                                  op0=ALU.mult, op1=ALU.add),
                          reads=["ypsC%d" % (rd % 2), "scol", "u2a%d" % sl], writes=["ypre"])

                def carry_update(j):
                    cin = carry[j % 2]; cout = carry[(j + 1) % 2]
                    ckin = "carry%d" % (j % 2); ckout = "carry%d" % ((j + 1) % 2)
                    TT("dve", cs_[:, 0, :], cin[:, 0, :], totp[:, :, 0], ALU.add, [ckin, "totp"], ["csum"])
                    TT("dve", cs_[:, 1, :], cin[:, 1, :], totp[:, :, 1], ALU.add, [ckin, "totp", "csum"], ["csum"])
                    TT("dve", cm[:, 0, :], A256[:, 0, :], cs_[:, 0, :], ALU.mult, ["csum"], ["cm"])
                    TT("dve", cm[:, 1, :], A256[:, 1, :], cs_[:, 1, :], ALU.mult, ["csum", "cm"], ["cm"])
                    TT("dve", cm[:, 2, :], A256[:, 0, :], cs_[:, 1, :], ALU.mult, ["csum", "cm"], ["cm"])
                    TT("dve", cm[:, 3, :], A256[:, 1, :], cs_[:, 0, :], ALU.mult, ["csum", "cm"], ["cm"])
                    TT("dve", cout[:, 0, :], cm[:, 0, :], cm[:, 1, :], ALU.subtract, ["cm"], [ckout])
                    TT("dve", cout[:, 1, :], cm[:, 2, :], cm[:, 3, :], ALU.add, ["cm", ckout], [ckout])

                load_u(0)
                pending = []
                for j in range(NOWN):
                    if j + 1 < NOWN:
                        load_u(j + 1)
                    it = 0
                    for ct in range(4):
                        for X in range(2):
                            prescale_iter(j, X, ct)
                            if pending and it < len(pending):
                                pending[it]()
                            it += 1
                        round_S(j, ct)
                        if ct >= 1:
                            round_X(j, ct - 1)
                        if ct >= 2:
                            round_Y(j, ct - 2)
                    pending = []
                    round_X(j, 3)
                    round_Y(j, 2)
                    totals(j)
                    round_Y(j, 3)
                    carry_update(j)
                    pending = tail_steps(j)
                for stp in pending:
                    stp()
                cx.barrier()

        if "D" in phases:
            pd = contextlib.ExitStack()
            with pd:
                def sd(name, shape, dt=F32):
                    return pd.enter_context(nc.sbuf_tensor("sb_" + name, list(shape), dt))

                def psd(name, shape, dt=F32):
                    return pd.enter_context(nc.psum_tensor("ps_" + name, list(shape), dt))

                wout = sd("wout", [128, 8, D], BF16)
                cx.dma("pool", "c5", wout[:], wout_in.rearrange("(k p) n -> p k n", p=128), writes=["wout"])
                wrt = sd("wrt", [128, 8, NE], BF16)
                cx.dma("pool", "c6", wrt[:], wrt_in.rearrange("(k p) n -> p k n", p=128), writes=["wrt"])
                lnf = sd("lnf", [128, D])
                cx.dma("sp", "c1", lnf[:], lnffn_rep, writes=["lnf"])
                brt = sd("brt", [128, NE])
                cx.dma("sp", "c2", brt[:], brt_rep, writes=["brt"])
                stri = sd("stri", [128, 128], BF16)
                triAd = sd("triAd", [128, 128], BF16)
                cx.dma("pool", "c7", triAd[:], triA_in, writes=["triAd"])
                cx.op("dve", lambda g: g.tensor_tensor(out=stri[:], in0=triAd[:], in1=ident[:], op=ALU.subtract),
                      reads=["triAd", "ident"], writes=["stri"])
                offs = sd("offs", [128, NE])
                cx.op("dve", lambda g: g.memset(offs[:], 0.0), writes=["offs"])
                bst = sd("bst", [128, NBLK])
                cx.dma("sp", "c3", bst[:], bstart_rep, writes=["bst"])

                mixb = [sd("mixb%d" % i, [128, 8, 128], BF16) for i in range(2)]
                xb = [sd("xb%d" % i, [128, D]) for i in range(2)]
                hb = [sd("hb%d" % i, [128, D]) for i in range(2)]
                hn_all = sd("hn_all", [128, NOWN, D], BF16)
                lg_all = sd("lg_all", [128, NOWN, NE])
                pos_all = sd("pos_all", [128, NOWN, NE])
                mx_all = sd("mx_all", [128, NOWN, 8])
                hnT = [sd("hnT%d" % i, [128, 8, 128], BF16) for i in range(2)]
                junkd = sd("junkD", [128, D], BF16)
                sd1 = [sd("sd1_%d" % i, [128, 4]) for i in range(2)]
                negm = [sd("negm%d" % i, [128, 1]) for i in range(2)]
                ex = [sd("ex%d" % i, [128, 4]) for i in range(2)]
                esum = [sd("esum%d" % i, [128, 2]) for i in range(2)]
                Mf = [sd("Mf%d" % i, [128, NE]) for i in range(2)]
                Mb = [sd("Mb%d" % i, [128, NE], BF16) for i in range(2)]
                sbase = sd("sbase", [128, NE])
                oh = sd("oh", [128, NE]); slotf = sd("slotf", [128, 4])
                hps = [psd("hps%d" % i, [128, 512]) for i in range(2)]
                tpd = [psd("tpD%d" % i, [128, 1024], BF16) for i in range(2)]
                lps = [psd("lpsD%d" % i, [128, 512]) for i in range(2)]
                cps = [psd("cpsD%d" % i, [128, 512]) for i in range(2)]
                ablk = [sd("ablk%d" % i, [128, 512], BF16) for i in range(2)]

                def load_d(j):
                    sl = j % 2
                    cx.dma("sp", "mixb%d" % sl, mixb[sl][:, 4:8, :], mixT_d[4:8, :, j * 128:(j + 1) * 128].rearrange("c p t -> p c t"),
                           writes=["mixs%d" % sl])
                    cx.dma("sp", "ablk%d" % sl, ablk[sl][:], a_d[j * 128:(j + 1) * 128, :], writes=["ablk%d" % sl])
                    cx.dma("sp", "xb%d" % sl, xb[sl][:], x_perm[j * 128:(j + 1) * 128, :], writes=["xb%d" % sl])

                def stage1(j):
                    sl = j % 2
                    T = tpd[sl]; TK = "tpd%d" % sl
                    for c4 in range(4):
                        cx.op("pe", lambda g: g.transpose(out=T[:, c4 * 128:(c4 + 1) * 128], in_=ablk[sl][:, c4 * 128:(c4 + 1) * 128],
                                                          identity=ident[:]), reads=["ablk%d" % sl, "ident"], writes=[TK])
                    cx.op("act", lambda g: g.activation(out=mixb[sl][:, 0:4, :].rearrange("p k t -> p (k t)"), in_=T[:, 0:512],
                                                        func=AF.Copy), reads=[TK], writes=["mixa%d" % sl])
                    for half in range(2):
                        for kt in range(8):
                            cx.op("pe", lambda g: g.matmul(hps[half][:], mixb[sl][:, kt, :],
                                                           wout[:, kt, half * 512:(half + 1) * 512],
                                                           start=(kt == 0), stop=(kt == 7)),
                                  reads=["mixa%d" % sl, "mixs%d" % sl, "wout"], writes=["hps%d" % half])
                        cx.op("dve", lambda g: g.tensor_tensor(out=hb[sl][:, half * 512:(half + 1) * 512], in0=hps[half][:],
                                                               in1=xb[sl][:, half * 512:(half + 1) * 512], op=ALU.add),
                              reads=["hps%d" % half, "xb%d" % sl], writes=["hb%d_%d" % (sl, half)])
                    HB = ["hb%d_0" % sl, "hb%d_1" % sl]
                    cx.dma("sp", "hst%d" % sl, h_d[j * 128:(j + 1) * 128, :], hb[sl][:], reads=HB)
                    S1 = sd1[sl]; SK = "sd1_%d" % sl
                    cx.op("act", lambda g: g.activation(out=junkd[:], in_=hb[sl][:], func=AF.Square, accum_out=S1[:, 0:1]),
                          reads=HB, writes=["junkD", SK])
                    cx.op("dve", lambda g: g.tensor_scalar(out=S1[:, 1:2], in0=S1[:, 0:1], scalar1=1.0 / D, scalar2=EPS,
                                                           op0=ALU.mult, op1=ALU.add), reads=[SK], writes=[SK + "b"])
                    cx.op("act", lambda g: g.activation(out=S1[:, 2:3], in_=S1[:, 1:2], func=AF.Ln), reads=[SK + "b"], writes=[SK + "c"])
                    cx.op("act", lambda g: g.activation(out=S1[:, 2:3], in_=S1[:, 2:3], func=AF.Exp, scale=-0.5),
                          reads=[SK + "c"], writes=[SK + "c"])
                    cx.op("dve", lambda g: g.scalar_tensor_tensor(out=hn_all[:, j, :], in0=hb[sl][:], scalar=S1[:, 2:3], in1=lnf[:],
                                                                  op0=ALU.mult, op1=ALU.mult),
                          reads=HB + [SK + "c", "lnf"], writes=["hn_%d" % j])

                def stage2(j):
                    sl = j % 2
                    HN = "hn_%d" % j
                    T = tpd[sl]; TK = "tpd%d" % sl
                    for kt in range(8):
                        cx.op("pe", lambda g: g.transpose(out=T[:, kt * 128:(kt + 1) * 128], in_=hn_all[:, j, kt * 128:(kt + 1) * 128],
                                                          identity=ident[:]), reads=[HN, "ident"], writes=[TK])
                    cx.op("act", lambda g: g.activation(out=hnT[sl][:].rearrange("p k t -> p (k t)"), in_=T[:], func=AF.Copy),
                          reads=[TK], writes=["hnT%d" % sl])
                    L = lps[sl]; LK = "lps%d" % sl
                    for kt in range(8):
                        cx.op("pe", lambda g: g.matmul(L[:, 0:NE], hnT[sl][:, kt, :], wrt[:, kt, :], start=(kt == 0), stop=(kt == 7)),
                              reads=["hnT%d" % sl, "wrt"], writes=[LK])
                    LG = "lg_%d" % j
                    cx.op("dve", lambda g: g.tensor_tensor(out=lg_all[:, j, :], in0=L[:, 0:NE], in1=brt[:], op=ALU.add),
                          reads=[LK, "brt"], writes=[LG])
                    MX = "mx_%d" % j
                    cx.op("dve", lambda g: g.max(out=mx_all[:, j, :], in_=lg_all[:, j, :]), reads=[LG], writes=[MX])
                    NK = "negm%d" % sl
                    cx.op("dve", lambda g: g.tensor_scalar(out=negm[sl][:], in0=mx_all[:, j, 0:1], scalar1=-1.0, scalar2=None, op0=ALU.mult),
                          reads=[MX], writes=[NK])
                    cx.op("act", lambda g: g.activation(out=ex[sl][:], in_=mx_all[:, j, 0:4], func=AF.Exp, bias=negm[sl][:],
                                                        accum_out=esum[sl][:, 0:1]),
                          reads=[MX, NK], writes=["ex%d" % sl, "esum%d" % sl])
                    cx.op("dve", lambda g: g.reciprocal(out=esum[sl][:, 1:2], in_=esum[sl][:, 0:1]), reads=["esum%d" % sl],
                          writes=["esumb%d" % sl])
                    cx.op("dve", lambda g: g.tensor_scalar(out=gates_all[:, j, :], in0=ex[sl][:], scalar1=esum[sl][:, 1:2], scalar2=None,
                                                           op0=ALU.mult), reads=["ex%d" % sl, "esumb%d" % sl], writes=["gates%d" % j])
                    cx.op("dve", lambda g: g.tensor_scalar(out=Mf[sl][:], in0=lg_all[:, j, :], scalar1=mx_all[:, j, 3:4], scalar2=None,
                                                           op0=ALU.is_ge), reads=[LG, MX], writes=["Mf%d" % sl])
                    cx.op("dve", lambda g: g.tensor_copy(out=Mb[sl][:], in_=Mf[sl][:]), reads=["Mf%d" % sl], writes=["Mb%d" % sl])
                    Cp = cps[sl]; CK = "cps%d" % sl
                    cx.op("pe", lambda g: g.matmul(Cp[:, 0:NE], stri[:], Mb[sl][:], start=True, stop=True),
                          reads=["stri", "Mb%d" % sl], writes=[CK])
                    cx.op("pe", lambda g: g.matmul(Cp[:, 64:64 + NE], onesb[:], Mb[sl][:], start=True, stop=True),
                          reads=["onesb", "Mb%d" % sl], writes=[CK])
                    cx.op("dve", lambda g: g.tensor_tensor(out=pos_all[:, j, :], in0=Cp[:, 0:NE], in1=offs[:], op=ALU.add),
                          reads=[CK, "offs"], writes=["pos_%d" % j])
                    cx.op("dve", lambda g: g.tensor_tensor(out=offs[:], in0=Cp[:, 64:64 + NE], in1=offs[:], op=ALU.add),
                          reads=[CK, "offs"], writes=["offs"])

                load_d(0)
                if NOWN > 1:
                    load_d(1)
                stage1(0)
                for j in range(NOWN):
                    if j + 1 < NOWN:
                        stage1(j + 1)
                    if j + 2 < NOWN:
                        load_d(j + 2)
                    stage2(j)
                cx.dma("sp", "c1", cnt_out, offs[:], reads=["offs"])
                padded = sd("padded", [128, NE]); pend = sd("pend", [128, NE]); pstart = sd("pstart", [128, NE])
                nbi = sd("nbi", [128, NE], I32)
                cx.op("dve", lambda g: g.tensor_scalar(out=padded[:], in0=offs[:], scalar1=511.0, scalar2=1.0 / 512,
                                                       op0=ALU.add, op1=ALU.mult), reads=["offs"], writes=["padded"])
                cx.op("dve", lambda g: g.tensor_scalar(out=nbi[:], in0=padded[:], scalar1=-511.0 / 1024, scalar2=None, op0=ALU.add),
                      reads=["padded"], writes=["nbi"])
                cx.op("dve", lambda g: g.tensor_copy(out=padded[:], in_=nbi[:]), reads=["nbi"], writes=["padded"])
                cx.op("dve", lambda g: g.tensor_scalar(out=padded[:], in0=padded[:], scalar1=512.0, scalar2=None, op0=ALU.mult),
                      reads=["padded"], writes=["padded"])
                cx.op("dve", lambda g: g.tensor_copy(out=pend[:, 0:1], in_=padded[:, 0:1]), reads=["padded"], writes=["pend"])
                for e_ in range(1, NE):
                    cx.op("dve", lambda g: g.tensor_tensor(out=pend[:, e_:e_ + 1], in0=pend[:, e_ - 1:e_], in1=padded[:, e_:e_ + 1],
                                                           op=ALU.add), reads=["padded", "pend"], writes=["pend"])
                cx.op("dve", lambda g: g.tensor_tensor(out=pstart[:], in0=pend[:], in1=padded[:], op=ALU.subtract),
                      reads=["pend", "padded"], writes=["pstart"])
                bacc = sd("bacc", [128, NBLK])
                cx.op("dve", lambda g: g.memset(bacc[:], 0.0), writes=["bacc"])
                for e_ in range(NE):
                    cx.op("dve", lambda g: g.scalar_tensor_tensor(out=bacc[:], in0=bst[:], scalar=pend[:, e_:e_ + 1], in1=bacc[:],
                                                                  op0=ALU.is_ge, op1=ALU.add),
                          reads=["bst", "pend", "bacc"], writes=["bacc"])
                cx.op("dve", lambda g: g.tensor_scalar(out=bacc[:], in0=bacc[:], scalar1=float(NE - 1), scalar2=None, op0=ALU.min),
                      reads=["bacc"], writes=["bacc"])
                widf = sd("widf", [128, NBLK, 8]); bidf = sd("bidf", [128, 2, NBLK]); kp = sd("kp", [128, 8])
                for kt in range(8):
                    cx.op("dve", lambda g: g.tensor_scalar(out=kp[:, kt:kt + 1], in0=iotas[:, 0:1], scalar1=float(kt * 128), scalar2=None,
                                                           op0=ALU.add), reads=["iotas", "kp"], writes=["kp"])
                for kt in range(8):
                    cx.op("dve", lambda g: g.tensor_scalar(out=widf[:, :, kt], in0=bacc[:], scalar1=float(D), scalar2=kp[:, kt:kt + 1],
                                                           op0=ALU.mult, op1=ALU.add), reads=["bacc", "kp", "widf"], writes=["widf"])
                cx.op("dve", lambda g: g.tensor_copy(out=widx[:], in_=widf[:]), reads=["widf"], writes=["widx"])
                cx.op("dve", lambda g: g.tensor_scalar(out=bidf[:, 0, :], in0=bacc[:], scalar1=128.0, scalar2=iotas[:, 0:1],
                                                       op0=ALU.mult, op1=ALU.add), reads=["bacc", "iotas"], writes=["bidf0"])
                cx.op("dve", lambda g: g.tensor_copy(out=bidx[:], in_=bidf[:, 0, :]), reads=["bidf0"], writes=["bidx"])
                cx.op("dve", lambda g: g.tensor_scalar(out=kp[:, 0:1], in0=iotas[:, 0:1], scalar1=float(NE), scalar2=None, op0=ALU.mult),
                      reads=["iotas", "kp", "widf"], writes=["kp"])
                cx.op("dve", lambda g: g.tensor_scalar(out=bidf[:, 1, :], in0=bacc[:], scalar1=kp[:, 0:1], scalar2=None, op0=ALU.add),
                      reads=["bacc", "kp"], writes=["bidf1"])
                cx.op("dve", lambda g: g.tensor_copy(out=gidx[:], in_=bidf[:, 1, :]), reads=["bidf1"], writes=["gidx"])
                for j in range(NOWN):
                    sl = j % 2
                    cx.op("dve", lambda g: g.tensor_tensor(out=sbase[:], in0=pos_all[:, j, :], in1=pstart[:], op=ALU.add),
                          reads=["pos_%d" % j, "pstart"], writes=["sbase"])
                    for k in range(4):
                        cx.op("dve", lambda g: g.tensor_scalar(out=oh[:], in0=lg_all[:, j, :], scalar1=mx_all[:, j, k:k + 1], scalar2=None,
                                                               op0=ALU.is_equal), reads=["lg_%d" % j, "mx_%d" % j], writes=["oh"])
                        cx.op("dve", lambda g: g.tensor_tensor(out=oh[:], in0=oh[:], in1=sbase[:], op=ALU.mult),
                              reads=["oh", "sbase"], writes=["oh"])
                        cx.op("dve", lambda g: g.tensor_reduce(out=slotf[:, k:k + 1], in_=oh[:], axis=AX.X, op=ALU.add),
                              reads=["oh"], writes=["slotf"])
                    cx.op("dve", lambda g: g.tensor_copy(out=slots_all[:, j, :], in_=slotf[:]), reads=["slotf"], writes=["slots%d" % j])
                    with cx.dma_group("scat%d" % sl):
                        for k in range(4):
                            cx.dma("pool", "scat%d" % sl, xpad_d, hn_all[:, j, :], reads=["hn_%d" % j, "slots%d" % j, "xpad"],
                                   indirect=dict(out_offset=bass.IndirectOffsetOnAxis(ap=slots_all[:, j, k:k + 1], axis=0),
                                                 in_offset=None, bounds_check=bc_reg, oob_is_err=False))
                cx.barrier()

        if "E" in phases:
            pe_ = contextlib.ExitStack()
            with pe_:
                def se(name, shape, dt=F32):
                    return pe_.enter_context(nc.sbuf_tensor("sb_" + name, list(shape), dt))

                def pse(name, shape, dt=F32):
                    return pe_.enter_context(nc.psum_tensor("ps_" + name, list(shape), dt))

                NSL = 512
                wgu = [se("wgu%d" % i, [128, 8, 2 * D], BF16) for i in range(2)]
                wdn = [se("wdn%d" % i, [128, 8, D], BF16) for i in range(2)]
                bdn = [se("bdn%d" % i, [128, D]) for i in range(2)]
                bgu = [se("bgu%d" % i, [128, 16]) for i in range(2)]
                xs = [se("xs%d" % i, [128, 4, D], BF16) for i in range(2)]
                xT = se("xT", [128, 8, NSL], BF16)
                gt = [se("gt%d" % i, [128, NSL]) for i in range(2)]
                sgm = [se("sgm%d" % i, [128, NSL]) for i in range(2)]
                ut = [se("ut%d" % i, [128, NSL]) for i in range(2)]
                p1 = [se("p1_%d" % i, [128, NSL]) for i in range(2)]
                actT = se("actT", [128, 8, NSL], BF16)
                ysb = [se("ysb%d" % i, [128, D]) for i in range(2)]
                tpe = [pse("tpE%d" % i, [128, 1024], BF16) for i in range(2)]
                gps = [pse("gps%d" % i, [128, 512]) for i in range(2)]
                ups = [pse("ups%d" % i, [128, 512]) for i in range(2)]
                yps = [pse("yps%d" % i, [128, 512]) for i in range(2)]

                wgu_rows = wgu_in.rearrange("e d n -> (e d) n")
                wdn_rows = wdn_in.rearrange("e d n -> (e d) n")
                bdn_rows = bdn_rep.rearrange("e p n -> (e p) n")
                bgu_rows = bgu_col.rearrange("p e n -> (p e) n")

                def load_w(bi):
                    sl = bi % 2
                    with cx.dma_group("wgu%d" % sl):
                        for kt in range(8):
                            cx.dma("pool", "wgu%d" % sl, wgu[sl][:, kt, :], wgu_rows, reads=["widx"], writes=["wgu%d" % sl],
                                   indirect=dict(out_offset=None, in_offset=bass.IndirectOffsetOnAxis(ap=widx[:, bi, kt:kt + 1], axis=0),
                                                 bounds_check=bcw_reg, oob_is_err=False))
                    with cx.dma_group("wdn%d" % sl):
                        for kt in range(8):
                            cx.dma("pool", "wdn%d" % sl, wdn[sl][:, kt, :], wdn_rows, reads=["widx"], writes=["wdn%d" % sl],
                                   indirect=dict(out_offset=None, in_offset=bass.IndirectOffsetOnAxis(ap=widx[:, bi, kt:kt + 1], axis=0),
                                                 bounds_check=bcw_reg, oob_is_err=False))
                    cx.dma("pool", "bdn%d" % sl, bdn[sl][:, :], bdn_rows, reads=["bidx"], writes=["bdn%d" % sl],
                           indirect=dict(out_offset=None, in_offset=bass.IndirectOffsetOnAxis(ap=bidx[:, bi:bi + 1], axis=0),
                                         bounds_check=bcb_reg, oob_is_err=False))
                    cx.dma("pool", "bgu%d" % sl, bgu[sl][:, :], bgu_rows, reads=["gidx"], writes=["bgu%d" % sl],
                           indirect=dict(out_offset=None, in_offset=bass.IndirectOffsetOnAxis(ap=gidx[:, bi:bi + 1], axis=0),
                                         bounds_check=bcb_reg, oob_is_err=False))

                ny = 0
                ntp = 0
                nft = 0

                def load_x(bi):
                    sl = bi % 2
                    r0 = bi * NSL
                    cx.dma("sp", "xs%d" % sl, xs[sl][:], xpad_d[r0:r0 + NSL, :].rearrange("(n p) d -> p n d", p=128),
                           writes=["xs%d" % sl])

                def emit_trans(bi):
                    nonlocal ntp
                    xsl = bi % 2
                    for kt in range(8):
                        tb = ntp % 2
                        ntp += 1
                        for n in range(4):
                            cx.op("pe", lambda g: g.transpose(out=tpe[tb][:, n * 128:(n + 1) * 128],
                                                              in_=xs[xsl][:, n, kt * 128:(kt + 1) * 128], identity=ident[:]),
                                  reads=["xs%d" % xsl, "ident"], writes=["tpe%d" % tb])
                        if kt % 2 == 0:
                            cx.op("act", lambda g: g.activation(out=xT[:, kt, :], in_=tpe[tb][:, 0:NSL], func=AF.Copy),
                                  reads=["tpe%d" % tb], writes=["xT%d" % kt])
                        else:
                            cx.op("dve", lambda g: g.tensor_copy(out=xT[:, kt, :], in_=tpe[tb][:, 0:NSL]),
                                  reads=["tpe%d" % tb], writes=["xT%d" % kt])

                load_w(0)
                load_x(0)
                for bi in range(NBLK):
                    wsl = bi % 2
                    xsl = bi % 2
                    if bi + 1 < NBLK:
                        load_w(bi + 1)
                        load_x(bi + 1)
                    r0 = bi * NSL
                    nsl = NSL
                    if bi == 0:
                        emit_trans(0)
                    XT = ["xT%d" % kt for kt in range(8)]
                    for ft in range(8):
                        b = nft % 2
                        nft += 1
                        for kt in range(8):
                            cx.op("pe", lambda g: g.matmul(gps[b][:], wgu[wsl][:, kt, ft * 128:(ft + 1) * 128],
                                                           xT[:, kt, :], start=(kt == 0), stop=(kt == 7)),
                                  reads=XT + ["wgu%d" % wsl], writes=["gps%d" % b])
                        for kt in range(8):
                            cx.op("pe", lambda g: g.matmul(ups[b][:], wgu[wsl][:, kt, D + ft * 128:D + (ft + 1) * 128],
                                                           xT[:, kt, :], start=(kt == 0), stop=(kt == 7)),
                                  reads=XT + ["wgu%d" % wsl], writes=["ups%d" % b])
                        cx.op("dve", lambda g: g.tensor_scalar(out=gt[b][:], in0=gps[b][:], scalar1=bgu[wsl][:, ft:ft + 1],
                                                               scalar2=7.0, op0=ALU.add, op1=ALU.min),
                              reads=["gps%d" % b, "bgu%d" % wsl], writes=["gt%d" % b])
                        cx.op("act", lambda g: g.activation(out=sgm[b][:], in_=gt[b][:], func=AF.Sigmoid, scale=1.702),
                              reads=["gt%d" % b], writes=["sgm%d" % b])
                        cx.op("dve", lambda g: g.tensor_scalar(out=ut[b][:], in0=ups[b][:], scalar1=bgu[wsl][:, 8 + ft:9 + ft],
                                                               scalar2=7.0, op0=ALU.add, op1=ALU.min),
                              reads=["ups%d" % b, "bgu%d" % wsl], writes=["ut%d" % b])
                        cx.op("dve", lambda g: g.tensor_scalar(out=ut[b][:], in0=ut[b][:], scalar1=-7.0, scalar2=1.0,
                                                               op0=ALU.max, op1=ALU.add),
                              reads=["ut%d" % b], writes=["ut%d" % b])
                        cx.op("dve", lambda g: g.tensor_tensor(out=p1[b][:], in0=gt[b][:], in1=sgm[b][:], op=ALU.mult),
                              reads=["gt%d" % b, "sgm%d" % b], writes=["p1_%d" % b])
                        cx.op("dve", lambda g: g.tensor_tensor(out=actT[:, ft, :], in0=p1[b][:], in1=ut[b][:], op=ALU.mult),
                              reads=["p1_%d" % b, "ut%d" % b], writes=["actT%d" % ft])
                    AT = ["actT%d" % ft for ft in range(8)]
                    if bi + 1 < NBLK:
                        emit_trans(bi + 1)
                    for n in range(4):
                        yb = ny % 2
                        ny += 1
                        for half in range(2):
                            for ft in range(8):
                                cx.op("pe", lambda g: g.matmul(yps[half][:], actT[:, ft, n * 128:(n + 1) * 128],
                                                               wdn[wsl][:, ft, half * 512:(half + 1) * 512],
                                                               start=(ft == 0), stop=(ft == 7)),
                                      reads=AT + ["wdn%d" % wsl], writes=["yps%d" % half])
                            cx.op("dve", lambda g: g.tensor_tensor(out=ysb[yb][:, half * 512:(half + 1) * 512], in0=yps[half][:],
                                                                   in1=bdn[wsl][:, half * 512:(half + 1) * 512], op=ALU.add),
                                  reads=["yps%d" % half, "bdn%d" % wsl], writes=["ysb%d_%d" % (yb, half)])
                        cx.dma("sp", "yst%d" % yb, y_d[r0 + n * 128:r0 + (n + 1) * 128, :], ysb[yb][:],
                               reads=["ysb%d_0" % yb, "ysb%d_1" % yb])
                cx.barrier()

        if "F" in phases:
            pf = contextlib.ExitStack()
            with pf:
                def sf(name, shape, dt=F32):
                    return pf.enter_context(nc.sbuf_tensor("sb_" + name, list(shape), dt))

                fin = sf("fin", [128, D])
                cx.dma("sp", "c1", fin[:], fin_rep, writes=["fin"])
                hf = [sf("hf%d" % i, [128, D]) for i in range(2)]
                gk = [[sf("gk%d_%d" % (i, k), [128, D]) for k in range(4)] for i in range(2)]
                acc = sf("accF", [128, D])
                junkf = sf("junkF", [128, D], BF16)
                sf1 = sf("sf1", [128, 4])
                ot = [sf("ot%d" % i, [128, D]) for i in range(2)]

                def load_f(j):
                    sl = j % 2
                    cx.dma("sp", "hf%d" % sl, hf[sl][:], h_d[j * 128:(j + 1) * 128, :], writes=["hf%d" % sl])
                    for k in range(4):
                        cx.op("pool", lambda g: g.memset(gk[sl][k][:], 0.0), writes=["gk%d_%d" % (sl, k)])
                    with cx.dma_group("gath%d" % sl):
                        for k in range(4):
                            cx.dma("pool", "gath%d" % sl, gk[sl][k][:, :], y_d, reads=["slots%d" % j, "gk%d_%d" % (sl, k)],
                                   writes=["gk%d_%d" % (sl, k)],
                                   indirect=dict(out_offset=None,
                                                 in_offset=bass.IndirectOffsetOnAxis(ap=slots_all[:, j, k:k + 1], axis=0),
                                                 bounds_check=bc_reg, oob_is_err=False))

                load_f(0)
                for j in range(NOWN):
                    sl = j % 2
                    if j + 1 < NOWN:
                        load_f(j + 1)
                    prev = hf[sl]
                    pk = "hf%d" % sl
                    for k in range(4):
                        cx.op("dve", lambda g: g.scalar_tensor_tensor(out=acc[:], in0=gk[sl][k][:], scalar=gates_all[:, j, k:k + 1],
                                                                      in1=prev[:], op0=ALU.mult, op1=ALU.add),
                              reads=["gk%d_%d" % (sl, k), "gates%d" % j, pk], writes=["acc"])
                        prev = acc
                        pk = "acc"
                    cx.op("act", lambda g: g.activation(out=junkf[:], in_=acc[:], func=AF.Square, accum_out=sf1[:, 0:1]),
                          reads=["acc"], writes=["junkF", "sf1"])
                    cx.op("dve", lambda g: g.tensor_scalar(out=sf1[:, 1:2], in0=sf1[:, 0:1], scalar1=1.0 / D, scalar2=EPS,
                                                           op0=ALU.mult, op1=ALU.add), reads=["sf1"], writes=["sf1b"])
                    cx.op("act", lambda g: g.activation(out=sf1[:, 2:3], in_=sf1[:, 1:2], func=AF.Ln), reads=["sf1b"], writes=["sf1c"])
                    cx.op("act", lambda g: g.activation(out=sf1[:, 2:3], in_=sf1[:, 2:3], func=AF.Exp, scale=-0.5),
                          reads=["sf1c"], writes=["sf1c"])
                    cx.op("dve", lambda g: g.scalar_tensor_tensor(out=ot[sl][:], in0=acc[:], scalar=sf1[:, 2:3], in1=fin[:],
                                                                  op0=ALU.mult, op1=ALU.mult),
                          reads=["acc", "sf1c", "fin"], writes=["ot%d" % sl])
                    cx.dma("sp", "ost%d" % sl, out[j * 128:(j + 1) * 128, :], ot[sl][:], reads=["ot%d" % sl])
                cx.barrier()

        cx.barrier()
    return nc


def _rep(v, n=128):
    v = np.asarray(v, np.float32).reshape(1, -1)
    return np.ascontiguousarray(np.broadcast_to(v, (n, v.shape[1])))


def prep(inputs, S, C):
    f32 = np.float32
    NB = S // 128
    x = np.asarray(inputs["x"], f32)
    positions = np.asarray(inputs["positions"], np.int32)
    w_in = np.asarray(inputs["w_in"], f32)[0]
    pidx = np.arange(128)
    hh_, mm_, rr_ = pidx // 32, (pidx // 16) % 2, pidx % 16
    def qk_cols(base):
        cols = [base + hh_ * 128 + mm_ * 64 + 16 * b_ + rr_ for b_ in range(4)]
        return np.concatenate(cols)
    partner = np.where(rr_ < 8, rr_ + 8, rr_ - 8)
    def perm_cols(base):
        return base + hh_ * 128 + mm_ * 64 + partner
    w_in_ext = np.ascontiguousarray(np.concatenate(
        [w_in[:, qk_cols(0)], w_in[:, qk_cols(512)], w_in[:, 1024:2048], w_in[:, perm_cols(0)], w_in[:, perm_cols(512)]], axis=1))
    inv_freq = (500000.0 ** (-np.arange(0, 16, 2, dtype=np.float32) / 16)).astype(f32)
    ropef = np.zeros((128, 2), f32)
    ropef[:, 0] = inv_freq[rr_ % 8]
    ropef[:, 1] = np.where(rr_ < 8, -1.0, 1.0)
    ident = np.eye(128, dtype=f32)
    kk = np.arange(128)
    triA = (kk[:, None] <= kk[None, :]).astype(f32)
    maskown = np.where(kk[:, None] <= kk[None, :], 0.0, NEG).astype(f32)
    iotas = np.zeros((128, 129), f32)
    iotas[:, 0] = kk
    iotas[:, 1:] = kk[None, :]
    shared = {
        "w_in_ext": w_in_ext, "ropef": ropef, "ident": ident, "triA": triA, "maskown": maskown,
        "lnmix_col": np.ascontiguousarray(np.asarray(inputs["ln_mix_g"], f32)[0].reshape(8, 128).T),
        "lamvec": np.ascontiguousarray(np.broadcast_to(
            np.stack([np.asarray(inputs[k], f32)[0] for k in ("lam_q1", "lam_k1", "lam_q2", "lam_k2")])[None],
            (128, 4, 64))),
        "diffg_rep": _rep(inputs["diff_norm_g"][0]),
        "iotas": iotas,
    }
    lre = np.asarray(inputs["ssm_lam_re"], f32)[0]
    lim = np.asarray(inputs["ssm_lam_im"], f32)[0]
    ldt = np.asarray(inputs["ssm_log_dt"], f32)[0]
    shared["lamre_row"] = _rep(lre.reshape(-1))
    shared["lamim_row"] = _rep(lim.reshape(-1))
    shared["logdt_row"] = _rep(np.repeat(ldt, 64))

    def col(a):
        return np.ascontiguousarray(a.reshape(16, 2, 64).transpose(1, 2, 0).reshape(128, 16))
    shared["lamre_col"] = col(lre)
    shared["lamim_col"] = col(lim)
    shared["logdt_col"] = col(np.repeat(ldt[:, None], 64, axis=1))
    bre = np.asarray(inputs["ssm_b_re"], f32)[0]
    bim = np.asarray(inputs["ssm_b_im"], f32)[0]
    cre = np.asarray(inputs["ssm_c_re"], f32)[0]
    cim = np.asarray(inputs["ssm_c_im"], f32)[0]

    def bblk(b):
        o = np.zeros((128, 4, 512), f32)
        for g in range(32):
            ct, gl = divmod(g, 8)
            o[gl * 16:(gl + 1) * 16, ct, gl * 64:(gl + 1) * 64] = b[g].T
        return o

    def cblk(c):
        o = np.zeros((128, 16, 128), f32)
        for g in range(32):
            pair, g2 = divmod(g, 2)
            gl = g % 8
            o[g2 * 64:(g2 + 1) * 64, pair, gl * 16:(gl + 1) * 16] = c[g].T
        return o
    shared["bre_blk"] = bblk(bre)
    shared["bim_blk"] = bblk(bim)
    shared["cre_blk"] = cblk(cre)
    shared["cim_blk"] = cblk(cim)
    ssmcols = np.zeros((128, 3, 4), f32)
    for i, k in enumerate(("ssm_d", "ssm_b_glu", "ssm_norm_g")):
        ssmcols[:, i, :] = np.asarray(inputs[k], f32)[0].reshape(4, 128).T
    shared["ssmcols"] = ssmcols
    shared["w_glu"] = np.ascontiguousarray(np.asarray(inputs["ssm_w_glu"], f32)[0])
    shared["w_out"] = np.ascontiguousarray(np.asarray(inputs["w_out"], f32)[0])
    shared["lnffn_rep"] = _rep(inputs["ln_ffn_g"][0])
    shared["w_router"] = np.ascontiguousarray(np.asarray(inputs["w_router"], f32)[0])
    shared["brt_rep"] = _rep(inputs["b_router"][0])
    nblk = (S // 2 * TOPK + NE * 511) // 512
    shared["bstart_rep"] = _rep(np.arange(nblk, dtype=f32) * 512.0)
    wgu = np.asarray(inputs["w_gate_up"], f32)[0]
    shared["w_gu"] = np.ascontiguousarray(np.concatenate([wgu[:, :, 0::2], wgu[:, :, 1::2]], axis=2))
    bgu = np.asarray(inputs["b_gate_up"], f32)[0]
    bgu2 = np.concatenate([bgu[:, 0::2], bgu[:, 1::2]], axis=1)
    shared["bgu_col"] = np.ascontiguousarray(bgu2.reshape(NE, 16, 128).transpose(2, 0, 1))
    shared["w_dn"] = np.ascontiguousarray(np.asarray(inputs["w_down"], f32)[0])
    bdn = np.asarray(inputs["b_down"], f32)[0]
    shared["bdn_rep"] = np.ascontiguousarray(np.broadcast_to(bdn[:, None, :], (NE, 128, D)))
    shared["fin_rep"] = _rep(inputs["final_norm_g"])

    in_maps = []
    for c in range(8):
        b, h = divmod(c, 2)
        own = np.arange(h, NB, 2)
        oth = np.arange(1 - h, NB, 2)
        order = np.concatenate([own, oth])
        tok = (order[:, None] * 128 + np.arange(128)[None, :]).reshape(-1)
        m = dict(shared)
        m["x_perm"] = np.ascontiguousarray(x[b][tok])
        m["pos_rep"] = np.ascontiguousarray(np.broadcast_to(positions[b][tok][None, :], (128, S)))
        parc = np.zeros((128, 4), f32)
        parc[:, 0] = 128.0 * h
        parc[:, 1] = 128.0 * (1 - h)
        m["par"] = parc
        m["triB"] = np.full((128, 128), float(h), f32)
        m["maskoth"] = np.full((128, 128), 0.0 if h == 1 else NEG, f32)
        in_maps.append(m)
    return in_maps


_CACHE = {}


def kernel(**inputs):
    S = int(np.asarray(inputs["x"]).shape[1])
    C = 0
    key = (S, C)
    if key not in _CACHE:
        _CACHE[key] = build(S, C)
    nc = _CACHE[key]
    in_maps = prep(inputs, S, C)
    res = run_bass_kernel_spmd(nc, in_maps, core_ids=list(range(8)))
    _CACHE["last_cnt"] = [np.asarray(r["cnt"])[0] for r in res.results]
    B = int(np.asarray(inputs["x"]).shape[0])
    NB = S // 128
    outp = np.zeros((B, S, D), np.float32)
    for c in range(8):
        b, h = divmod(c, 2)
        own = np.arange(h, NB, 2)
        tok = (own[:, None] * 128 + np.arange(128)[None, :]).reshape(-1)
        outp[b][tok] = res.results[c]["out"]
    return outp
```

```python
import contextlib
import math
import numpy as np
import concourse.bass as bass
import concourse.mybir as mybir
from concourse.bass_utils import run_bass_kernel_spmd

F32 = mybir.dt.float32
BF16 = mybir.dt.bfloat16
I32 = mybir.dt.int32
AF = mybir.ActivationFunctionType
ALU = mybir.AluOpType
AX = mybir.AxisListType

D = 1024
NE = 32
TOPK = 4
EPS = 1e-6
NEG = -30000.0
TWO_PI = 2.0 * math.pi
PI_LO = 3.1415925
LAMBDA_INIT = 0.8 - 0.6 * math.exp(0.0)


class Ctx:
    def __init__(self, nc, es):
        self.nc = nc
        self.es = es
        self.eng = {"pe": nc.tensor, "act": nc.scalar, "dve": nc.vector,
                    "pool": nc.gpsimd, "sp": nc.sync}
        self.sems = {}
        self.cnt = {}
        for k in ("pe", "act", "dve", "pool"):
            self.sems["s_" + k] = es.enter_context(nc.semaphore("s_" + k))
            self.cnt["s_" + k] = 0
        self.seen = {k: {} for k in self.eng}
        self.w = {}
        self.r = {}
        self.group = None

    def _wait(self, e, tk, kind):
        if tk is None:
            return
        name, val = tk
        own = (name == "s_" + e)
        if own and e == "pe":
            return
        if self.seen[e].get(name, 0) >= val:
            return
        self.eng[e].wait_ge(self.sems[name], val)
        self.seen[e][name] = val

    def _deps(self, e, reads, writes):
        for b in reads:
            self._wait(e, self.w.get(b), "raw")
        for b in writes:
            self._wait(e, self.w.get(b), "waw")
            for name, val in self.r.get(b, {}).items():
                self._wait(e, (name, val), "war")

    def _commit(self, tk, reads, writes):
        for b in writes:
            self.w[b] = tk
            self.r[b] = {}
        for b in reads:
            d = self.r.setdefault(b, {})
            d[tk[0]] = max(d.get(tk[0], 0), tk[1])

    def op(self, e, fn, reads=(), writes=()):
        self._deps(e, reads, writes)
        inst = fn(self.eng[e])
        name = "s_" + e
        self.cnt[name] += 1
        inst.then_inc(self.sems[name], 1)
        self._commit((name, self.cnt[name]), reads, writes)

    def dma(self, e, key, out, in_, reads=(), writes=(), indirect=None):
        self._deps(e, reads, writes)
        if key not in self.sems:
            self.sems[key] = self.es.enter_context(self.nc.semaphore("d_" + key))
            self.cnt[key] = 0
        if self.cnt[key] > 0 and (self.group is None or not self.group[1]):
            self._wait(e, (key, self.cnt[key]), "raw")
        if indirect is None:
            inst = self.eng[e].dma_start(out=out, in_=in_)
        else:
            inst = self.eng[e].indirect_dma_start(out=out, in_=in_, **indirect)
        self.cnt[key] += 16
        inst.then_inc(self.sems[key], 16)
        if self.group is not None:
            assert self.group[0] == key
            self.group[1].append((reads, writes))
        else:
            self._commit((key, self.cnt[key]), reads, writes)

    @contextlib.contextmanager
    def dma_group(self, key):
        self.group = (key, [])
        yield
        pend = self.group[1]
        self.group = None
        for reads, writes in pend:
            self._commit((key, self.cnt[key]), reads, writes)

    def barrier(self):
        for e in self.eng:
            for name, val in self.cnt.items():
                if val > 0 and self.seen[e].get(name, 0) < val:
                    self.eng[e].wait_ge(self.sems[name], val)
                    self.seen[e][name] = val
        self.w = {}
        self.r = {}


def build(S, C, dbg=False, phases="ABCDEF"):
    NB = S // 128
    NOWN = NB // 2
    SO = S // 2
    GT = min(512, SO)
    NBG = GT // 128
    NG = S // GT
    NGO = SO // GT
    NBLK = (SO * TOPK + NE * 511) // 512
    NSLOT = NBLK * 512

    nc = bass.Bass("TRN2", target_bir_lowering=False)
    es = contextlib.ExitStack()

    def din(name, shape, dt=F32):
        return nc.dram_tensor(name, list(shape), dt, kind="ExternalInput").ap()

    def dscr(name, shape, dt):
        kind = "ExternalOutput" if dbg else "Internal"
        return nc.dram_tensor(name, list(shape), dt, kind=kind).ap()

    x_perm = din("x_perm", [S, D])
    pos_rep = din("pos_rep", [128, S], I32)
    par = din("par", [128, 4])
    triB_in = din("triB", [128, 128])
    maskoth_in = din("maskoth", [128, 128])
    w_in_ext = din("w_in_ext", [D, 2304])
    ropef = din("ropef", [128, 2])
    ident_in = din("ident", [128, 128])
    triA_in = din("triA", [128, 128])
    maskown_in = din("maskown", [128, 128])
    lnmix_col = din("lnmix_col", [128, 8])
    lamvec = din("lamvec", [128, 4, 64])
    diffg_rep = din("diffg_rep", [128, 128])
    lamre_row = din("lamre_row", [128, 2048])
    lamim_row = din("lamim_row", [128, 2048])
    logdt_row = din("logdt_row", [128, 2048])
    lamre_col = din("lamre_col", [128, 16])
    lamim_col = din("lamim_col", [128, 16])
    logdt_col = din("logdt_col", [128, 16])
    bre_blk = din("bre_blk", [128, 4, 512])
    bim_blk = din("bim_blk", [128, 4, 512])
    cre_blk = din("cre_blk", [128, 16, 128])
    cim_blk = din("cim_blk", [128, 16, 128])
    ssmcols = din("ssmcols", [128, 3, 4])
    wglu_in = din("w_glu", [512, 512])
    iota_in = din("iotas", [128, 129])
    wout_in = din("w_out", [D, D])
    lnffn_rep = din("lnffn_rep", [128, D])
    wrt_in = din("w_router", [D, NE])
    brt_rep = din("brt_rep", [128, NE])
    bstart_rep = din("bstart_rep", [128, NBLK])
    bgu_col = din("bgu_col", [128, NE, 16])
    if "E" in phases:
        wgu_in = din("w_gu", [NE, D, 2 * D])
        wdn_in = din("w_dn", [NE, D, D])
        bdn_rep = din("bdn_rep", [NE, 128, D])
    fin_rep = din("fin_rep", [128, D])

    out = nc.dram_tensor("out", [SO, D], F32, kind="ExternalOutput").ap()
    cnt_out = nc.dram_tensor("cnt", [128, NE], F32, kind="ExternalOutput").ap()

    kT_d = dscr("kT_d", [4, 128, S], BF16)
    qT_d = dscr("qT_d", [4, 128, SO], BF16)
    uT_d = dscr("uT_d", [4, 128, S], BF16)
    v_d = dscr("v_d", [S, 512], BF16)
    mixT_d = dscr("mixT_d", [8, 128, SO], BF16)
    a_d = dscr("a_d", [SO, 512], BF16)
    h_d = dscr("h_d", [SO, D], F32)
    xpad_d = dscr("xpad_d", [NSLOT, D], BF16)
    y_d = dscr("y_d", [NSLOT, D], F32)

    with es:
        cx = Ctx(nc, es)

        def sb(name, shape, dt=F32):
            return es.enter_context(nc.sbuf_tensor("sb_" + name, list(shape), dt))

        ident = sb("ident", [128, 128], BF16)
        cx.dma("pool", "c0", ident[:], ident_in, writes=["ident"])
        onesb = sb("onesb", [128, 128], BF16)
        cx.op("dve", lambda e: e.memset(onesb[:], 1.0), writes=["onesb"])
        parsb = sb("parsb", [128, 4])
        cx.dma("sp", "c1", parsb[:], par, writes=["par"])
        iotas = sb("iotas", [128, 129])
        cx.dma("sp", "c2", iotas[:], iota_in, writes=["iotas"])
        slots_all = sb("slots_all", [128, NOWN, 4], I32)
        gates_all = sb("gates_all", [128, NOWN, 4])
        widx = sb("widx", [128, NBLK, 8], I32)
        bidx = sb("bidx", [128, NBLK], I32)
        gidx = sb("gidx", [128, NBLK], I32)
        bcw_reg = nc.gpsimd.to_reg(NE * D - 1)
        bcb_reg = nc.gpsimd.to_reg(NE * 128 - 1)
        bc_reg = nc.gpsimd.to_reg(NSLOT - 1)

        if "A" in phases:
            pa = contextlib.ExitStack()
            with pa:
                def sa(name, shape, dt=F32):
                    return pa.enter_context(nc.sbuf_tensor("sb_" + name, list(shape), dt))

                def psa(name, shape, dt=F32):
                    return pa.enter_context(nc.psum_tensor("ps_" + name, list(shape), dt))

                win = sa("win", [128, 8, 2304], BF16)
                with cx.dma_group("win"):
                    for kt in range(8):
                        cx.dma("pool", "win", win[:, kt, :],
                               w_in_ext[kt * 128:(kt + 1) * 128, :], writes=["win%d" % kt])
                WIN = ["win%d" % kt for kt in range(8)]
                gcol = sa("gcol", [128, 8])
                cx.dma("sp", "c3", gcol[:], lnmix_col, writes=["gcol"])
                rf = sa("rf", [128, 2])
                cx.dma("sp", "c4", rf[:], ropef, writes=["rf"])

                xt = [sa("xt%d" % i, [128, NBG, D]) for i in range(2)]
                xn = [sa("xn%d" % i, [128, NBG, D], BF16) for i in range(2)]
                xnT = [sa("xnT%d" % i, [128, 8, GT], BF16) for i in range(2)]
                junk = sa("junkA", [128, D], BF16)
                ss = sa("ssA", [128, 2, NBG])
                rstd = sa("rstdA", [128, 2, NBG])
                posi = [sa("posi%d" % i, [128, GT], I32) for i in range(2)]
                ang = sa("ang", [128, GT])
                angk = sa("angk", [128, GT], I32)
                angf = sa("angf", [128, GT])
                angc = sa("angc", [128, GT])
                ctab = [sa("ctab%d" % i, [128, GT]) for i in range(2)]
                stab = [sa("stab%d" % i, [128, GT]) for i in range(2)]
                t1 = [sa("t1_%d" % i, [128, GT]) for i in range(2)]
                t2 = [sa("t2_%d" % i, [128, GT]) for i in range(2)]
                kst = [sa("kst%d" % i, [128, 4, GT], BF16) for i in range(2)]
                qst = [sa("qst%d" % i, [128, 4, GT], BF16) for i in range(2)]
                ust = [sa("ust%d" % i, [128, 4, GT], BF16) for i in range(2)]
                vst = [sa("vst%d" % i, [128, NBG, 512], BF16) for i in range(2)]
                tp = [psa("tpA%d" % i, [128, 1024], BF16) for i in range(2)]
                pm = [psa("pmA%d" % i, [128, 512]) for i in range(2)]
                pp = [psa("ppA%d" % i, [128, 512]) for i in range(2)]
                pv = [psa("pvA%d" % i, [128, 512]) for i in range(2)]
                ctr = {"tp": 0, "pm": 0, "pv": 0, "t": 0, "pb": 0}
                pbank = [pm[0], pm[1], pp[0], pp[1], pv[0], pv[1]]

                def next_bank():
                    i = ctr["pb"] % 6
                    ctr["pb"] += 1
                    return pbank[i], "pb%d" % i

                def load_group(gi):
                    s = gi % 2
                    cx.dma("sp", "xt%d" % s, xt[s][:],
                           x_perm[gi * GT:(gi + 1) * GT, :].rearrange("(n p) d -> p n d", p=128),
                           writes=["xt%d" % s])
                    cx.dma("sp", "posi%d" % s, posi[s][:], pos_rep[:, gi * GT:(gi + 1) * GT],
                           writes=["posi%d" % s])

                def stage1(gi):
                    s = gi % 2
                    cx.op("dve", lambda e: e.tensor_copy(out=ang[:], in_=posi[s][:]),
                          reads=["posi%d" % s], writes=["ang"])
                    cx.op("dve", lambda e: e.tensor_scalar(out=ang[:], in0=ang[:], scalar1=rf[:, 0:1],
                                                            scalar2=None, op0=ALU.mult),
                          reads=["ang", "rf"], writes=["ang"])

                    def sin_of(dst, key, shift):
                        cx.op("dve", lambda e: e.tensor_scalar(out=angc[:], in0=ang[:], scalar1=float(shift),
                                                                scalar2=None, op0=ALU.add),
                              reads=["ang"], writes=["angc"])
                        cx.op("dve", lambda e: e.tensor_scalar(out=angk[:], in0=angc[:],
                                                                scalar1=float(1.0 / TWO_PI), scalar2=None,
                                                                op0=ALU.mult),
                              reads=["angc"], writes=["angk"])
                        cx.op("dve", lambda e: e.tensor_copy(out=angf[:], in_=angk[:]),
                              reads=["angk"], writes=["angf"])
                        cx.op("dve", lambda e: e.tensor_scalar(out=angf[:], in0=angf[:], scalar1=float(-TWO_PI),
                                                                scalar2=None, op0=ALU.mult),
                              reads=["angf"], writes=["angf"])
                        cx.op("dve", lambda e: e.tensor_tensor(out=angc[:], in0=angc[:], in1=angf[:], op=ALU.add),
                              reads=["angf", "angc"], writes=["angc"])
                        cx.op("dve", lambda e: e.tensor_scalar(out=angc[:], in0=angc[:], scalar1=PI_LO, scalar2=-PI_LO,
                                                                op0=ALU.min, op1=ALU.max),
                              reads=["angc"], writes=["angc"])
                        cx.op("act", lambda e: e.activation(out=dst[:], in_=angc[:], func=AF.Sin),
                              reads=["angc"], writes=[key])

                    sin_of(stab[s], "stab%d" % s, 0.0)
                    cx.op("dve", lambda e: e.tensor_scalar(out=stab[s][:], in0=stab[s][:], scalar1=rf[:, 1:2],
                                                            scalar2=None, op0=ALU.mult),
                          reads=["stab%d" % s, "rf"], writes=["stab%d" % s])
                    sin_of(ctab[s], "ctab%d" % s, math.pi / 2)

                    for n in range(NBG):
                        cx.op("act", lambda e: e.activation(out=junk[:], in_=xt[s][:, n, :], func=AF.Square,
                                                            accum_out=ss[:, s, n:n + 1]),
                              reads=["xt%d" % s], writes=["junkA", "ss%d_%d" % (s, n)])
                    SSK = ["ss%d_%d" % (s, n) for n in range(NBG)]
                    cx.op("dve", lambda e: e.tensor_scalar(out=rstd[:, s, :], in0=ss[:, s, :], scalar1=1.0 / D,
                                                           scalar2=EPS, op0=ALU.mult, op1=ALU.add),
                          reads=SSK, writes=["rstd%d" % s])
                    cx.op("act", lambda e: e.activation(out=rstd[:, s, :], in_=rstd[:, s, :], func=AF.Sqrt),
                          reads=["rstd%d" % s], writes=["rstd%d" % s])
                    cx.op("dve", lambda e: e.reciprocal(out=rstd[:, s, :], in_=rstd[:, s, :]),
                          reads=["rstd%d" % s], writes=["rstd%d" % s])
                    for n in range(NBG):
                        cx.op("dve", lambda e: e.tensor_scalar(out=xn[s][:, n, :], in0=xt[s][:, n, :],
                                                               scalar1=rstd[:, s, n:n + 1], scalar2=None,
                                                               op0=ALU.mult),
                              reads=["xt%d" % s, "rstd%d" % s], writes=["xn%d_%d" % (s, n)])

                def stage1c(gi):
                    s = gi % 2
                    XNK = ["xn%d_%d" % (s, n) for n in range(NBG)]
                    for kt in range(8):
                        p = ctr["tp"] % 2
                        ctr["tp"] += 1
                        for n in range(NBG):
                            cx.op("pe", lambda e: e.transpose(out=tp[p][:, n * 128:(n + 1) * 128],
                                                              in_=xn[s][:, n, kt * 128:(kt + 1) * 128],
                                                              identity=ident[:]),
                                  reads=XNK + ["ident"], writes=["tp%d" % p])
                        if kt % 2 == 0:
                            cx.op("act", lambda e: e.activation(out=xnT[s][:, kt, :], in_=tp[p][:, 0:GT],
                                                                func=AF.Copy, scale=gcol[:, kt:kt + 1]),
                                  reads=["tp%d" % p, "gcol"], writes=["xnT%d_%d" % (s, kt)])
                        else:
                            cx.op("dve", lambda e: e.tensor_scalar(out=xnT[s][:, kt, :], in0=tp[p][:, 0:GT],
                                                                   scalar1=gcol[:, kt:kt + 1], scalar2=None,
                                                                   op0=ALU.mult),
                                  reads=["tp%d" % p, "gcol"], writes=["xnT%d_%d" % (s, kt)])
                    XTK = ["xnT%d_%d" % (s, kt) for kt in range(8)]


                def stage2(gi, part):
                    s = gi % 2
                    own = gi < NGO
                    XTK = ["xnT%d_%d" % (s, kt) for kt in range(8)]
                    def proj(ps, pskey, col0):
                        for kt in range(8):
                            cx.op("pe", lambda e: e.matmul(ps[:, 0:GT], win[:, kt, col0:col0 + 128],
                                                           xnT[s][:, kt, :], start=(kt == 0), stop=(kt == 7)),
                                  reads=XTK + WIN, writes=[pskey])

                    def rope_tile(col_main, col_perm, dst, dkey):
                        bm, km = next_bank()
                        bp, kp_ = next_bank()
                        proj(bm, km, col_main)
                        proj(bp, kp_, col_perm)
                        j = ctr["t"] % 2
                        ctr["t"] += 1
                        cx.op("dve", lambda e: e.tensor_tensor(out=t1[j][:], in0=bm[:, 0:GT], in1=ctab[s][:],
                                                               op=ALU.mult),
                              reads=[km, "ctab%d" % s], writes=["t1_%d" % j])
                        cx.op("dve", lambda e: e.tensor_tensor(out=t2[j][:], in0=bp[:, 0:GT], in1=stab[s][:],
                                                               op=ALU.mult),
                              reads=[kp_, "stab%d" % s], writes=["t2_%d" % j])
                        cx.op("dve", lambda e: e.tensor_tensor(out=dst, in0=t1[j][:], in1=t2[j][:], op=ALU.add),
                              reads=["t1_%d" % j, "t2_%d" % j], writes=[dkey])

                    def plain_tile(col0, dst, dkey):
                        bm, km = next_bank()
                        proj(bm, km, col0)
                        cx.op("act", lambda e: e.activation(out=dst, in_=bm[:, 0:GT], func=AF.Copy),
                              reads=[km], writes=[dkey])

                    def qk_tiles(col0, colperm, stage, skey, dst_d):
                        rope_tile(col0, colperm, stage[:, 0, :], skey)
                        for b_ in range(1, 4):
                            plain_tile(col0 + b_ * 128, stage[:, b_, :], skey)
                        with cx.dma_group(skey):
                            for hh in range(4):
                                for m in range(2):
                                    p0 = hh * 32 + m * 16
                                    cx.dma("sp", skey, dst_d[hh, m * 64:(m + 1) * 64, gi * GT:(gi + 1) * GT].rearrange(
                                        "(b r) t -> r b t", r=16), stage[p0:p0 + 16, :, :], reads=[skey])

                    if part == 0:
                        qk_tiles(512, 2176, kst[s], "kst%d" % s, kT_d)
                        if own:
                            qk_tiles(0, 2048, qst[s], "qst%d" % s, qT_d)
                        return
                    for ct in range(4):
                        bm, km = next_bank()
                        proj(bm, km, 1536 + ct * 128)
                        cx.op("act", lambda e: e.activation(out=ust[s][:, ct, :], in_=bm[:, 0:GT], func=AF.Copy),
                              reads=[km], writes=["ust%d" % s])
                    cx.dma("sp", "ust%d" % s, uT_d[:, :, gi * GT:(gi + 1) * GT].rearrange("c p t -> p c t"),
                           ust[s][:], reads=["ust%d" % s])
                    for n in range(NBG):
                        bm, km = next_bank()
                        for kt in range(8):
                            cx.op("pe", lambda e: e.matmul(bm[:], xnT[s][:, kt, n * 128:(n + 1) * 128],
                                                           win[:, kt, 1024:1536], start=(kt == 0), stop=(kt == 7)),
                                  reads=XTK + WIN, writes=[km])
                        cx.op("act", lambda e: e.activation(out=vst[s][:, n, :], in_=bm[:], func=AF.Copy),
                              reads=[km], writes=["vst%d" % s])
                    cx.dma("sp", "vst%d" % s, v_d[gi * GT:(gi + 1) * GT, :].rearrange("(n p) c -> p n c", p=128),
                           vst[s][:], reads=["vst%d" % s])
                load_group(0)
                if NG > 1:
                    load_group(1)
                stage1(0)
                stage1c(0)
                if NG > 1:
                    stage1(1)
                if NG > 2:
                    load_group(2)
                for gi in range(NG):
                    stage2(gi, 0)
                    if gi + 1 < NG:
                        stage1c(gi + 1)
                    if gi + 2 < NG:
                        stage1(gi + 2)
                    if gi + 3 < NG:
                        load_group(gi + 3)
                    stage2(gi, 1)
                cx.barrier()

        if "B" in phases:
            pb = contextlib.ExitStack()
            with pb:
                def sbb(name, shape, dt=F32):
                    return pb.enter_context(nc.sbuf_tensor("sb_" + name, list(shape), dt))

                def psb(name, shape, dt=F32):
                    return pb.enter_context(nc.psum_tensor("ps_" + name, list(shape), dt))

                mown = sbb("mown", [128, 128], BF16)
                moth = sbb("moth", [128, 128], BF16)
                cx.dma("pool", "c5", mown[:], maskown_in, writes=["mown"])
                cx.dma("pool", "c6", moth[:], maskoth_in, writes=["moth"])
                lv = sbb("lv", [128, 4, 64])
                cx.dma("sp", "c1", lv[:], lamvec, writes=["lv"])
                dg = sbb("dg", [128, 128])
                cx.dma("sp", "c2", dg[:], diffg_rep, writes=["dg"])
                cx.op("dve", lambda e: e.tensor_scalar(out=dg[:], in0=dg[:], scalar1=float(1.0 - LAMBDA_INIT),
                                                       scalar2=None, op0=ALU.mult), reads=["dg"], writes=["dg"])
                lprod = sbb("lprod", [128, 2, 64])
                lsum = sbb("lsum", [128, 2])
                neglam = sbb("neglam", [128, 1])
                cx.op("dve", lambda e: e.tensor_tensor(out=lprod[:, 0, :], in0=lv[:, 0, :], in1=lv[:, 1, :], op=ALU.mult),
                      reads=["lv"], writes=["lprod"])
                cx.op("dve", lambda e: e.tensor_tensor(out=lprod[:, 1, :], in0=lv[:, 2, :], in1=lv[:, 3, :], op=ALU.mult),
                      reads=["lv", "lprod"], writes=["lprod"])
                cx.op("dve", lambda e: e.tensor_reduce(out=lsum[:], in_=lprod[:], axis=AX.X, op=ALU.add),
                      reads=["lprod"], writes=["lsum"])
                cx.op("act", lambda e: e.activation(out=lsum[:], in_=lsum[:], func=AF.Exp),
                      reads=["lsum"], writes=["lsum"])
                cx.op("dve", lambda e: e.scalar_tensor_tensor(out=neglam[:], in0=lsum[:, 1:2], scalar=float(-LAMBDA_INIT),
                                                              in1=lsum[:, 0:1], op0=ALU.add, op1=ALU.subtract),
                      reads=["lsum"], writes=["neglam"])

                kTs = [sbb("kTs%d" % i, [128, S], BF16) for i in range(2)]
                qq = [sbb("qq%d" % i, [128, NOWN, 2, 128], BF16) for i in range(2)]
                vs = [sbb("vs%d" % i, [128, NB, 129], BF16) for i in range(2)]
                ast = [sbb("ast%d" % i, [128, NOWN, 128], BF16) for i in range(2)]
                for i in range(2):
                    cx.op("dve", lambda e: e.memset(vs[i][:, :, 128:129], 1.0), writes=["vs%d" % i])
                    cx.op("dve", lambda e: e.memset(qq[i][:], 0.0), writes=["qq%d" % i])
                pt = [sbb("pt%d" % i, [128, 4, 2, 128], BF16) for i in range(2)]
                mk2 = {}
                for nm, src in (("mown", mown), ("moth", moth)):
                    t_ = sbb(nm + "2", [128, 2, 128], BF16)
                    for m in range(2):
                        cx.op("dve", lambda e: e.tensor_copy(out=t_[:, m, :], in_=src[:]), reads=[nm], writes=[nm + "2"])
                    mk2[nm] = t_
                rr = [sbb("rrB%d" % i, [128, 4]) for i in range(2)]
                a1 = sbb("a1B", [128, 128])
                a2 = sbb("a2B", [128, 128])
                asq = sbb("asqB", [128, 128])
                ssb = [sbb("ssB%d" % i, [128, 2]) for i in range(2)]
                scq = [psb("scB%d" % i, [128, 4, 2, 128]) for i in range(2)]
                o1 = [psb("o1B%d" % i, [128, 512]) for i in range(2)]
                o2 = [psb("o2B%d" % i, [128, 512]) for i in range(2)]

                def load_head(hh):
                    s = hh % 2
                    cx.dma("sp", "kTs%d" % s, kTs[s][:], kT_d[hh], writes=["kTs%d" % s])
                    with cx.dma_group("qq%d" % s):
                        for m in range(2):
                            cx.dma("sp", "qq%d" % s, qq[s][m * 64:(m + 1) * 64, :, m, :],
                                   qT_d[hh, m * 64:(m + 1) * 64, :].rearrange("p (n t) -> p n t", t=128),
                                   reads=["qq%d" % s], writes=["qq%d" % s])
                    with cx.dma_group("vs%d" % s):
                        for n0 in range(0, NB, 16):
                            n1 = min(NB, n0 + 16)
                            cx.dma("sp", "vs%d" % s, vs[s][:, n0:n1, 0:128],
                                   v_d[n0 * 128:n1 * 128, hh * 128:(hh + 1) * 128].rearrange("(n p) c -> p n c", p=128),
                                   reads=["vs%d" % s], writes=["vs%d" % s])

                groups = []
                nqb = 0
                for hh in range(4):
                    for j in range(NOWN):
                        tl = []
                        for i in range(j + 1):
                            tl.append((i, "mown" if i == j else None))
                            tl.append((NOWN + i, "moth" if i == j else None))
                        ngrp = (len(tl) + 3) // 4
                        for gi_ in range(ngrp):
                            groups.append(dict(hh=hh, j=j, tiles=tl[gi_ * 4:(gi_ + 1) * 4], first=(gi_ == 0),
                                               last=(gi_ == ngrp - 1), ob=nqb % 2, b=len(groups) % 2))
                        nqb += 1

                def emit_scores(G):
                    s = G["hh"] % 2
                    b = G["b"]
                    j = G["j"]
                    for t, (tile, mk) in enumerate(G["tiles"]):
                        ks = slice(tile * 128, (tile + 1) * 128)
                        cx.op("pe", lambda e: e.matmul(scq[b][:, t, :, :], kTs[s][:, ks], qq[s][:, j, :, :],
                                                       start=True, stop=(mk is None)),
                              reads=["kTs%d" % s, "qq%d" % s], writes=["sc%d" % b])
                        if mk is not None:
                            cx.op("pe", lambda e: e.matmul(scq[b][:, t, :, :], ident[:], mk2[mk][:], start=False, stop=True),
                                  reads=["ident", "mown2", "moth2"], writes=["sc%d" % b])
                    n = len(G["tiles"])
                    cx.op("act", lambda e: e.activation(out=pt[b][:, 0:n, :, :], in_=scq[b][:, 0:n, :, :], func=AF.Exp, scale=0.125),
                          reads=["sc%d" % b], writes=["pt%d" % b])

                def emit_pv(G):
                    s = G["hh"] % 2
                    b = G["b"]
                    ob = G["ob"]
                    n = len(G["tiles"])
                    for t, (tile, mk) in enumerate(G["tiles"]):
                        first = (G["first"] and t == 0)
                        last = (G["last"] and t == n - 1)
                        cx.op("pe", lambda e: e.matmul(o1[ob][:, 0:129], pt[b][:, t, 0, :], vs[s][:, tile, :], start=first, stop=last),
                              reads=["pt%d" % b, "vs%d" % s], writes=["o1_%d" % ob])
                        cx.op("pe", lambda e: e.matmul(o2[ob][:, 0:129], pt[b][:, t, 1, :], vs[s][:, tile, :], start=first, stop=last),
                              reads=["pt%d" % b, "vs%d" % s], writes=["o2_%d" % ob])

                def emit_epilogue(G):
                    s = G["hh"] % 2
                    ob = G["ob"]
                    j = G["j"]
                    O1 = o1[ob]; O2 = o2[ob]; R = rr[ob]; SS = ssb[ob]
                    RK = "rr%d" % ob; SK = "ssb%d" % ob
                    cx.op("dve", lambda e: e.reciprocal(out=R[:, 0:1], in_=O1[:, 128:129]), reads=["o1_%d" % ob], writes=[RK])
                    cx.op("dve", lambda e: e.reciprocal(out=R[:, 1:2], in_=O2[:, 128:129]), reads=["o2_%d" % ob, RK], writes=[RK])
                    cx.op("dve", lambda e: e.tensor_tensor(out=R[:, 2:3], in0=R[:, 1:2], in1=neglam[:], op=ALU.mult),
                          reads=[RK, "neglam"], writes=[RK])
                    cx.op("dve", lambda e: e.tensor_scalar(out=a1[:], in0=O1[:, 0:128], scalar1=R[:, 0:1], scalar2=None, op0=ALU.mult),
                          reads=["o1_%d" % ob, RK], writes=["a1"])
                    cx.op("dve", lambda e: e.scalar_tensor_tensor(out=a2[:], in0=O2[:, 0:128], scalar=R[:, 2:3], in1=a1[:],
                                                                  op0=ALU.mult, op1=ALU.add),
                          reads=["o2_%d" % ob, RK, "a1"], writes=["a2"])
                    cx.op("dve", lambda e: e.tensor_tensor(out=asq[:], in0=a2[:], in1=a2[:], op=ALU.mult), reads=["a2"], writes=["asq"])
                    cx.op("dve", lambda e: e.tensor_reduce(out=SS[:, 0:1], in_=asq[:], axis=AX.X, op=ALU.add), reads=["asq"], writes=[SK])
                    cx.op("dve", lambda e: e.tensor_scalar(out=SS[:, 0:1], in0=SS[:, 0:1], scalar1=1.0 / 128, scalar2=EPS,
                                                           op0=ALU.mult, op1=ALU.add), reads=[SK], writes=[SK])
                    cx.op("act", lambda e: e.activation(out=SS[:, 1:2], in_=SS[:, 0:1], func=AF.Ln), reads=[SK], writes=[SK + "b"])
                    cx.op("act", lambda e: e.activation(out=SS[:, 1:2], in_=SS[:, 1:2], func=AF.Exp, scale=-0.5),
                          reads=[SK + "b"], writes=[SK + "b"])
                    cx.op("dve", lambda e: e.scalar_tensor_tensor(out=ast[s][:, j, :], in0=a2[:], scalar=SS[:, 1:2], in1=dg[:],
                                                                  op0=ALU.mult, op1=ALU.mult),
                          reads=["a2", SK + "b", "dg"], writes=["ast%d" % s])
                    if j == NOWN - 1:
                        hh = G["hh"]
                        cx.dma("sp", "ast%d" % s, a_d[:, hh * 128:(hh + 1) * 128].rearrange("(n p) c -> p n c", p=128),
                               ast[s][:], reads=["ast%d" % s])

                load_head(0)
                zt = sbb("zt", [128, 8, D], BF16)
                cx.op("dve", lambda g: g.memset(zt[:], 0.0), writes=["zt"])
                nblk = NSLOT // 128
                with cx.dma_group("zero"):
                    for b0 in range(0, nblk, 8):
                        nb8 = min(8, nblk - b0)
                        cx.dma("sp", "zero", xpad_d[b0 * 128:(b0 + nb8) * 128, :].rearrange("(n p) d -> p n d", p=128),
                               zt[:, 0:nb8, :], reads=["zt"], writes=["xpad"])
                emit_scores(groups[0])
                for gi_, G in enumerate(groups):
                    if G["first"] and G["j"] == 0 and G["hh"] + 1 < 4:
                        load_head(G["hh"] + 1)
                    if gi_ + 1 < len(groups):
                        emit_scores(groups[gi_ + 1])
                    emit_pv(G)
                    if G["last"]:
                        emit_epilogue(G)
                cx.barrier()

        if "C" in phases:
            pc = contextlib.ExitStack()
            with pc:
                def sc_(name, shape, dt=F32):
                    return pc.enter_context(nc.sbuf_tensor("sb_" + name, list(shape), dt))

                def psc(name, shape, dt=F32):
                    return pc.enter_context(nc.psum_tensor("ps_" + name, list(shape), dt))

                def TT(e, o, a, b, op, R, W):
                    cx.op(e, lambda g: g.tensor_tensor(out=o, in0=a, in1=b, op=op), reads=R, writes=W)

                def TS(e, o, a, s1, op0, R, W, s2=None, op1=None):
                    if op1 is None:
                        cx.op(e, lambda g: g.tensor_scalar(out=o, in0=a, scalar1=s1, scalar2=None, op0=op0),
                              reads=R, writes=W)
                    else:
                        cx.op(e, lambda g: g.tensor_scalar(out=o, in0=a, scalar1=s1, scalar2=s2, op0=op0, op1=op1),
                              reads=R, writes=W)

                def ACT(o, a, func, R, W, **kw):
                    cx.op("act", lambda g: g.activation(out=o, in_=a, func=func, **kw), reads=R, writes=W)

                PAre = sc_("PAre", [128, 2048]); PAim = sc_("PAim", [128, 2048])
                PBre = sc_("PBre", [128, 2048]); PBim = sc_("PBim", [128, 2048])
                Qre = sc_("Qre", [128, 16, 128]); Qim = sc_("Qim", [128, 16, 128])
                A256 = sc_("A256", [128, 2, 16])
                Bblk = sc_("Bblk", [128, 4, 1024], BF16)
                Cre = sc_("Cre", [128, 16, 128], BF16)
                Cimn = sc_("Cimn", [128, 16, 128], BF16)
                wglu = sc_("wglu", [128, 4, 512], BF16)
                scol = sc_("scol", [128, 3, 4])
                triA = sc_("triA", [128, 128], BF16)
                triB = sc_("triB", [128, 128], BF16)
                cx.dma("pool", "c5", triA[:], triA_in, writes=["triA"])
                cx.dma("pool", "c6", triB[:], triB_in, writes=["triB"])
                triAn = sc_("triAn", [128, 128], BF16); triBn = sc_("triBn", [128, 128], BF16)
                onesn = sc_("onesn", [128, 1], BF16); Cren = sc_("Cren", [128, 16, 128], BF16)
                cx.op("dve", lambda g: g.tensor_scalar(out=triAn[:], in0=triA[:], scalar1=-1.0, scalar2=None, op0=ALU.mult),
                      reads=["triA"], writes=["triAn"])
                cx.op("dve", lambda g: g.tensor_scalar(out=triBn[:], in0=triB[:], scalar1=-1.0, scalar2=None, op0=ALU.mult),
                      reads=["triB"], writes=["triBn"])
                cx.op("dve", lambda g: g.memset(onesn[:], -1.0), writes=["onesn"])
                cx.dma("pool", "c7", Cre[:], cre_blk, writes=["Cre"])
                cx.dma("pool", "c8", Cimn[:], cim_blk, writes=["Cimn"])
                cx.op("dve", lambda g: g.tensor_scalar(out=Cimn[:], in0=Cimn[:], scalar1=-1.0, scalar2=None, op0=ALU.mult),
                      reads=["Cimn"], writes=["Cimn"])
                cx.op("dve", lambda g: g.tensor_scalar(out=Cren[:], in0=Cre[:], scalar1=-1.0, scalar2=None, op0=ALU.mult),
                      reads=["Cre"], writes=["Cren"])
                cx.dma("pool", "c9", wglu[:], wglu_in.rearrange("(k p) n -> p k n", p=128), writes=["wglu"])
                cx.dma("sp", "c3", scol[:], ssmcols, writes=["scol"])

                pcs = contextlib.ExitStack()
                with pcs:
                    def st_(name, shape, dt=F32):
                        return pcs.enter_context(nc.sbuf_tensor("sb_" + name, list(shape), dt))
                    ar = st_("ar", [128, 2048]); ai = st_("ai", [128, 2048]); dtr = st_("dtr", [128, 2048])
                    lre = st_("lre", [128, 2048]); lim = st_("lim", [128, 2048])
                    mag = st_("mag", [128, 2048]); ph = st_("ph", [128, 2048]); ph2 = st_("ph2", [128, 2048])
                    phk = st_("phk", [128, 2048], I32); phf = st_("phf", [128, 2048])
                    sn = st_("sn", [128, 2048]); cs = st_("cs", [128, 2048])
                    cx.dma("sp", "c1", lre[:], lamre_row, writes=["lre"])
                    cx.dma("sp", "c2", lim[:], lamim_row, writes=["lim"])
                    cx.dma("sp", "c4", dtr[:], logdt_row, writes=["dtr"])
                    ACT(dtr[:], dtr[:], AF.Exp, ["dtr"], ["dtr"])
                    TT("dve", ar[:], lre[:], dtr[:], ALU.mult, ["lre", "dtr"], ["ar"])
                    TT("dve", ai[:], lim[:], dtr[:], ALU.mult, ["lim", "dtr"], ["ai"])
                    posc = st_("posc", [128, 4])
                    TS("dve", posc[:, 0:1], iotas[:, 0:1], parsb[:, 0:1], ALU.add, ["iotas", "par"], ["posc"], -1.0, ALU.mult)
                    TS("dve", posc[:, 1:2], iotas[:, 0:1], parsb[:, 1:2], ALU.add, ["iotas", "par", "posc"], ["posc"], -1.0, ALU.mult)
                    cx.op("dve", lambda g: g.memset(posc[:, 2:3], 1.0), reads=["posc"], writes=["posc"])

                    def sincos(n):
                        for shift, dst, key in ((0.0, sn, "sn"), (math.pi / 2, cs, "cs")):
                            TS("dve", ph2[:, :n], ph[:, :n], float(shift), ALU.add, ["ph"], ["ph2"])
                            TS("dve", phk[:, :n], ph2[:, :n], float(1.0 / TWO_PI), ALU.mult, ["ph2"], ["phk"])
                            cx.op("dve", lambda g: g.tensor_copy(out=phf[:, :n], in_=phk[:, :n]), reads=["phk"], writes=["phf"])
                            cx.op("dve", lambda g: g.scalar_tensor_tensor(out=ph2[:, :n], in0=phf[:, :n], scalar=float(-TWO_PI),
                                                                          in1=ph2[:, :n], op0=ALU.mult, op1=ALU.add),
                                  reads=["phf", "ph2"], writes=["ph2"])
                            TS("dve", ph2[:, :n], ph2[:, :n], PI_LO, ALU.min, ["ph2"], ["ph2"], -PI_LO, ALU.max)
                            ACT(dst[:, :n], ph2[:, :n], AF.Sin, ["ph2"], [key])

                    def row_table(dre, dim, kre, kim, pcol):
                        ACT(mag[:], ar[:], AF.Exp, ["ar", "posc"], ["mag"], scale=posc[:, pcol:pcol + 1])
                        TS("dve", ph[:], ai[:], posc[:, pcol:pcol + 1], ALU.mult, ["ai", "posc"], ["ph"])
                        sincos(2048)
                        TT("dve", dre, mag[:], cs[:], ALU.mult, ["mag", "cs"], [kre])
                        TT("dve", dim, mag[:], sn[:], ALU.mult, ["mag", "sn"], [kim])

                    row_table(PAre[:], PAim[:], "PAre", "PAim", 0)
                    row_table(PBre[:], PBim[:], "PBre", "PBim", 1)
                    are = st_("are", [128, 2048]); aim = st_("aim", [128, 2048])
                    row_table(are[:], aim[:], "are", "aim", 2)
                    TS("dve", are[:], are[:], -1.0, ALU.add, ["are"], ["are"])
                    den = mag
                    TT("dve", den[:], lre[:], lre[:], ALU.mult, ["lre", "mag"], ["mag"])
                    TT("dve", ph[:], lim[:], lim[:], ALU.mult, ["lim", "ph"], ["ph"])
                    TT("dve", den[:], den[:], ph[:], ALU.add, ["mag", "ph"], ["mag"])
                    cx.op("dve", lambda g: g.reciprocal(out=den[:], in_=den[:]), reads=["mag"], writes=["mag"])
                    fre = sn; fim = cs
                    TT("dve", ph[:], are[:], lre[:], ALU.mult, ["are", "lre"], ["ph"])
                    TT("dve", ph2[:], aim[:], lim[:], ALU.mult, ["aim", "lim"], ["ph2"])
                    TT("dve", ph[:], ph[:], ph2[:], ALU.add, ["ph", "ph2"], ["ph"])
                    TT("dve", fre[:], ph[:], den[:], ALU.mult, ["ph", "mag", "sn"], ["sn"])
                    TT("dve", ph[:], aim[:], lre[:], ALU.mult, ["aim", "lre"], ["ph"])
                    TT("dve", ph2[:], are[:], lim[:], ALU.mult, ["are", "lim"], ["ph2"])
                    TT("dve", ph[:], ph[:], ph2[:], ALU.subtract, ["ph", "ph2"], ["ph"])
                    TT("dve", fim[:], ph[:], den[:], ALU.mult, ["ph", "mag", "cs"], ["cs"])
                    braw = st_("braw", [128, 2, 4, 512])
                    cx.dma("sp", "c1", braw[:, 0], bre_blk, writes=["braw0"])
                    cx.dma("sp", "c2", braw[:, 1], bim_blk, writes=["braw1"])
                    for ct in range(4):
                        fs = slice(ct * 512, (ct + 1) * 512)
                        TT("dve", ph[:, 0:512], braw[:, 0, ct, :], fre[:, fs], ALU.mult, ["braw0", "sn"], ["ph"])
                        TT("dve", ph2[:, 0:512], braw[:, 1, ct, :], fim[:, fs], ALU.mult, ["braw1", "cs"], ["ph2"])
                        TT("dve", Bblk[:, ct, 0:512], ph[:, 0:512], ph2[:, 0:512], ALU.subtract, ["ph", "ph2"], ["Bblk"])
                        TT("dve", ph[:, 0:512], braw[:, 0, ct, :], fim[:, fs], ALU.mult, ["braw0", "cs"], ["ph"])
                        TT("dve", ph2[:, 0:512], braw[:, 1, ct, :], fre[:, fs], ALU.mult, ["braw1", "sn"], ["ph2"])
                        TT("dve", Bblk[:, ct, 512:1024], ph[:, 0:512], ph2[:, 0:512], ALU.add, ["ph", "ph2"], ["Bblk"])
                    cc = st_("cc", [128, 3, 16])
                    cx.dma("sp", "c1", cc[:, 0, :], lamre_col, writes=["cc0"])
                    cx.dma("sp", "c2", cc[:, 1, :], lamim_col, writes=["cc1"])
                    cx.dma("sp", "c4", cc[:, 2, :], logdt_col, writes=["cc2"])
                    ACT(cc[:, 2, :], cc[:, 2, :], AF.Exp, ["cc2"], ["cc2"])
                    TT("dve", cc[:, 0, :], cc[:, 0, :], cc[:, 2, :], ALU.mult, ["cc0", "cc2"], ["cc0"])
                    TT("dve", cc[:, 1, :], cc[:, 1, :], cc[:, 2, :], ALU.mult, ["cc1", "cc2"], ["cc1"])
                    tpos = st_("tpos", [128, 128])
                    TS("dve", tpos[:], iotas[:, 1:129], parsb[:, 0:1], ALU.add, ["iotas", "par"], ["tpos"])
                    for p in range(16):
                        ACT(mag[:, p * 128:(p + 1) * 128], tpos[:], AF.Exp, ["tpos", "cc0"], ["mag"], scale=cc[:, 0, p:p + 1])
                        TS("dve", ph[:, p * 128:(p + 1) * 128], tpos[:], cc[:, 1, p:p + 1], ALU.mult, ["tpos", "cc1"], ["ph"])
                    sincos(2048)
                    TT("dve", Qre[:].rearrange("p a t -> p (a t)"), mag[:], cs[:], ALU.mult, ["mag", "cs"], ["Qre"])
                    TT("dve", Qim[:].rearrange("p a t -> p (a t)"), mag[:], sn[:], ALU.mult, ["mag", "sn"], ["Qim"])
                    ACT(mag[:, 0:16], cc[:, 0, :], AF.Exp, ["cc0", "mag"], ["mag"], scale=256.0)
                    TS("dve", ph[:, 0:16], cc[:, 1, :], 256.0, ALU.mult, ["cc1", "ph"], ["ph"])
                    sincos(16)
                    TT("dve", A256[:, 0, :], mag[:, 0:16], cs[:, 0:16], ALU.mult, ["mag", "cs"], ["A256"])
                    TT("dve", A256[:, 1, :], mag[:, 0:16], sn[:, 0:16], ALU.mult, ["mag", "sn", "A256"], ["A256"])
                    cx.barrier()

                u2 = [sc_("u2_%d" % i, [128, 4, 2, 128], BF16) for i in range(2)]
                sbq = [[sc_("sbq%d_%d" % (x, q), [128, 2048], BF16) for q in range(4)] for x in range(2)]
                bus = [[sc_("bus%d_%d" % (i, r), [128, 512]) for r in range(2)] for i in range(2)]
                carry = [sc_("carry%d" % i, [128, 2, 16]) for i in range(2)]
                cs_ = sc_("csum", [128, 2, 16]); cm = sc_("cmul", [128, 4, 16])
                spre = [sc_("spre%d" % i, [128, 4, 128]) for i in range(2)]
                spim = [sc_("spim%d" % i, [128, 4, 128]) for i in range(2)]
                mq = [[sc_("mq%d_%d" % (i, k), [128, 4, 128], BF16) for i in range(4)] for k in range(2)]
                ypre = sc_("ypre", [128, 4, 128]); yg = sc_("yg", [128, 4, 128]); ygb = sc_("ygb", [128, 4, 128], BF16)
                sg = sc_("sg", [128, 4, 128]); y2 = sc_("y2", [128, 4, 128]); sq = sc_("sq", [128, 4, 128], BF16)
                rs = sc_("rsC", [128, 128]); rs2 = sc_("rsC2", [128, 128])
                sst = [sc_("sst%d" % i, [128, 4, 128], BF16) for i in range(2)]
                bu1 = [psc("bu0_%d" % r, [128, 512]) for r in range(2)]
                bu = [bu1, bu1]
                ypsC = [psc("ypsC%d" % i, [128, 512]) for i in range(2)]
                Sps = [psc("Sps%d" % r, [128, 4, 128]) for r in range(2)]
                totp = psc("totp", [128, 16, 2])
                misc = psc("miscC", [128, 4, 128])
                cx.op("dve", lambda g: g.memset(carry[0][:], 0.0), writes=["carry0"])
                st = {"nbu": 0, "nrd": 0}
                SBKC = [["sbq%d_%d_%d" % (x, q, c_) for x in range(2) for q in range(4)] for c_ in range(4)]
                SBK = [k_ for c_ in range(4) for k_ in SBKC[c_]]

                def load_u(j):
                    sl = j % 2
                    cx.dma("sp", "u2a%d" % sl, u2[sl][:, :, 0, :],
                           uT_d[:, :, j * 128:(j + 1) * 128].rearrange("c p t -> p c t"), writes=["u2a%d" % sl])
                    cx.dma("sp", "u2b%d" % sl, u2[sl][:, :, 1, :],
                           uT_d[:, :, (NOWN + j) * 128:(NOWN + j + 1) * 128].rearrange("c p t -> p c t"),
                           writes=["u2b%d" % sl])

                def prescale_iter(j, X, ct):
                    sl = j % 2
                    UK = ["u2a%d" % sl, "u2b%d" % sl]
                    Pre = PAre if X == 0 else PBre
                    Pim = PAim if X == 0 else PBim
                    b = st["nbu"] % 2
                    st["nbu"] += 1
                    fs = slice(ct * 512, (ct + 1) * 512)
                    for r in range(2):
                        cx.op("pe", lambda g: g.matmul(bu[b][r][:], u2[sl][:, ct, X, :],
                                                       Bblk[:, ct, r * 512:(r + 1) * 512], start=True, stop=True),
                              reads=UK + ["Bblk"], writes=["bu0_%d" % r])
                    Q = sbq[X]
                    TT("dve", Q[0][:, fs], bu[b][0][:], Pre[:, fs], ALU.mult, ["bu0_0"], ["sbq%d_0_%d" % (X, ct)])
                    TT("dve", Q[1][:, fs], bu[b][1][:], Pim[:, fs], ALU.mult, ["bu0_1"], ["sbq%d_1_%d" % (X, ct)])
                    TT("dve", Q[2][:, fs], bu[b][0][:], Pim[:, fs], ALU.mult, ["bu0_0"], ["sbq%d_2_%d" % (X, ct)])
                    TT("dve", Q[3][:, fs], bu[b][1][:], Pre[:, fs], ALU.mult, ["bu0_1"], ["sbq%d_3_%d" % (X, ct)])

                def tail_steps(j):
                    sl = j % 2
                    so = sst[sl]
                    steps = []

                    def s0():
                        ACT(yg[:], ypre[:], AF.Gelu, ["ypre"], ["yg"])
                        cx.op("pool", lambda g: g.tensor_copy(out=ygb[:], in_=yg[:]), reads=["yg"], writes=["ygb"])

                    def s1():
                        for co in range(4):
                            for kt in range(4):
                                cx.op("pe", lambda g: g.matmul(misc[:, co, :], wglu[:, kt, co * 128:(co + 1) * 128],
                                                               ygb[:, kt, :], start=(kt == 0), stop=(kt == 3)),
                                      reads=["wglu", "ygb"], writes=["misc"])
                        for co in range(4):
                            ACT(sg[:, co, :], misc[:, co, :], AF.Sigmoid, ["misc", "scol"], ["sg"], bias=scol[:, 1, co:co + 1])

                    def s2():
                        TT("dve", y2[:], yg[:], sg[:], ALU.mult, ["yg", "sg"], ["y2"])
                        TT("pool", sq[:], y2[:], y2[:], ALU.mult, ["y2"], ["sq"])

                    def s3():
                        for kt in range(4):
                            cx.op("pe", lambda g: g.matmul(misc[:, 0, :], onesb[:], sq[:, kt, :], start=(kt == 0), stop=(kt == 3)),
                                  reads=["onesb", "sq"], writes=["misc"])

                    def s4():
                        TS("dve", rs[:], misc[:, 0, :], 1.0 / 512, ALU.mult, ["misc"], ["rs"], EPS, ALU.add)
                        ACT(rs2[:], rs[:], AF.Ln, ["rs"], ["rs2"])
                        ACT(rs2[:], rs2[:], AF.Exp, ["rs2"], ["rs2"], scale=-0.5)

                    def s5():
                        for ct in range(4):
                            cx.op("dve", lambda g: g.scalar_tensor_tensor(out=so[:, ct, :], in0=y2[:, ct, :],
                                                                          scalar=scol[:, 2, ct:ct + 1], in1=rs2[:],
                                                                          op0=ALU.mult, op1=ALU.mult),
                                  reads=["y2", "scol", "rs2"], writes=["sst%d" % sl])
                        cx.dma("sp", "sst%d" % sl, mixT_d[4:8, :, j * 128:(j + 1) * 128].rearrange("c p t -> p c t"),
                               so[:], reads=["sst%d" % sl])
                    return [s0, s1, s2, s3, s4, s5]

                def totals(j):
                    for p in range(16):
                        cols = slice(p * 128, (p + 1) * 128)
                        for r in range(2):
                            terms = [(x, 2 * r + q, (onesn if (r == 0 and q == 1) else onesb)) for x in range(2) for q in range(2)]
                            for ti, (x, q, ov) in enumerate(terms):
                                cx.op("pe", lambda g: g.matmul(totp[:, p, r:r + 1], sbq[x][q][:, cols], ov[:, 0:1],
                                                               start=(ti == 0), stop=(ti == 3)),
                                      reads=SBK + ["onesb", "onesn"], writes=["totp"])

                def round_S(j, rd):
                    cin = carry[j % 2]; ckin = "carry%d" % (j % 2)
                    k = rd % 2
                    for r in range(2):
                        for pp in range(4):
                            cols = slice((rd * 4 + pp) * 128, (rd * 4 + pp + 1) * 128)
                            terms = []
                            for x in range(2):
                                tp_, tn_ = (triA, triAn) if x == 0 else (triB, triBn)
                                terms.append((x, 2 * r, tp_))
                                terms.append((x, 2 * r + 1, tn_ if r == 0 else tp_))
                            for ti, (x, q, tm) in enumerate(terms):
                                cx.op("pe", lambda g: g.matmul(Sps[r][:, pp, :], sbq[x][q][:, cols], tm[:],
                                                               start=(ti == 0), stop=(ti == 3)),
                                      reads=SBKC[rd] + ["triA", "triB", "triAn", "triBn"], writes=["Sps%d" % r])
                    for pp in range(4):
                        p = rd * 4 + pp
                        ACT(spre[k][:, pp, :], Sps[0][:, pp, :], AF.Identity, ["Sps0", ckin], ["spre%d" % k], bias=cin[:, 0, p:p + 1])
                        ACT(spim[k][:, pp, :], Sps[1][:, pp, :], AF.Identity, ["Sps1", ckin], ["spim%d" % k], bias=cin[:, 1, p:p + 1])

                def round_X(j, rd):
                    sl = j % 2
                    k = rd % 2
                    M = mq[k]
                    MK = ["mq%d_%d" % (i, k) for i in range(4)]
                    qs_ = slice(rd * 4, rd * 4 + 4)
                    TT("dve", M[0][:], spre[k][:], Qre[:, qs_, :], ALU.mult, ["spre%d" % k], [MK[0]])
                    TT("dve", M[1][:], spim[k][:], Qim[:, qs_, :], ALU.mult, ["spim%d" % k], [MK[1]])
                    TT("dve", M[2][:], spre[k][:], Qim[:, qs_, :], ALU.mult, ["spre%d" % k], [MK[2]])
                    TT("dve", M[3][:], spim[k][:], Qre[:, qs_, :], ALU.mult, ["spim%d" % k], [MK[3]])
                    cw = [Cre, Cren, Cimn, Cimn]
                    for pp in range(4):
                        p = rd * 4 + pp
                        for q in range(4):
                            cx.op("pe", lambda g: g.matmul(ypsC[rd % 2][:, 0:128], cw[q][:, p, :], M[q][:, pp, :],
                                                           start=(pp == 0 and q == 0), stop=(pp == 3 and q == 3)),
                                  reads=["Cre", "Cren", "Cimn", MK[q]], writes=["ypsC%d" % (rd % 2)])

                def round_Y(j, rd):
                    sl = j % 2
                    cx.op("dve", lambda g: g.scalar_tensor_tensor(out=ypre[:, rd, :], in0=u2[sl][:, rd, 0, :],
                                                                  scalar=scol[:, 0, rd:rd + 1], in1=ypsC[rd % 2][:, 0:128],
                                                                  op0=ALU.mult, op1=ALU.add),
                          reads=["ypsC%d" % (rd % 2), "scol", "u2a%d" % sl], writes=["ypre"])

                def carry_update(j):
                    cin = carry[j % 2]; cout = carry[(j + 1) % 2]
                    ckin = "carry%d" % (j % 2); ckout = "carry%d" % ((j + 1) % 2)
                    TT("dve", cs_[:, 0, :], cin[:, 0, :], totp[:, :, 0], ALU.add, [ckin, "totp"], ["csum"])
                    TT("dve", cs_[:, 1, :], cin[:, 1, :], totp[:, :, 1], ALU.add, [ckin, "totp", "csum"], ["csum"])
                    TT("dve", cm[:, 0, :], A256[:, 0, :], cs_[:, 0, :], ALU.mult, ["csum"], ["cm"])
                    TT("dve", cm[:, 1, :], A256[:, 1, :], cs_[:, 1, :], ALU.mult, ["csum", "cm"], ["cm"])
                    TT("dve", cm[:, 2, :], A256[:, 0, :], cs_[:, 1, :], ALU.mult, ["csum", "cm"], ["cm"])
                    TT("dve", cm[:, 3, :], A256[:, 1, :], cs_[:, 0, :], ALU.mult, ["csum", "cm"], ["cm"])
                    TT("dve", cout[:, 0, :], cm[:, 0, :], cm[:, 1, :], ALU.subtract, ["cm"], [ckout])
                    TT("dve", cout[:, 1, :], cm[:, 2, :], cm[:, 3, :], ALU.add, ["cm", ckout], [ckout])

                load_u(0)
                pending = []
                for j in range(NOWN):
                    if j + 1 < NOWN:
                        load_u(j + 1)
                    it = 0
                    for ct in range(4):
                        for X in range(2):
                            prescale_iter(j, X, ct)
                            if pending and it < len(pending):
                                pending[it]()
                            it += 1
                        round_S(j, ct)
                        if ct >= 1:
                            round_X(j, ct - 1)
                        if ct >= 2:
                            round_Y(j, ct - 2)
                    pending = []
                    round_X(j, 3)
                    round_Y(j, 2)
                    totals(j)
                    round_Y(j, 3)
                    carry_update(j)
                    pending = tail_steps(j)
                for stp in pending:
                    stp()
                cx.barrier()

        if "D" in phases:
            pd = contextlib.ExitStack()
            with pd:
                def sd(name, shape, dt=F32):
                    return pd.enter_context(nc.sbuf_tensor("sb_" + name, list(shape), dt))

                def psd(name, shape, dt=F32):
                    return pd.enter_context(nc.psum_tensor("ps_" + name, list(shape), dt))

                wout = sd("wout", [128, 8, D], BF16)
                cx.dma("pool", "c5", wout[:], wout_in.rearrange("(k p) n -> p k n", p=128), writes=["wout"])
                wrt = sd("wrt", [128, 8, NE], BF16)
                cx.dma("pool", "c6", wrt[:], wrt_in.rearrange("(k p) n -> p k n", p=128), writes=["wrt"])
                lnf = sd("lnf", [128, D])
                cx.dma("sp", "c1", lnf[:], lnffn_rep, writes=["lnf"])
                brt = sd("brt", [128, NE])
                cx.dma("sp", "c2", brt[:], brt_rep, writes=["brt"])
                stri = sd("stri", [128, 128], BF16)
                triAd = sd("triAd", [128, 128], BF16)
                cx.dma("pool", "c7", triAd[:], triA_in, writes=["triAd"])
                cx.op("dve", lambda g: g.tensor_tensor(out=stri[:], in0=triAd[:], in1=ident[:], op=ALU.subtract),
                      reads=["triAd", "ident"], writes=["stri"])
                offs = sd("offs", [128, NE])
                cx.op("dve", lambda g: g.memset(offs[:], 0.0), writes=["offs"])
                bst = sd("bst", [128, NBLK])
                cx.dma("sp", "c3", bst[:], bstart_rep, writes=["bst"])

                mixb = [sd("mixb%d" % i, [128, 8, 128], BF16) for i in range(2)]
                xb = [sd("xb%d" % i, [128, D]) for i in range(2)]
                hb = [sd("hb%d" % i, [128, D]) for i in range(2)]
                hn_all = sd("hn_all", [128, NOWN, D], BF16)
                lg_all = sd("lg_all", [128, NOWN, NE])
                pos_all = sd("pos_all", [128, NOWN, NE])
                mx_all = sd("mx_all", [128, NOWN, 8])
                hnT = [sd("hnT%d" % i, [128, 8, 128], BF16) for i in range(2)]
                junkd = sd("junkD", [128, D], BF16)
                sd1 = [sd("sd1_%d" % i, [128, 4]) for i in range(2)]
                negm = [sd("negm%d" % i, [128, 1]) for i in range(2)]
                ex = [sd("ex%d" % i, [128, 4]) for i in range(2)]
                esum = [sd("esum%d" % i, [128, 2]) for i in range(2)]
                Mf = [sd("Mf%d" % i, [128, NE]) for i in range(2)]
                Mb = [sd("Mb%d" % i, [128, NE], BF16) for i in range(2)]
                sbase = sd("sbase", [128, NE])
                oh = sd("oh", [128, NE]); slotf = sd("slotf", [128, 4])
                hps = [psd("hps%d" % i, [128, 512]) for i in range(2)]
                tpd = [psd("tpD%d" % i, [128, 1024], BF16) for i in range(2)]
                lps = [psd("lpsD%d" % i, [128, 512]) for i in range(2)]
                cps = [psd("cpsD%d" % i, [128, 512]) for i in range(2)]
                ablk = [sd("ablk%d" % i, [128, 512], BF16) for i in range(2)]

                def load_d(j):
                    sl = j % 2
                    cx.dma("sp", "mixb%d" % sl, mixb[sl][:, 4:8, :], mixT_d[4:8, :, j * 128:(j + 1) * 128].rearrange("c p t -> p c t"),
                           writes=["mixs%d" % sl])
                    cx.dma("sp", "ablk%d" % sl, ablk[sl][:], a_d[j * 128:(j + 1) * 128, :], writes=["ablk%d" % sl])
                    cx.dma("sp", "xb%d" % sl, xb[sl][:], x_perm[j * 128:(j + 1) * 128, :], writes=["xb%d" % sl])

                def stage1(j):
                    sl = j % 2
                    T = tpd[sl]; TK = "tpd%d" % sl
                    for c4 in range(4):
                        cx.op("pe", lambda g: g.transpose(out=T[:, c4 * 128:(c4 + 1) * 128], in_=ablk[sl][:, c4 * 128:(c4 + 1) * 128],
                                                          identity=ident[:]), reads=["ablk%d" % sl, "ident"], writes=[TK])
                    cx.op("act", lambda g: g.activation(out=mixb[sl][:, 0:4, :].rearrange("p k t -> p (k t)"), in_=T[:, 0:512],
                                                        func=AF.Copy), reads=[TK], writes=["mixa%d" % sl])
                    for half in range(2):
                        for kt in range(8):
                            cx.op("pe", lambda g: g.matmul(hps[half][:], mixb[sl][:, kt, :],
                                                           wout[:, kt, half * 512:(half + 1) * 512],
                                                           start=(kt == 0), stop=(kt == 7)),
                                  reads=["mixa%d" % sl, "mixs%d" % sl, "wout"], writes=["hps%d" % half])
                        cx.op("dve", lambda g: g.tensor_tensor(out=hb[sl][:, half * 512:(half + 1) * 512], in0=hps[half][:],
                                                               in1=xb[sl][:, half * 512:(half + 1) * 512], op=ALU.add),
                              reads=["hps%d" % half, "xb%d" % sl], writes=["hb%d_%d" % (sl, half)])
                    HB = ["hb%d_0" % sl, "hb%d_1" % sl]
                    cx.dma("sp", "hst%d" % sl, h_d[j * 128:(j + 1) * 128, :], hb[sl][:], reads=HB)
                    S1 = sd1[sl]; SK = "sd1_%d" % sl
                    cx.op("act", lambda g: g.activation(out=junkd[:], in_=hb[sl][:], func=AF.Square, accum_out=S1[:, 0:1]),
                          reads=HB, writes=["junkD", SK])
                    cx.op("dve", lambda g: g.tensor_scalar(out=S1[:, 1:2], in0=S1[:, 0:1], scalar1=1.0 / D, scalar2=EPS,
                                                           op0=ALU.mult, op1=ALU.add), reads=[SK], writes=[SK + "b"])
                    cx.op("act", lambda g: g.activation(out=S1[:, 2:3], in_=S1[:, 1:2], func=AF.Ln), reads=[SK + "b"], writes=[SK + "c"])
                    cx.op("act", lambda g: g.activation(out=S1[:, 2:3], in_=S1[:, 2:3], func=AF.Exp, scale=-0.5),
                          reads=[SK + "c"], writes=[SK + "c"])
                    cx.op("dve", lambda g: g.scalar_tensor_tensor(out=hn_all[:, j, :], in0=hb[sl][:], scalar=S1[:, 2:3], in1=lnf[:],
                                                                  op0=ALU.mult, op1=ALU.mult),
                          reads=HB + [SK + "c", "lnf"], writes=["hn_%d" % j])

                def stage2(j):
                    sl = j % 2
                    HN = "hn_%d" % j
                    T = tpd[sl]; TK = "tpd%d" % sl
                    for kt in range(8):
                        cx.op("pe", lambda g: g.transpose(out=T[:, kt * 128:(kt + 1) * 128], in_=hn_all[:, j, kt * 128:(kt + 1) * 128],
                                                          identity=ident[:]), reads=[HN, "ident"], writes=[TK])
                    cx.op("act", lambda g: g.activation(out=hnT[sl][:].rearrange("p k t -> p (k t)"), in_=T[:], func=AF.Copy),
                          reads=[TK], writes=["hnT%d" % sl])
                    L = lps[sl]; LK = "lps%d" % sl
                    for kt in range(8):
                        cx.op("pe", lambda g: g.matmul(L[:, 0:NE], hnT[sl][:, kt, :], wrt[:, kt, :], start=(kt == 0), stop=(kt == 7)),
                              reads=["hnT%d" % sl, "wrt"], writes=[LK])
                    LG = "lg_%d" % j
                    cx.op("dve", lambda g: g.tensor_tensor(out=lg_all[:, j, :], in0=L[:, 0:NE], in1=brt[:], op=ALU.add),
                          reads=[LK, "brt"], writes=[LG])
                    MX = "mx_%d" % j
                    cx.op("dve", lambda g: g.max(out=mx_all[:, j, :], in_=lg_all[:, j, :]), reads=[LG], writes=[MX])
                    NK = "negm%d" % sl
                    cx.op("dve", lambda g: g.tensor_scalar(out=negm[sl][:], in0=mx_all[:, j, 0:1], scalar1=-1.0, scalar2=None, op0=ALU.mult),
                          reads=[MX], writes=[NK])
                    cx.op("act", lambda g: g.activation(out=ex[sl][:], in_=mx_all[:, j, 0:4], func=AF.Exp, bias=negm[sl][:],
                                                        accum_out=esum[sl][:, 0:1]),
                          reads=[MX, NK], writes=["ex%d" % sl, "esum%d" % sl])
                    cx.op("dve", lambda g: g.reciprocal(out=esum[sl][:, 1:2], in_=esum[sl][:, 0:1]), reads=["esum%d" % sl],
                          writes=["esumb%d" % sl])
                    cx.op("dve", lambda g: g.tensor_scalar(out=gates_all[:, j, :], in0=ex[sl][:], scalar1=esum[sl][:, 1:2], scalar2=None,
                                                           op0=ALU.mult), reads=["ex%d" % sl, "esumb%d" % sl], writes=["gates%d" % j])
                    cx.op("dve", lambda g: g.tensor_scalar(out=Mf[sl][:], in0=lg_all[:, j, :], scalar1=mx_all[:, j, 3:4], scalar2=None,
                                                           op0=ALU.is_ge), reads=[LG, MX], writes=["Mf%d" % sl])
                    cx.op("dve", lambda g: g.tensor_copy(out=Mb[sl][:], in_=Mf[sl][:]), reads=["Mf%d" % sl], writes=["Mb%d" % sl])
                    Cp = cps[sl]; CK = "cps%d" % sl
                    cx.op("pe", lambda g: g.matmul(Cp[:, 0:NE], stri[:], Mb[sl][:], start=True, stop=True),
                          reads=["stri", "Mb%d" % sl], writes=[CK])
                    cx.op("pe", lambda g: g.matmul(Cp[:, 64:64 + NE], onesb[:], Mb[sl][:], start=True, stop=True),
                          reads=["onesb", "Mb%d" % sl], writes=[CK])
                    cx.op("dve", lambda g: g.tensor_tensor(out=pos_all[:, j, :], in0=Cp[:, 0:NE], in1=offs[:], op=ALU.add),
                          reads=[CK, "offs"], writes=["pos_%d" % j])
                    cx.op("dve", lambda g: g.tensor_tensor(out=offs[:], in0=Cp[:, 64:64 + NE], in1=offs[:], op=ALU.add),
                          reads=[CK, "offs"], writes=["offs"])

                load_d(0)
                if NOWN > 1:
                    load_d(1)
                stage1(0)
                for j in range(NOWN):
                    if j + 1 < NOWN:
                        stage1(j + 1)
                    if j + 2 < NOWN:
                        load_d(j + 2)
                    stage2(j)
                cx.dma("sp", "c1", cnt_out, offs[:], reads=["offs"])
                padded = sd("padded", [128, NE]); pend = sd("pend", [128, NE]); pstart = sd("pstart", [128, NE])
                nbi = sd("nbi", [128, NE], I32)
                cx.op("dve", lambda g: g.tensor_scalar(out=padded[:], in0=offs[:], scalar1=511.0, scalar2=1.0 / 512,
                                                       op0=ALU.add, op1=ALU.mult), reads=["offs"], writes=["padded"])
                cx.op("dve", lambda g: g.tensor_scalar(out=nbi[:], in0=padded[:], scalar1=-511.0 / 1024, scalar2=None, op0=ALU.add),
                      reads=["padded"], writes=["nbi"])
                cx.op("dve", lambda g: g.tensor_copy(out=padded[:], in_=nbi[:]), reads=["nbi"], writes=["padded"])
                cx.op("dve", lambda g: g.tensor_scalar(out=padded[:], in0=padded[:], scalar1=512.0, scalar2=None, op0=ALU.mult),
                      reads=["padded"], writes=["padded"])
                cx.op("dve", lambda g: g.tensor_copy(out=pend[:, 0:1], in_=padded[:, 0:1]), reads=["padded"], writes=["pend"])
                for e_ in range(1, NE):
                    cx.op("dve", lambda g: g.tensor_tensor(out=pend[:, e_:e_ + 1], in0=pend[:, e_ - 1:e_], in1=padded[:, e_:e_ + 1],
                                                           op=ALU.add), reads=["padded", "pend"], writes=["pend"])
                cx.op("dve", lambda g: g.tensor_tensor(out=pstart[:], in0=pend[:], in1=padded[:], op=ALU.subtract),
                      reads=["pend", "padded"], writes=["pstart"])
                bacc = sd("bacc", [128, NBLK])
                cx.op("dve", lambda g: g.memset(bacc[:], 0.0), writes=["bacc"])
                for e_ in range(NE):
                    cx.op("dve", lambda g: g.scalar_tensor_tensor(out=bacc[:], in0=bst[:], scalar=pend[:, e_:e_ + 1], in1=bacc[:],
                                                                  op0=ALU.is_ge, op1=ALU.add),
                          reads=["bst", "pend", "bacc"], writes=["bacc"])
                cx.op("dve", lambda g: g.tensor_scalar(out=bacc[:], in0=bacc[:], scalar1=float(NE - 1), scalar2=None, op0=ALU.min),
                      reads=["bacc"], writes=["bacc"])
                widf = sd("widf", [128, NBLK, 8]); bidf = sd("bidf", [128, 2, NBLK]); kp = sd("kp", [128, 8])
                for kt in range(8):
                    cx.op("dve", lambda g: g.tensor_scalar(out=kp[:, kt:kt + 1], in0=iotas[:, 0:1], scalar1=float(kt * 128), scalar2=None,
                                                           op0=ALU.add), reads=["iotas", "kp"], writes=["kp"])
                for kt in range(8):
                    cx.op("dve", lambda g: g.tensor_scalar(out=widf[:, :, kt], in0=bacc[:], scalar1=float(D), scalar2=kp[:, kt:kt + 1],
                                                           op0=ALU.mult, op1=ALU.add), reads=["bacc", "kp", "widf"], writes=["widf"])
                cx.op("dve", lambda g: g.tensor_copy(out=widx[:], in_=widf[:]), reads=["widf"], writes=["widx"])
                cx.op("dve", lambda g: g.tensor_scalar(out=bidf[:, 0, :], in0=bacc[:], scalar1=128.0, scalar2=iotas[:, 0:1],
                                                       op0=ALU.mult, op1=ALU.add), reads=["bacc", "iotas"], writes=["bidf0"])
                cx.op("dve", lambda g: g.tensor_copy(out=bidx[:], in_=bidf[:, 0, :]), reads=["bidf0"], writes=["bidx"])
                cx.op("dve", lambda g: g.tensor_scalar(out=kp[:, 0:1], in0=iotas[:, 0:1], scalar1=float(NE), scalar2=None, op0=ALU.mult),
                      reads=["iotas", "kp", "widf"], writes=["kp"])
                cx.op("dve", lambda g: g.tensor_scalar(out=bidf[:, 1, :], in0=bacc[:], scalar1=kp[:, 0:1], scalar2=None, op0=ALU.add),
                      reads=["bacc", "kp"], writes=["bidf1"])
                cx.op("dve", lambda g: g.tensor_copy(out=gidx[:], in_=bidf[:, 1, :]), reads=["bidf1"], writes=["gidx"])
                for j in range(NOWN):
                    sl = j % 2
                    cx.op("dve", lambda g: g.tensor_tensor(out=sbase[:], in0=pos_all[:, j, :], in1=pstart[:], op=ALU.add),
                          reads=["pos_%d" % j, "pstart"], writes=["sbase"])
                    for k in range(4):
                        cx.op("dve", lambda g: g.tensor_scalar(out=oh[:], in0=lg_all[:, j, :], scalar1=mx_all[:, j, k:k + 1], scalar2=None,
                                                               op0=ALU.is_equal), reads=["lg_%d" % j, "mx_%d" % j], writes=["oh"])
                        cx.op("dve", lambda g: g.tensor_tensor(out=oh[:], in0=oh[:], in1=sbase[:], op=ALU.mult),
                              reads=["oh", "sbase"], writes=["oh"])
                        cx.op("dve", lambda g: g.tensor_reduce(out=slotf[:, k:k + 1], in_=oh[:], axis=AX.X, op=ALU.add),
                              reads=["oh"], writes=["slotf"])
                    cx.op("dve", lambda g: g.tensor_copy(out=slots_all[:, j, :], in_=slotf[:]), reads=["slotf"], writes=["slots%d" % j])
                    with cx.dma_group("scat%d" % sl):
                        for k in range(4):
                            cx.dma("pool", "scat%d" % sl, xpad_d, hn_all[:, j, :], reads=["hn_%d" % j, "slots%d" % j, "xpad"],
                                   indirect=dict(out_offset=bass.IndirectOffsetOnAxis(ap=slots_all[:, j, k:k + 1], axis=0),
                                                 in_offset=None, bounds_check=bc_reg, oob_is_err=False))
                cx.barrier()

        if "E" in phases:
            pe_ = contextlib.ExitStack()
            with pe_:
                def se(name, shape, dt=F32):
                    return pe_.enter_context(nc.sbuf_tensor("sb_" + name, list(shape), dt))

                def pse(name, shape, dt=F32):
                    return pe_.enter_context(nc.psum_tensor("ps_" + name, list(shape), dt))

                NSL = 512
                wgu = [se("wgu%d" % i, [128, 8, 2 * D], BF16) for i in range(2)]
                wdn = [se("wdn%d" % i, [128, 8, D], BF16) for i in range(2)]
                bdn = [se("bdn%d" % i, [128, D]) for i in range(2)]
                bgu = [se("bgu%d" % i, [128, 16]) for i in range(2)]
                xs = [se("xs%d" % i, [128, 4, D], BF16) for i in range(2)]
                xT = se("xT", [128, 8, NSL], BF16)
                gt = [se("gt%d" % i, [128, NSL]) for i in range(2)]
                sgm = [se("sgm%d" % i, [128, NSL]) for i in range(2)]
                ut = [se("ut%d" % i, [128, NSL]) for i in range(2)]
                p1 = [se("p1_%d" % i, [128, NSL]) for i in range(2)]
                actT = se("actT", [128, 8, NSL], BF16)
                ysb = [se("ysb%d" % i, [128, D]) for i in range(2)]
                tpe = [pse("tpE%d" % i, [128, 1024], BF16) for i in range(2)]
                gps = [pse("gps%d" % i, [128, 512]) for i in range(2)]
                ups = [pse("ups%d" % i, [128, 512]) for i in range(2)]
                yps = [pse("yps%d" % i, [128, 512]) for i in range(2)]

                wgu_rows = wgu_in.rearrange("e d n -> (e d) n")
                wdn_rows = wdn_in.rearrange("e d n -> (e d) n")
                bdn_rows = bdn_rep.rearrange("e p n -> (e p) n")
                bgu_rows = bgu_col.rearrange("p e n -> (p e) n")

                def load_w(bi):
                    sl = bi % 2
                    with cx.dma_group("wgu%d" % sl):
                        for kt in range(8):
                            cx.dma("pool", "wgu%d" % sl, wgu[sl][:, kt, :], wgu_rows, reads=["widx"], writes=["wgu%d" % sl],
                                   indirect=dict(out_offset=None, in_offset=bass.IndirectOffsetOnAxis(ap=widx[:, bi, kt:kt + 1], axis=0),
                                                 bounds_check=bcw_reg, oob_is_err=False))
                    with cx.dma_group("wdn%d" % sl):
                        for kt in range(8):
                            cx.dma("pool", "wdn%d" % sl, wdn[sl][:, kt, :], wdn_rows, reads=["widx"], writes=["wdn%d" % sl],
                                   indirect=dict(out_offset=None, in_offset=bass.IndirectOffsetOnAxis(ap=widx[:, bi, kt:kt + 1], axis=0),
                                                 bounds_check=bcw_reg, oob_is_err=False))
                    cx.dma("pool", "bdn%d" % sl, bdn[sl][:, :], bdn_rows, reads=["bidx"], writes=["bdn%d" % sl],
                           indirect=dict(out_offset=None, in_offset=bass.IndirectOffsetOnAxis(ap=bidx[:, bi:bi + 1], axis=0),
                                         bounds_check=bcb_reg, oob_is_err=False))
                    cx.dma("pool", "bgu%d" % sl, bgu[sl][:, :], bgu_rows, reads=["gidx"], writes=["bgu%d" % sl],
                           indirect=dict(out_offset=None, in_offset=bass.IndirectOffsetOnAxis(ap=gidx[:, bi:bi + 1], axis=0),
                                         bounds_check=bcb_reg, oob_is_err=False))

                ny = 0
                ntp = 0
                nft = 0

                def load_x(bi):
                    sl = bi % 2
                    r0 = bi * NSL
                    cx.dma("sp", "xs%d" % sl, xs[sl][:], xpad_d[r0:r0 + NSL, :].rearrange("(n p) d -> p n d", p=128),
                           writes=["xs%d" % sl])

                def emit_trans(bi):
                    nonlocal ntp
                    xsl = bi % 2
                    for kt in range(8):
                        tb = ntp % 2
                        ntp += 1
                        for n in range(4):
                            cx.op("pe", lambda g: g.transpose(out=tpe[tb][:, n * 128:(n + 1) * 128],
                                                              in_=xs[xsl][:, n, kt * 128:(kt + 1) * 128], identity=ident[:]),
                                  reads=["xs%d" % xsl, "ident"], writes=["tpe%d" % tb])
                        if kt % 2 == 0:
                            cx.op("act", lambda g: g.activation(out=xT[:, kt, :], in_=tpe[tb][:, 0:NSL], func=AF.Copy),
                                  reads=["tpe%d" % tb], writes=["xT%d" % kt])
                        else:
                            cx.op("dve", lambda g: g.tensor_copy(out=xT[:, kt, :], in_=tpe[tb][:, 0:NSL]),
                                  reads=["tpe%d" % tb], writes=["xT%d" % kt])

                load_w(0)
                load_x(0)
                for bi in range(NBLK):
                    wsl = bi % 2
                    xsl = bi % 2
                    if bi + 1 < NBLK:
                        load_w(bi + 1)
                        load_x(bi + 1)
                    r0 = bi * NSL
                    nsl = NSL
                    if bi == 0:
                        emit_trans(0)
                    XT = ["xT%d" % kt for kt in range(8)]
                    for ft in range(8):
                        b = nft % 2
                        nft += 1
                        for kt in range(8):
                            cx.op("pe", lambda g: g.matmul(gps[b][:], wgu[wsl][:, kt, ft * 128:(ft + 1) * 128],
                                                           xT[:, kt, :], start=(kt == 0), stop=(kt == 7)),
                                  reads=XT + ["wgu%d" % wsl], writes=["gps%d" % b])
                        for kt in range(8):
                            cx.op("pe", lambda g: g.matmul(ups[b][:], wgu[wsl][:, kt, D + ft * 128:D + (ft + 1) * 128],
                                                           xT[:, kt, :], start=(kt == 0), stop=(kt == 7)),
                                  reads=XT + ["wgu%d" % wsl], writes=["ups%d" % b])
                        cx.op("dve", lambda g: g.tensor_scalar(out=gt[b][:], in0=gps[b][:], scalar1=bgu[wsl][:, ft:ft + 1],
                                                               scalar2=7.0, op0=ALU.add, op1=ALU.min),
                              reads=["gps%d" % b, "bgu%d" % wsl], writes=["gt%d" % b])
                        cx.op("act", lambda g: g.activation(out=sgm[b][:], in_=gt[b][:], func=AF.Sigmoid, scale=1.702),
                              reads=["gt%d" % b], writes=["sgm%d" % b])
                        cx.op("dve", lambda g: g.tensor_scalar(out=ut[b][:], in0=ups[b][:], scalar1=bgu[wsl][:, 8 + ft:9 + ft],
                                                               scalar2=7.0, op0=ALU.add, op1=ALU.min),
                              reads=["ups%d" % b, "bgu%d" % wsl], writes=["ut%d" % b])
                        cx.op("dve", lambda g: g.tensor_scalar(out=ut[b][:], in0=ut[b][:], scalar1=-7.0, scalar2=1.0,
                                                               op0=ALU.max, op1=ALU.add),
                              reads=["ut%d" % b], writes=["ut%d" % b])
                        cx.op("dve", lambda g: g.tensor_tensor(out=p1[b][:], in0=gt[b][:], in1=sgm[b][:], op=ALU.mult),
                              reads=["gt%d" % b, "sgm%d" % b], writes=["p1_%d" % b])
                        cx.op("dve", lambda g: g.tensor_tensor(out=actT[:, ft, :], in0=p1[b][:], in1=ut[b][:], op=ALU.mult),
                              reads=["p1_%d" % b, "ut%d" % b], writes=["actT%d" % ft])
                    AT = ["actT%d" % ft for ft in range(8)]
                    if bi + 1 < NBLK:
                        emit_trans(bi + 1)
                    for n in range(4):
                        yb = ny % 2
                        ny += 1
                        for half in range(2):
                            for ft in range(8):
                                cx.op("pe", lambda g: g.matmul(yps[half][:], actT[:, ft, n * 128:(n + 1) * 128],
                                                               wdn[wsl][:, ft, half * 512:(half + 1) * 512],
                                                               start=(ft == 0), stop=(ft == 7)),
                                      reads=AT + ["wdn%d" % wsl], writes=["yps%d" % half])
                            cx.op("dve", lambda g: g.tensor_tensor(out=ysb[yb][:, half * 512:(half + 1) * 512], in0=yps[half][:],
                                                                   in1=bdn[wsl][:, half * 512:(half + 1) * 512], op=ALU.add),
                                  reads=["yps%d" % half, "bdn%d" % wsl], writes=["ysb%d_%d" % (yb, half)])
                        cx.dma("sp", "yst%d" % yb, y_d[r0 + n * 128:r0 + (n + 1) * 128, :], ysb[yb][:],
                               reads=["ysb%d_0" % yb, "ysb%d_1" % yb])
                cx.barrier()

        if "F" in phases:
            pf = contextlib.ExitStack()
            with pf:
                def sf(name, shape, dt=F32):
                    return pf.enter_context(nc.sbuf_tensor("sb_" + name, list(shape), dt))

                fin = sf("fin", [128, D])
                cx.dma("sp", "c1", fin[:], fin_rep, writes=["fin"])
                hf = [sf("hf%d" % i, [128, D]) for i in range(2)]
                gk = [[sf("gk%d_%d" % (i, k), [128, D]) for k in range(4)] for i in range(2)]
                acc = sf("accF", [128, D])
                junkf = sf("junkF", [128, D], BF16)
                sf1 = sf("sf1", [128, 4])
                ot = [sf("ot%d" % i, [128, D]) for i in range(2)]

                def load_f(j):
                    sl = j % 2
                    cx.dma("sp", "hf%d" % sl, hf[sl][:], h_d[j * 128:(j + 1) * 128, :], writes=["hf%d" % sl])
                    for k in range(4):
                        cx.op("pool", lambda g: g.memset(gk[sl][k][:], 0.0), writes=["gk%d_%d" % (sl, k)])
                    with cx.dma_group("gath%d" % sl):
                        for k in range(4):
                            cx.dma("pool", "gath%d" % sl, gk[sl][k][:, :], y_d, reads=["slots%d" % j, "gk%d_%d" % (sl, k)],
                                   writes=["gk%d_%d" % (sl, k)],
                                   indirect=dict(out_offset=None,
                                                 in_offset=bass.IndirectOffsetOnAxis(ap=slots_all[:, j, k:k + 1], axis=0),
                                                 bounds_check=bc_reg, oob_is_err=False))

                load_f(0)
                for j in range(NOWN):
                    sl = j % 2
                    if j + 1 < NOWN:
                        load_f(j + 1)
                    prev = hf[sl]
                    pk = "hf%d" % sl
                    for k in range(4):
                        cx.op("dve", lambda g: g.scalar_tensor_tensor(out=acc[:], in0=gk[sl][k][:], scalar=gates_all[:, j, k:k + 1],
                                                                      in1=prev[:], op0=ALU.mult, op1=ALU.add),
                              reads=["gk%d_%d" % (sl, k), "gates%d" % j, pk], writes=["acc"])
                        prev = acc
                        pk = "acc"
                    cx.op("act", lambda g: g.activation(out=junkf[:], in_=acc[:], func=AF.Square, accum_out=sf1[:, 0:1]),
                          reads=["acc"], writes=["junkF", "sf1"])
                    cx.op("dve", lambda g: g.tensor_scalar(out=sf1[:, 1:2], in0=sf1[:, 0:1], scalar1=1.0 / D, scalar2=EPS,
                                                           op0=ALU.mult, op1=ALU.add), reads=["sf1"], writes=["sf1b"])
                    cx.op("act", lambda g: g.activation(out=sf1[:, 2:3], in_=sf1[:, 1:2], func=AF.Ln), reads=["sf1b"], writes=["sf1c"])
                    cx.op("act", lambda g: g.activation(out=sf1[:, 2:3], in_=sf1[:, 2:3], func=AF.Exp, scale=-0.5),
                          reads=["sf1c"], writes=["sf1c"])
                    cx.op("dve", lambda g: g.scalar_tensor_tensor(out=ot[sl][:], in0=acc[:], scalar=sf1[:, 2:3], in1=fin[:],
                                                                  op0=ALU.mult, op1=ALU.mult),
                          reads=["acc", "sf1c", "fin"], writes=["ot%d" % sl])
                    cx.dma("sp", "ost%d" % sl, out[j * 128:(j + 1) * 128, :], ot[sl][:], reads=["ot%d" % sl])
                cx.barrier()

        cx.barrier()
    return nc


def _rep(v, n=128):
    v = np.asarray(v, np.float32).reshape(1, -1)
    return np.ascontiguousarray(np.broadcast_to(v, (n, v.shape[1])))


def prep(inputs, S, C):
    f32 = np.float32
    NB = S // 128
    x = np.asarray(inputs["x"], f32)
    positions = np.asarray(inputs["positions"], np.int32)
    w_in = np.asarray(inputs["w_in"], f32)[0]
    pidx = np.arange(128)
    hh_, mm_, rr_ = pidx // 32, (pidx // 16) % 2, pidx % 16
    def qk_cols(base):
        cols = [base + hh_ * 128 + mm_ * 64 + 16 * b_ + rr_ for b_ in range(4)]
        return np.concatenate(cols)
    partner = np.where(rr_ < 8, rr_ + 8, rr_ - 8)
    def perm_cols(base):
        return base + hh_ * 128 + mm_ * 64 + partner
    w_in_ext = np.ascontiguousarray(np.concatenate(
        [w_in[:, qk_cols(0)], w_in[:, qk_cols(512)], w_in[:, 1024:2048], w_in[:, perm_cols(0)], w_in[:, perm_cols(512)]], axis=1))
    inv_freq = (500000.0 ** (-np.arange(0, 16, 2, dtype=np.float32) / 16)).astype(f32)
    ropef = np.zeros((128, 2), f32)
    ropef[:, 0] = inv_freq[rr_ % 8]
    ropef[:, 1] = np.where(rr_ < 8, -1.0, 1.0)
    ident = np.eye(128, dtype=f32)
    kk = np.arange(128)
    triA = (kk[:, None] <= kk[None, :]).astype(f32)
    maskown = np.where(kk[:, None] <= kk[None, :], 0.0, NEG).astype(f32)
    iotas = np.zeros((128, 129), f32)
    iotas[:, 0] = kk
    iotas[:, 1:] = kk[None, :]
    shared = {
        "w_in_ext": w_in_ext, "ropef": ropef, "ident": ident, "triA": triA, "maskown": maskown,
        "lnmix_col": np.ascontiguousarray(np.asarray(inputs["ln_mix_g"], f32)[0].reshape(8, 128).T),
        "lamvec": np.ascontiguousarray(np.broadcast_to(
            np.stack([np.asarray(inputs[k], f32)[0] for k in ("lam_q1", "lam_k1", "lam_q2", "lam_k2")])[None],
            (128, 4, 64))),
        "diffg_rep": _rep(inputs["diff_norm_g"][0]),
        "iotas": iotas,
    }
    lre = np.asarray(inputs["ssm_lam_re"], f32)[0]
    lim = np.asarray(inputs["ssm_lam_im"], f32)[0]
    ldt = np.asarray(inputs["ssm_log_dt"], f32)[0]
    shared["lamre_row"] = _rep(lre.reshape(-1))
    shared["lamim_row"] = _rep(lim.reshape(-1))
    shared["logdt_row"] = _rep(np.repeat(ldt, 64))

    def col(a):
        return np.ascontiguousarray(a.reshape(16, 2, 64).transpose(1, 2, 0).reshape(128, 16))
    shared["lamre_col"] = col(lre)
    shared["lamim_col"] = col(lim)
    shared["logdt_col"] = col(np.repeat(ldt[:, None], 64, axis=1))
    bre = np.asarray(inputs["ssm_b_re"], f32)[0]
    bim = np.asarray(inputs["ssm_b_im"], f32)[0]
    cre = np.asarray(inputs["ssm_c_re"], f32)[0]
    cim = np.asarray(inputs["ssm_c_im"], f32)[0]

    def bblk(b):
        o = np.zeros((128, 4, 512), f32)
        for g in range(32):
            ct, gl = divmod(g, 8)
            o[gl * 16:(gl + 1) * 16, ct, gl * 64:(gl + 1) * 64] = b[g].T
        return o

    def cblk(c):
        o = np.zeros((128, 16, 128), f32)
        for g in range(32):
            pair, g2 = divmod(g, 2)
            gl = g % 8
            o[g2 * 64:(g2 + 1) * 64, pair, gl * 16:(gl + 1) * 16] = c[g].T
        return o
    shared["bre_blk"] = bblk(bre)
    shared["bim_blk"] = bblk(bim)
    shared["cre_blk"] = cblk(cre)
    shared["cim_blk"] = cblk(cim)
    ssmcols = np.zeros((128, 3, 4), f32)
    for i, k in enumerate(("ssm_d", "ssm_b_glu", "ssm_norm_g")):
        ssmcols[:, i, :] = np.asarray(inputs[k], f32)[0].reshape(4, 128).T
    shared["ssmcols"] = ssmcols
    shared["w_glu"] = np.ascontiguousarray(np.asarray(inputs["ssm_w_glu"], f32)[0])
    shared["w_out"] = np.ascontiguousarray(np.asarray(inputs["w_out"], f32)[0])
    shared["lnffn_rep"] = _rep(inputs["ln_ffn_g"][0])
    shared["w_router"] = np.ascontiguousarray(np.asarray(inputs["w_router"], f32)[0])
    shared["brt_rep"] = _rep(inputs["b_router"][0])
    nblk = (S // 2 * TOPK + NE * 511) // 512
    shared["bstart_rep"] = _rep(np.arange(nblk, dtype=f32) * 512.0)
    wgu = np.asarray(inputs["w_gate_up"], f32)[0]
    shared["w_gu"] = np.ascontiguousarray(np.concatenate([wgu[:, :, 0::2], wgu[:, :, 1::2]], axis=2))
    bgu = np.asarray(inputs["b_gate_up"], f32)[0]
    bgu2 = np.concatenate([bgu[:, 0::2], bgu[:, 1::2]], axis=1)
    shared["bgu_col"] = np.ascontiguousarray(bgu2.reshape(NE, 16, 128).transpose(2, 0, 1))
    shared["w_dn"] = np.ascontiguousarray(np.asarray(inputs["w_down"], f32)[0])
    bdn = np.asarray(inputs["b_down"], f32)[0]
    shared["bdn_rep"] = np.ascontiguousarray(np.broadcast_to(bdn[:, None, :], (NE, 128, D)))
    shared["fin_rep"] = _rep(inputs["final_norm_g"])

    in_maps = []
    for c in range(8):
        b, h = divmod(c, 2)
        own = np.arange(h, NB, 2)
        oth = np.arange(1 - h, NB, 2)
        order = np.concatenate([own, oth])
        tok = (order[:, None] * 128 + np.arange(128)[None, :]).reshape(-1)
        m = dict(shared)
        m["x_perm"] = np.ascontiguousarray(x[b][tok])
        m["pos_rep"] = np.ascontiguousarray(np.broadcast_to(positions[b][tok][None, :], (128, S)))
        parc = np.zeros((128, 4), f32)
        parc[:, 0] = 128.0 * h
        parc[:, 1] = 128.0 * (1 - h)
        m["par"] = parc
        m["triB"] = np.full((128, 128), float(h), f32)
        m["maskoth"] = np.full((128, 128), 0.0 if h == 1 else NEG, f32)
        in_maps.append(m)
    return in_maps


_CACHE = {}


def kernel(**inputs):
    S = int(np.asarray(inputs["x"]).shape[1])
    C = 0
    key = (S, C)
    if key not in _CACHE:
        _CACHE[key] = build(S, C)
    nc = _CACHE[key]
    in_maps = prep(inputs, S, C)
    res = run_bass_kernel_spmd(nc, in_maps, core_ids=list(range(8)))
    _CACHE["last_cnt"] = [np.asarray(r["cnt"])[0] for r in res.results]
    B = int(np.asarray(inputs["x"]).shape[0])
    NB = S // 128
    outp = np.zeros((B, S, D), np.float32)
    for c in range(8):
        b, h = divmod(c, 2)
        own = np.arange(h, NB, 2)
        tok = (own[:, None] * 128 + np.arange(128)[None, :]).reshape(-1)
        outp[b][tok] = res.results[c]["out"]
    return outp
```
